# Optimizing a Trainium2 kernel written in Bass

```python
import math
import jax, jax.numpy as jnp
from jax import lax
import numpy as np

D_MODEL = 2048
BATCH = 8
SEQ = 4096
DEPTH = 1

D_MIX = D_MODEL
HEAD_DIM = 128
DN_WIDTH = D_MIX // 2
DN_HEADS = DN_WIDTH // HEAD_DIM
FOX_WIDTH = D_MIX - DN_WIDTH
FOX_HEADS = FOX_WIDTH // HEAD_DIM
CONV_WIDTH = 4
DN_CHUNK = 64
Q_BLOCK = 128
MIX_SPLITS = (3 * DN_WIDTH, 4 * DN_WIDTH, 4 * DN_WIDTH + DN_HEADS, 4 * DN_WIDTH + 2 * DN_HEADS,
              4 * DN_WIDTH + 2 * DN_HEADS + 3 * FOX_WIDTH)
D_IN = 4 * DN_WIDTH + 2 * DN_HEADS + 3 * FOX_WIDTH + FOX_HEADS
N_GROUPS = 4
EXPERTS_PER_GROUP = 8
N_EXPERTS = N_GROUPS * EXPERTS_PER_GROUP
TOP_K = 2
D_EXPERT = D_MODEL // 4
MOE_BLOCK = 256
LN_EPS = 1e-5
NORM_EPS = 1e-6

kernel_name = 'hybrid_deltanet_fox_hmoe_deepnorm_adaln'


def layer_norm(x, g, b):
    xf = x.astype(jnp.float32)
    mu = jnp.mean(xf, axis=-1, keepdims=True)
    var = jnp.mean(jnp.square(xf - mu), axis=-1, keepdims=True)
    return ((xf - mu) * lax.rsqrt(var + LN_EPS) * g.astype(jnp.float32) + b.astype(jnp.float32)).astype(x.dtype)


def l2_normalize(x):
    return x * lax.rsqrt(jnp.sum(jnp.square(x), axis=-1, keepdims=True) + NORM_EPS)


def causal_short_conv(x, w):
    S = x.shape[1]
    xp = jnp.pad(x, ((0, 0), (CONV_WIDTH - 1, 0), (0, 0)))
    return sum(xp[:, i:i + S] * w[i] for i in range(CONV_WIDTH))


def gated_delta_rule(q, k, v, g, beta):
    B, H, S, Dk = q.shape
    Dv = v.shape[-1]
    C = DN_CHUNK
    nc = S // C
    chunk = lambda t: t.reshape(B, H, nc, C, *t.shape[3:])
    q = chunk(q * Dk ** -0.5)
    k = chunk(k)
    v = chunk(v)
    beta = chunk(beta)
    G = jnp.cumsum(chunk(g), axis=-1)
    incl = jnp.tril(jnp.ones((C, C), bool))
    strict = jnp.tril(jnp.ones((C, C), bool), -1)
    diff = G[..., :, None] - G[..., None, :]
    decay = jnp.where(incl, jnp.exp(jnp.where(incl, diff, 0.0)), 0.0)
    kb = k * beta[..., None]
    A = jnp.where(strict, jnp.einsum('bhncd,bhnsd->bhncs', kb, k) * decay, 0.0)
    eye = jnp.eye(C, dtype=jnp.float32)
    T = lax.linalg.triangular_solve(eye + A, jnp.broadcast_to(eye, A.shape),
                                    left_side=True, lower=True, unit_diagonal=True)
    u = T @ (v * beta[..., None])
    w = T @ (kb * jnp.exp(G)[..., None])
    qk = jnp.where(incl, jnp.einsum('bhncd,bhnsd->bhncs', q, k) * decay, 0.0)

    def step(state, inp):
        qc, kc, uc, wc, qkc, Gc = inp
        v_new = uc - wc @ state
        o = (qc * jnp.exp(Gc)[..., None]) @ state + qkc @ v_new
        G_last = Gc[..., -1:]
        state = state * jnp.exp(G_last)[..., None] + jnp.einsum(
            'bhcd,bhce->bhde', kc * jnp.exp(G_last - Gc)[..., None], v_new)
        return state, o

    state0 = jnp.zeros((B, H, Dk, Dv), jnp.float32)
    xs = tuple(jnp.moveaxis(t, 2, 0) for t in (q, k, u, w, qk, G))
    _, o = lax.scan(step, state0, xs)
    return jnp.moveaxis(o, 0, 2).reshape(B, H, S, Dv)


def forgetting_attention(q, k, v, log_f):
    B, H, S, Dh = q.shape
    nb = S // Q_BLOCK
    F = jnp.cumsum(log_f, axis=-1)
    qb = jnp.moveaxis(q.reshape(B, H, nb, Q_BLOCK, Dh), 2, 0)
    Fb = jnp.moveaxis(F.reshape(B, H, nb, Q_BLOCK), 2, 0)
    k_pos = jnp.arange(S)

    def block(inp):
        qi, Fi, i = inp
        logits = jnp.einsum('bhqd,bhkd->bhqk', qi, k).astype(jnp.float32) * Dh ** -0.5
        logits = logits + Fi[..., :, None] - F[..., None, :]
        q_pos = i * Q_BLOCK + jnp.arange(Q_BLOCK)
        logits = jnp.where(k_pos[None, :] <= q_pos[:, None], logits, -jnp.inf)
        p = jax.nn.softmax(logits, axis=-1)
        return jnp.einsum('bhqk,bhkd->bhqd', p.astype(v.dtype), v)

    o = lax.map(block, (qb, Fb, jnp.arange(nb)))
    return jnp.moveaxis(o, 0, 2).reshape(B, H, S, Dh)


def hybrid_mixer(h, w_in, conv_w, a_log, dt_bias, norm_w, f_bias, w_out):
    B, S, _ = h.shape
    proj = h @ w_in
    dn_qkv, dn_z, dn_b, dn_a, fox_qkv, fox_f = jnp.split(proj, MIX_SPLITS, axis=-1)
    heads = lambda t, nh: t.reshape(B, S, nh, -1).transpose(0, 2, 1, 3)
    f32 = jnp.float32
    dn_qkv = jax.nn.silu(causal_short_conv(dn_qkv, conv_w))
    dq, dk, dv = jnp.split(dn_qkv.astype(f32), 3, axis=-1)
    dq = l2_normalize(heads(dq, DN_HEADS))
    dk = l2_normalize(heads(dk, DN_HEADS))
    dv = heads(dv, DN_HEADS)
    beta = jax.nn.sigmoid(dn_b.astype(f32)).transpose(0, 2, 1)
    g = (-jnp.exp(a_log.astype(f32)) * jax.nn.softplus(dn_a.astype(f32) + dt_bias.astype(f32))).transpose(0, 2, 1)
    o_dn = gated_delta_rule(dq, dk, dv, g, beta).transpose(0, 2, 1, 3)
    z = dn_z.astype(f32).reshape(B, S, DN_HEADS, HEAD_DIM)
    o_dn = o_dn * lax.rsqrt(jnp.mean(jnp.square(o_dn), axis=-1, keepdims=True) + NORM_EPS) \
        * norm_w.astype(f32) * jax.nn.silu(z)
    o_dn = o_dn.reshape(B, S, DN_WIDTH).astype(h.dtype)
    fq, fk, fv = jnp.split(fox_qkv, 3, axis=-1)
    log_f = jax.nn.log_sigmoid(fox_f.astype(f32) + f_bias.astype(f32)).transpose(0, 2, 1)
    o_fox = forgetting_attention(heads(fq, FOX_HEADS), heads(fk, FOX_HEADS), heads(fv, FOX_HEADS), log_f)
    o_fox = o_fox.transpose(0, 2, 1, 3).reshape(B, S, FOX_WIDTH)
    return jnp.concatenate([o_dn, o_fox], axis=-1) @ w_out


def hier_moe(h, w_rg, b_rg, w_re, b_re, w_gate, w_up, w_down):
    B, S, D = h.shape
    hf = h.reshape(-1, D)
    N = hf.shape[0]
    f32 = jnp.float32
    g_logits = (hf @ w_rg + b_rg).astype(f32)
    g_prob = jax.nn.softmax(g_logits, axis=-1)
    g_idx = jnp.argmax(g_logits, axis=-1)
    g_w = jnp.take_along_axis(g_prob, g_idx[:, None], axis=-1)
    e_logits = jnp.einsum('nd,gde->nge', hf, w_re) + b_re
    e_logits = jnp.take_along_axis(e_logits, g_idx[:, None, None], axis=1)[:, 0].astype(f32)
    e_prob = jax.nn.softmax(e_logits, axis=-1)
    top_p, top_e = lax.top_k(e_prob, TOP_K)
    weights = g_w * top_p / jnp.sum(top_p, axis=-1, keepdims=True)
    expert = g_idx[:, None].astype(jnp.int32) * EXPERTS_PER_GROUP + top_e.astype(jnp.int32)
    M = N * TOP_K
    P = (-(-M // MOE_BLOCK) + N_EXPERTS) * MOE_BLOCK
    n_blocks = P // MOE_BLOCK
    e_flat = expert.reshape(-1)
    tok_flat = jnp.repeat(jnp.arange(N, dtype=jnp.int32), TOP_K)
    w_flat = weights.reshape(-1)
    order = jnp.argsort(e_flat)
    e_s, tok_s, w_s = e_flat[order], tok_flat[order], w_flat[order]
    sizes = jnp.zeros((N_EXPERTS,), jnp.int32).at[e_flat].add(1)
    padded = (sizes + MOE_BLOCK - 1) // MOE_BLOCK * MOE_BLOCK
    p_end = jnp.cumsum(padded)
    p_start = p_end - padded
    u_start = jnp.cumsum(sizes) - sizes
    dest = p_start[e_s] + jnp.arange(M, dtype=jnp.int32) - u_start[e_s]
    tok_buf = jnp.zeros((P,), jnp.int32).at[dest].set(tok_s)
    w_buf = jnp.zeros((P,), hf.dtype).at[dest].set(w_s.astype(hf.dtype))
    blk_expert = jnp.minimum(jnp.searchsorted(p_end, jnp.arange(n_blocks, dtype=jnp.int32) * MOE_BLOCK,
                                              side='right'), N_EXPERTS - 1)

    def expert_block(inp):
        tok, wt, e = inp
        xb = hf[tok]
        hid = jax.nn.silu(xb @ w_gate[e]) * (xb @ w_up[e])
        return (hid @ w_down[e]) * wt[:, None]

    y_buf = lax.map(expert_block, (tok_buf.reshape(n_blocks, MOE_BLOCK),
                                   w_buf.reshape(n_blocks, MOE_BLOCK), blk_expert))
    y = jax.ops.segment_sum(y_buf.reshape(P, D), tok_buf, num_segments=N)
    return y.reshape(B, S, D)


def setup_inputs(seed: int = 0) -> dict:
    key = jax.random.key(seed)
    ks = jax.random.split(key, 32)
    L = DEPTH
    f32 = jnp.float32
    nrm = lambda k, shape, scale: jax.random.normal(k, shape, f32) * scale
    beta_dn = (8 * DEPTH) ** -0.25
    s_in = D_MODEL ** -0.5
    x = nrm(ks[0], (BATCH, SEQ, D_MODEL), 1.0)
    c = nrm(ks[1], (BATCH, D_MODEL), 1.0)
    w_ada = nrm(ks[2], (L, D_MODEL, 6 * D_MODEL), 0.1 * s_in)
    b_ada = nrm(ks[3], (L, 6 * D_MODEL), 0.01)
    w_in = jnp.concatenate([
        nrm(ks[4], (L, D_MODEL, 2 * DN_WIDTH), s_in),
        nrm(ks[5], (L, D_MODEL, DN_WIDTH), s_in * beta_dn),
        nrm(ks[6], (L, D_MODEL, DN_WIDTH + 2 * DN_HEADS), s_in),
        nrm(ks[7], (L, D_MODEL, 2 * FOX_WIDTH), s_in),
        nrm(ks[8], (L, D_MODEL, FOX_WIDTH), s_in * beta_dn),
        nrm(ks[9], (L, D_MODEL, FOX_HEADS), s_in)], axis=-1)
    dn_conv_w = nrm(ks[10], (L, CONV_WIDTH, 3 * DN_WIDTH), CONV_WIDTH ** -0.5)
    dn_a_log = jnp.log(jax.random.uniform(ks[11], (L, DN_HEADS), f32, 1.0, 16.0))
    dt = jnp.exp(jax.random.uniform(ks[12], (L, DN_HEADS), f32, math.log(1e-3), math.log(1e-1)))
    dn_dt_bias = dt + jnp.log(-jnp.expm1(-dt))
    dn_norm_w = 1.0 + nrm(ks[13], (L, HEAD_DIM), 0.02)
    fox_f_bias = jax.random.uniform(ks[14], (L, FOX_HEADS), f32, 1.0, 5.0)
    w_out = nrm(ks[15], (L, D_MIX, D_MODEL), D_MIX ** -0.5 * beta_dn)
    ln1_g = 1.0 + nrm(ks[16], (L, D_MODEL), 0.02)
    ln1_b = nrm(ks[17], (L, D_MODEL), 0.02)
    w_router_group = nrm(ks[18], (L, D_MODEL, N_GROUPS), s_in)
    b_router_group = nrm(ks[19], (L, N_GROUPS), 0.01)
    w_router_expert = nrm(ks[20], (L, N_GROUPS, D_MODEL, EXPERTS_PER_GROUP), s_in)
    b_router_expert = nrm(ks[21], (L, N_GROUPS, EXPERTS_PER_GROUP), 0.01)
    w_gate = nrm(ks[22], (L, N_EXPERTS, D_MODEL, D_EXPERT), s_in)
    w_up = nrm(ks[23], (L, N_EXPERTS, D_MODEL, D_EXPERT), s_in)
    w_down = nrm(ks[24], (L, N_EXPERTS, D_EXPERT, D_MODEL), D_EXPERT ** -0.5 * beta_dn)
    ln2_g = 1.0 + nrm(ks[25], (L, D_MODEL), 0.02)
    ln2_b = nrm(ks[26], (L, D_MODEL), 0.02)
    return {'x': x, 'c': c, 'w_ada': w_ada, 'b_ada': b_ada, 'w_in': w_in, 'dn_conv_w': dn_conv_w,
            'dn_a_log': dn_a_log, 'dn_dt_bias': dn_dt_bias, 'dn_norm_w': dn_norm_w,
            'fox_f_bias': fox_f_bias, 'w_out': w_out, 'ln1_g': ln1_g, 'ln1_b': ln1_b,
            'w_router_group': w_router_group, 'b_router_group': b_router_group,
            'w_router_expert': w_router_expert, 'b_router_expert': b_router_expert,
            'w_gate': w_gate, 'w_up': w_up, 'w_down': w_down, 'ln2_g': ln2_g, 'ln2_b': ln2_b}


def reference(x, c, w_ada, b_ada, w_in, dn_conv_w, dn_a_log, dn_dt_bias, dn_norm_w, fox_f_bias,
              w_out, ln1_g, ln1_b, w_router_group, b_router_group, w_router_expert, b_router_expert,
              w_gate, w_up, w_down, ln2_g, ln2_b):
    alpha = (2 * DEPTH) ** 0.25
    for l in range(DEPTH):
        mod = jax.nn.silu(c) @ w_ada[l] + b_ada[l]
        sh1, sc1, g1, sh2, sc2, g2 = jnp.split(mod[:, None, :], 6, axis=-1)
        h = x * (1.0 + sc1) + sh1
        y = hybrid_mixer(h, w_in[l], dn_conv_w[l], dn_a_log[l], dn_dt_bias[l], dn_norm_w[l],
                         fox_f_bias[l], w_out[l])
        x = layer_norm(alpha * x + (1.0 + g1) * y, ln1_g[l], ln1_b[l])
        h = x * (1.0 + sc2) + sh2
        y = hier_moe(h, w_router_group[l], b_router_group[l], w_router_expert[l], b_router_expert[l],
                     w_gate[l], w_up[l], w_down[l])
        x = layer_norm(alpha * x + (1.0 + g2) * y, ln2_g[l], ln2_b[l])
    return x
```

```python
import os
import numpy as np
from contextlib import ExitStack
import concourse.bass as bass
import concourse.mybir as mybir
from concourse.bass_utils import run_bass_kernel_spmd

F32 = mybir.dt.float32
BF16 = mybir.dt.bfloat16
I32 = mybir.dt.int32
AF = mybir.ActivationFunctionType
ALU = mybir.AluOpType
AX = mybir.AxisListType

D = 2048
S = 4096
KC = D // 128
NT = S // 128
NG = S // 512
NH = 8
DIN = 7192
ALPHA = 2.0 ** 0.25


class Bank:
    __slots__ = ("acc",)

    def __init__(self):
        self.acc = {}


class Buf:
    __slots__ = ("name", "w", "r", "bank")

    def __init__(self, name="", bank=None):
        self.name = name
        self.w = None
        self.r = {}
        self.bank = bank


class KS:
    ENG = ("pe", "act", "dve", "pool", "sp")

    def __init__(self, nc, stack, n_dsem=12, same_eng_sync=True):
        self.nc = nc
        self.eng = {"pe": nc.tensor, "act": nc.scalar, "dve": nc.vector,
                    "pool": nc.gpsimd, "sp": nc.sync}
        self.same_eng_sync = same_eng_sync
        self.sems = {}
        self.cnt = {}
        for e in self.ENG:
            self.sems[e] = stack.enter_context(nc.semaphore("c_" + e))
            self.cnt[e] = 0
        self.dq = {}
        for q in ("sp", "pool", "act"):
            lst = []
            for i in range(n_dsem):
                key = "d_%s_%d" % (q, i)
                self.sems[key] = stack.enter_context(nc.semaphore(key))
                self.cnt[key] = 0
                lst.append(key)
            self.dq[q] = [lst, 0]
        self.waited = {e: {} for e in self.ENG}
        self.n_wait = 0
        self.n_inst = 0

    def _wait(self, e, dep):
        if dep is None:
            return
        key, val = dep
        if key == e and (e == "pe" or not self.same_eng_sync):
            return
        if self.waited[e].get(key, 0) >= val:
            return
        self.eng[e].wait_ge(self.sems[key], val)
        self.waited[e][key] = val
        self.n_wait += 1

    def _deps(self, e, reads, writes):
        for b in reads:
            self._wait(e, b.w)
        for b in writes:
            self._wait(e, b.w)
            for k, v in b.r.items():
                self._wait(e, (k, v))
        for b in list(reads) + list(writes):
            if b.bank is not None:
                for k, v in b.bank.acc.items():
                    if k != e:
                        self._wait(e, (k, v))

    def _mark(self, tag, reads, writes):
        key, val = tag
        for b in reads:
            if b.r.get(key, 0) < val:
                b.r[key] = val
        for b in writes:
            b.w = tag
            b.r = {}
        for b in list(reads) + list(writes):
            if b.bank is not None:
                b.bank.acc[key] = val

    def op(self, e, fn, reads=(), writes=()):
        self._deps(e, reads, writes)
        inst = fn(self.eng[e])
        self.cnt[e] += 1
        inst.then_inc(self.sems[e], 1)
        self._mark((e, self.cnt[e]), reads, writes)
        self.n_inst += 1
        return inst

    def dma(self, q, out, in_, reads=(), writes=(), fn=None, **kw):
        lst, idx = self.dq[q]
        key = lst[idx % len(lst)]
        self.dq[q][1] = idx + 1
        if self.cnt[key] > 0:
            self._wait(q, (key, self.cnt[key]))
        self._deps(q, reads, writes)
        if fn is not None:
            inst = fn(self.eng[q])
        else:
            inst = self.eng[q].dma_start(out=out, in_=in_, **kw)
        self.cnt[key] += 16
        inst.then_inc(self.sems[key], 16)
        self._mark((key, self.cnt[key]), reads, writes)
        self.n_inst += 1
        return inst

    def wait_all(self, e, bufs):
        for b in bufs:
            self._wait(e, b.w)

    def barrier(self):
        for e in self.ENG:
            for key, val in self.cnt.items():
                if val > 0 and key != e:
                    self._wait(e, (key, val))
            if e != "pe" and self.cnt[e] > 0 and self.same_eng_sync:
                self._wait(e, (e, self.cnt[e]))


def build_nc(debug=(), stop_after=None):
    nc = bass.Bass("TRN2", target_bir_lowering=False)
    dbg = set(debug)

    def din(name, shape, dt=F32):
        return nc.dram_tensor(name, list(shape), dt, kind="ExternalInput").ap()

    def dscr(name, shape, dt=F32):
        kind = "ExternalOutput" if name in dbg else "Internal"
        return nc.dram_tensor(name, list(shape), dt, kind=kind).ap()

    xT = din("xT", [D, S])
    xtok = din("x", [S, D])
    ccol = din("ccol", [128, KC])
    w_ada = din("w_ada", [D, 6 * D])
    b_ada = din("b_ada", [1, 6 * D])
    w_in = din("w_in", [D, DIN])
    convw = din("convw", [128, 24, 4])
    a_log = din("a_log", [1, NH])
    dt_bias = din("dt_bias", [1, NH])
    f_bias = din("f_bias", [1, NH])
    norm_w = din("norm_w", [128, 1])
    w_out = din("w_out", [D, D])
    ln1_g = din("ln1_g", [1, D]); ln1_b = din("ln1_b", [1, D])
    ln2_g = din("ln2_g", [1, D]); ln2_b = din("ln2_b", [1, D])
    w_r = din("w_r", [D, 36]); b_r = din("b_r", [1, 36])
    out = nc.dram_tensor("out", [S, D], F32, kind="ExternalOutput").ap()

    modrow = dscr("modrow", [1, 6 * D])
    projT = dscr("projT", [48, 128, S], BF16)
    vtok = dscr("vtok", [S, 1024], BF16)
    gat = dscr("gat", [S, 24], F32)

    with ExitStack() as st:
        ks = KS(nc, st, same_eng_sync=(os.environ.get("SES", "1") == "1"))
        cst = ExitStack(); st.enter_context(cst)

        def sbt(stack, name, shape, dt):
            return stack.enter_context(nc.sbuf_tensor(name, list(shape), dt))

        BANKS = {}

        def pst(stack, name, shape, dt=F32):
            full = 512 if dt == F32 else 1024
            t_ = stack.enter_context(nc.psum_tensor(name, [128, full], dt))
            BANKS[name] = Bank()
            P = shape[0]
            n = 1
            for d_ in shape[1:]:
                n *= d_
            v = t_[0:P, 0:n]
            if len(shape) == 3:
                v = v.rearrange("p (a b) -> p a b", b=shape[2])
            return v

        def PB(name):
            return Buf(name, BANKS[name])

        ident_f = sbt(cst, "ident_f", [128, 128], F32); B_ident = Buf("ident")
        ident_h = sbt(cst, "ident_h", [128, 128], BF16)
        ks.op("pool", lambda e: e.memset(ident_f[:], 1.0), writes=[B_ident])
        ks.op("pool", lambda e: e.affine_select(out=ident_f[:], in_=ident_f[:], pattern=[[-1, 128]],
                                                compare_op=ALU.is_equal, fill=0.0, base=0, channel_multiplier=1),
              reads=[B_ident], writes=[B_ident])
        ks.op("pool", lambda e: e.tensor_copy(ident_h[:], ident_f[:]), reads=[B_ident], writes=[B_ident])
        sc1c = sbt(cst, "sc1c", [128, KC], F32)
        sh1c = sbt(cst, "sh1c", [128, KC], F32)
        B_mc = Buf("modcols")
        B_modrow = Buf("modrow")

        with ExitStack() as ph:
            cc = sbt(ph, "cc", [128, KC], F32); B_cc = Buf()
            sc = sbt(ph, "sc", [128, KC], F32); B_sc = Buf()
            brow = sbt(ph, "brow", [1, 6 * D], F32); B_brow = Buf()
            mrow = sbt(ph, "mrow", [1, 6 * D], F32); B_mrow = Buf()
            wa = [sbt(ph, "wa%d" % i, [128, KC, 512], F32) for i in range(2)]; B_wa = [Buf(), Buf()]
            pm = [pst(ph, "pm%d" % i, [1, 512]) for i in range(2)]; B_pm = [PB("pm0"), PB("pm1")]
            ks.dma("sp", cc[:], ccol[:, :], writes=[B_cc])
            ks.dma("sp", brow[:], b_ada[:, :], writes=[B_brow])
            ks.op("act", lambda e: e.activation(out=sc[:], in_=cc[:], func=AF.Silu), reads=[B_cc], writes=[B_sc])
            w_ada_v = w_ada.rearrange("(kc p) n -> p kc n", p=128)
            NCG = 6 * D // 512
            for cg in range(NCG):
                i = cg % 2
                ks.dma("sp", wa[i][:], w_ada_v[:, :, cg * 512:(cg + 1) * 512], writes=[B_wa[i]])
                for kc in range(KC):
                    ks.op("pe", lambda e, kc=kc, i=i: e.matmul(pm[i][:], lhsT=sc[:, kc:kc + 1], rhs=wa[i][:, kc, :],
                                                             start=(kc == 0), stop=(kc == KC - 1)),
                          reads=[B_sc, B_wa[i]], writes=[B_pm[i]])
                ks.op("dve", lambda e, cg=cg, i=i: e.tensor_tensor(out=mrow[:, cg * 512:(cg + 1) * 512], in0=pm[i][:],
                                                                   in1=brow[:, cg * 512:(cg + 1) * 512], op=ALU.add),
                      reads=[B_pm[i], B_brow], writes=[B_mrow])
            ks.dma("sp", modrow[:, :], mrow[:], reads=[B_mrow], writes=[B_modrow])
            t16 = sbt(ph, "t16", [KC, 2, 128], F32); B_t16 = Buf()
            ks.dma("sp", t16[:, 0, :], modrow[0, 0:D].rearrange("(kc p) -> kc p", p=128), reads=[B_modrow], writes=[B_t16])
            ks.dma("sp", t16[:, 1, :], modrow[0, D:2 * D].rearrange("(kc p) -> kc p", p=128), reads=[B_modrow], writes=[B_t16])
            pt = pst(ph, "pt", [128, 2, KC]); B_pt = PB("pt")
            for j in range(2):
                ks.op("pe", lambda e, j=j: e.transpose(pt[:, j, :], t16[:, j, :], ident_f[0:KC, 0:KC]),
                      reads=[B_t16, B_ident], writes=[B_pt])
            ks.op("dve", lambda e: e.tensor_copy(sh1c[:], pt[:, 0, :]), reads=[B_pt], writes=[B_mc])
            ks.op("dve", lambda e: e.tensor_scalar(out=sc1c[:], in0=pt[:, 1, :], scalar1=1.0, scalar2=None, op0=ALU.add),
                  reads=[B_pt], writes=[B_mc])
            ks.barrier()
        if stop_after == 0:
            return _finish(nc, ks, [B_modrow])

        B_projT = [Buf("projT%d" % j) for j in range(48)]
        B_vtok = Buf("vtok"); B_gat = Buf("gat")
        with ExitStack() as ph:
            hT = sbt(ph, "hT", [128, KC, S], BF16); B_hT = [Buf() for _ in range(KC)]
            wf = [sbt(ph, "wf%d" % i, [128, KC, 256], F32) for i in range(2)]; B_wf = [Buf(), Buf()]
            xs = [wf[i][:, 0:8, :].rearrange("p a b -> p (a b)") for i in range(2)]; B_xs = B_wf
            xT_v = xT.rearrange("(kc p) t -> p kc t", p=128)
            n = 0
            for kc in range(KC):
                for hf in range(2):
                    i = n % 2; n += 1
                    ks.dma("sp", xs[i], xT_v[:, kc, hf * 2048:(hf + 1) * 2048], writes=[B_xs[i]])
                    ks.op("act", lambda e, kc=kc, hf=hf, i=i: e.activation(
                        out=hT[:, kc, hf * 2048:(hf + 1) * 2048], in_=xs[i], func=AF.Identity,
                        scale=sc1c[:, kc:kc + 1], bias=sh1c[:, kc:kc + 1]),
                        reads=[B_xs[i], B_mc], writes=[B_hT[kc]])
            wh = [sbt(ph, "wh%d" % i, [128, KC, 256], BF16) for i in range(2)]; B_wh = [Buf(), Buf()]
            stg_all = sbt(ph, "stg", [128, 2 * S], BF16)
            stg = [stg_all[:, i * S:(i + 1) * S] for i in range(2)]; B_stg = [Buf(), Buf()]
            vst = stg_all[:, :].rearrange("p (t c) -> p t c", c=256)
            gst = sbt(ph, "gst", [128, NT, 24], F32); B_gst = Buf()
            pp = [pst(ph, "pp%d" % i, [128, 512]) for i in range(4)]; B_pp = [PB("pp%d" % i) for i in range(4)]
            w_in_v = w_in.rearrange("(kc p) n -> p kc n", p=128)
            n_cg = (DIN + 255) // 256
            pi = 0; si = 0; ev = 0
            for cg in range(n_cg):
                i = cg % 2
                c0 = cg * 256
                ncol = min(256, DIN - c0)
                ks.dma("sp", wf[i][:, :, 0:ncol], w_in_v[:, :, c0:c0 + ncol], writes=[B_wf[i]])
                ks.op("pool", lambda e, i=i, ncol=ncol: e.tensor_copy(wh[i][:, :, 0:ncol], wf[i][:, :, 0:ncol]),
                      reads=[B_wf[i]], writes=[B_wh[i]])
                if c0 < 6144:
                    for jb in range(2):
                        j = cg * 2 + jb
                        s_ = si % 2; si += 1
                        for tg in range(NG):
                            p_ = pi % 4; pi += 1
                            for kc in range(KC):
                                ks.op("pe", lambda e, kc=kc, i=i, jb=jb, tg=tg, p_=p_: e.matmul(
                                    pp[p_][:], lhsT=wh[i][:, kc, jb * 128:(jb + 1) * 128],
                                    rhs=hT[:, kc, tg * 512:(tg + 1) * 512], start=(kc == 0), stop=(kc == KC - 1)),
                                    reads=[B_wh[i], B_hT[kc]], writes=[B_pp[p_]])
                            if ev % 2 == 0:
                                ks.op("act", lambda e, s_=s_, tg=tg, p_=p_: e.activation(
                                    out=stg[s_][:, tg * 512:(tg + 1) * 512], in_=pp[p_][:], func=AF.Copy),
                                    reads=[B_pp[p_]], writes=[B_stg[s_]])
                            else:
                                ks.op("dve", lambda e, s_=s_, tg=tg, p_=p_: e.tensor_copy(
                                    stg[s_][:, tg * 512:(tg + 1) * 512], pp[p_][:]),
                                    reads=[B_pp[p_]], writes=[B_stg[s_]])
                            ev += 1
                        ks.dma("sp", projT[j], stg[s_], reads=[B_stg[s_]], writes=[B_projT[j]])
                else:
                    isg = ncol < 256
                    for tt in range(NT):
                        p_ = pi % 4; pi += 1
                        for kc in range(KC):
                            ks.op("pe", lambda e, kc=kc, i=i, tt=tt, p_=p_, ncol=ncol: e.matmul(
                                pp[p_][:, 0:ncol], lhsT=hT[:, kc, tt * 128:(tt + 1) * 128],
                                rhs=wh[i][:, kc, 0:ncol], start=(kc == 0), stop=(kc == KC - 1)),
                                reads=[B_wh[i], B_hT[kc]], writes=[B_pp[p_]])
                        if isg:
                            ks.op("dve", lambda e, tt=tt, p_=p_: e.tensor_copy(gst[:, tt, :], pp[p_][:, 0:24]),
                                  reads=[B_pp[p_]], writes=[B_gst])
                        elif ev % 2 == 0:
                            ks.op("act", lambda e, tt=tt, p_=p_: e.activation(out=vst[:, tt, :], in_=pp[p_][:, 0:256], func=AF.Copy),
                                  reads=[B_pp[p_]], writes=B_stg)
                        else:
                            ks.op("dve", lambda e, tt=tt, p_=p_: e.tensor_copy(vst[:, tt, :], pp[p_][:, 0:256]),
                                  reads=[B_pp[p_]], writes=B_stg)
                        ev += 1
                    if isg:
                        ks.dma("sp", gat.rearrange("(tt p) c -> p tt c", p=128), gst[:], reads=[B_gst], writes=[B_gat])
                    else:
                        v0 = c0 - 6144
                        ks.dma("sp", vtok[:, v0:v0 + 256].rearrange("(tt p) c -> p tt c", p=128), vst,
                               reads=B_stg, writes=[B_vtok])
            ks.barrier()
        if stop_after == 1:
            return _finish(nc, ks, B_projT + [B_vtok, B_gat])

        def act(out, in_, func, R, W, **kw):
            return ks.op("act", lambda e: e.activation(out=out, in_=in_, func=func, **kw), reads=R, writes=W)

        def ts(eng, out, in0, s1, op0, R, W, s2=None, op1=None):
            kw = dict(out=out, in0=in0, scalar1=s1, scalar2=s2, op0=op0)
            if op1 is not None:
                kw["op1"] = op1
            return ks.op(eng, lambda e: e.tensor_scalar(**kw), reads=R, writes=W)

        def tt(eng, out, in0, in1, op, R, W):
            return ks.op(eng, lambda e: e.tensor_tensor(out=out, in0=in0, in1=in1, op=op), reads=R, writes=W)

        def stt(out, in0, scalar, in1, op0, op1, R, W):
            return ks.op("dve", lambda e: e.scalar_tensor_tensor(out=out, in0=in0, scalar=scalar, in1=in1, op0=op0, op1=op1),
                         reads=R, writes=W)

        def mm(out, lhsT, rhs, start, stop, R, W):
            return ks.op("pe", lambda e: e.matmul(out, lhsT=lhsT, rhs=rhs, start=start, stop=stop), reads=R, writes=W)

        def tr(out, in_, idn, R, W):
            return ks.op("pe", lambda e: e.transpose(out, in_, idn), reads=R, writes=W)

        def cp(eng, out, in_, R, W):
            if eng == "act":
                return act(out, in_, AF.Copy, R, W)
            return ks.op(eng, lambda e: e.tensor_copy(out, in_), reads=R, writes=W)

        def asel(out, in_, cmp, fill, R, W):
            if cmp == "le":
                pat, cm, base = [[1, 128]], -1, 0
            else:
                pat, cm, base = [[-1, 128]], 1, -1
            return ks.op("pool", lambda e: e.affine_select(out=out, in_=in_, pattern=pat, compare_op=ALU.is_ge,
                                                           fill=fill, base=base, channel_multiplier=cm), reads=R, writes=W)

        B_c = Buf("consts")
        ones_f = sbt(cst, "ones_f", [128, 128], F32); ones_h = sbt(cst, "ones_h", [128, 128], BF16)
        UT_f = sbt(cst, "UT_f", [128, 128], F32)
        maskL = sbt(cst, "maskL", [128, 128], F32); maskU = sbt(cst, "maskU", [128, 128], F32)
        ccols = sbt(cst, "ccols", [128, 4], F32)
        ks.op("pool", lambda e: e.memset(ones_f[:], 1.0), writes=[B_c])
        ks.op("pool", lambda e: e.memset(ones_h[:], 1.0), writes=[B_c])
        ks.op("pool", lambda e: e.memset(UT_f[:], 1.0), writes=[B_c])
        asel(UT_f[:], UT_f[:], "le", 0.0, [B_c], [B_c])
        ks.op("pool", lambda e: e.memset(maskL[:], 0.0), writes=[B_c])
        asel(maskL[:], maskL[:], "gt", 1.0e4, [B_c], [B_c])
        ks.op("pool", lambda e: e.memset(maskU[:], 0.0), writes=[B_c])
        asel(maskU[:], maskU[:], "le", -1.0e4, [B_c], [B_c])
        ks.op("pool", lambda e: e.memset(ccols[:, 0:1], 0.0), writes=[B_c])
        ks.op("pool", lambda e: e.memset(ccols[:, 1:2], 1.0), writes=[B_c])
        ks.op("pool", lambda e: e.memset(ccols[:, 2:3], 1.0e-6), writes=[B_c])
        ks.op("pool", lambda e: e.memset(ccols[:, 3:4], 1.0e-5), writes=[B_c])
        c0_, c1_, ce6, ce5 = ccols[:, 0:1], ccols[:, 1:2], ccols[:, 2:3], ccols[:, 3:4]
        STOPG = int(os.environ.get("STOPG", "0"))
        if STOPG == 1:
            return _finish(nc, ks, [])

        oT = dscr("oT", [16, 128, S], BF16)
        B_oT = [[Buf() for _ in range(NG)] for _ in range(16)]

        gst_ = ExitStack(); st.enter_context(gst_)
        B_g = Buf("gates")
        gt = sbt(gst_, "gt", [128, NT, 24], F32)
        prm = sbt(gst_, "prm", [128, 3, NH], F32)
        G8 = lambda nm: sbt(gst_, nm, [128, NH, NT], F32)
        beta = G8("beta"); nbeta = G8("nbeta"); gl = G8("gl"); Gc = G8("Gc"); nbg = G8("nbg")
        kds = G8("kds"); eGl = G8("eGl"); tmpg = G8("tmpg"); lf = G8("lf"); Fc = G8("Fc"); nF = G8("nF")
        nea = sbt(gst_, "nea", [128, NH], F32); nfb = sbt(gst_, "nfb", [128, NH], F32)
        ks.dma("sp", gt[:], gat.rearrange("(tt p) c -> p tt c", p=128), reads=[B_gat], writes=[B_g])
        ks.dma("sp", prm[:, 0, :], a_log[0:1, :].broadcast_to([128, NH]), writes=[B_g])
        ks.dma("sp", prm[:, 1, :], dt_bias[0:1, :].broadcast_to([128, NH]), writes=[B_g])
        ks.dma("sp", prm[:, 2, :], f_bias[0:1, :].broadcast_to([128, NH]), writes=[B_g])
        RG = [B_g, B_c]
        act(nea[:], prm[:, 0, :], AF.Exp, RG, [B_g])
        ts("dve", nea[:], nea[:], -1.0, ALU.mult, RG, [B_g])
        ts("dve", nfb[:], prm[:, 2, :], -1.0, ALU.mult, RG, [B_g])
        if STOPG == 2:
            return _finish(nc, ks, [])
        for h in range(NH):
            act(beta[:, h, :], gt[:, :, h], AF.Sigmoid, RG, [B_g])
        for h in range(NH):
            act(tmpg[:, h, :], gt[:, :, 8 + h], AF.Exp, RG, [B_g], bias=prm[:, 1, h:h + 1], scale=1.0)
            act(lf[:, h, :], gt[:, :, 16 + h], AF.Exp, RG, [B_g], bias=nfb[:, h:h + 1], scale=-1.0)
        gflat = lambda t_: t_[:, :, :].rearrange("p h t -> p (h t)")
        act(gflat(tmpg), gflat(tmpg), AF.Ln, RG, [B_g], bias=c1_, scale=1.0)
        act(gflat(lf), gflat(lf), AF.Ln, RG, [B_g], bias=c1_, scale=1.0)
        ts("dve", gflat(lf), gflat(lf), -1.0, ALU.mult, RG, [B_g])
        for h in range(NH):
            ts("dve", gl[:, h, :], tmpg[:, h, :], nea[:, h:h + 1], ALU.mult, RG, [B_g])
        ts("dve", gflat(nbeta), gflat(beta), -1.0, ALU.mult, RG, [B_g])
        if STOPG == 3:
            return _finish(nc, ks, [])
        with ExitStack() as gp:
            pg = pst(gp, "pg", [128, 2, NH * NT]); B_pg = PB("pg")
            mm(pg[:, 0, :], UT_f[:], gflat(gl), True, True, RG, [B_pg])
            mm(pg[:, 1, :], ones_f[:], gflat(gl), True, True, RG, [B_pg])
            cp("dve", gflat(Gc), pg[:, 0, :], [B_pg], [B_g])
            act(gflat(eGl), pg[:, 1, :], AF.Exp, [B_pg, B_c], [B_g])
            tt("dve", gflat(kds), pg[:, 1, :], gflat(Gc), ALU.subtract, [B_pg, B_g], [B_g])
            act(gflat(kds), gflat(kds), AF.Exp, RG, [B_g])
            act(gflat(tmpg), gflat(Gc), AF.Exp, RG, [B_g])
            tt("dve", gflat(nbg), gflat(tmpg), gflat(nbeta), ALU.mult, RG, [B_g])
            if STOPG == 4:
                return _finish(nc, ks, [])
            mm(pg[:, 0, :], UT_f[:], gflat(lf), True, True, RG, [B_pg])
            mm(pg[:, 1, :], ones_f[:], gflat(lf), True, True, RG, [B_pg])
            cp("dve", gflat(Fc), pg[:, 0, :], [B_pg], [B_g])
            cp("dve", gflat(nF), pg[:, 1, :], [B_pg], [B_g])
            for h in range(NH):
                ks.op("dve", lambda e, h=h: e.tensor_tensor_scan(out=tmpg[:, h, :], data0=ones_f[:, 0:NT], data1=nF[:, h, :],
                                                                 initial=0.0, op0=ALU.mult, op1=ALU.add), reads=RG, writes=[B_g])
            if STOPG == 5:
                return _finish(nc, ks, [])
            tt("dve", gflat(tmpg), gflat(tmpg), gflat(nF), ALU.subtract, RG, [B_g])
            tt("dve", gflat(Fc), gflat(Fc), gflat(tmpg), ALU.add, RG, [B_g])
            ts("dve", gflat(nF), gflat(Fc), -1.0, ALU.mult, RG, [B_g])
            if STOPG == 6:
                return _finish(nc, ks, [])
            ks.barrier()

        if stop_after == 1.5:
            return _finish(nc, ks, [])
        with ExitStack() as ph:
            raw = [[sbt(ph, "raw%d_%d" % (p_, i), [128, 515], BF16) for i in range(3)] for p_ in range(2)]
            B_raw = [[Buf() for i in range(3)] for p_ in range(2)]
            cw = sbt(ph, "cw", [128, 24, 4], F32); B_cw = Buf()
            nw = sbt(ph, "nw", [128, 1], F32)
            ks.dma("sp", cw[:], convw[:, :, :], writes=[B_cw])
            ks.dma("sp", nw[:], norm_w[:, :], writes=[B_cw])
            cacc = [sbt(ph, "cacc%d" % i, [128, 512], F32) for i in range(3)]; B_cacc = [Buf() for _ in range(3)]
            sqb = [sbt(ph, "sqb%d" % i, [128, 512], BF16) for i in range(2)]; B_sqb = [Buf(), Buf()]
            rsb = [sbt(ph, "rsb%d" % i, [128, 512], F32) for i in range(2)]; B_rsb = [Buf(), Buf()]
            qnG = sbt(ph, "qnG", [128, 2, NH, 512], BF16); knG = sbt(ph, "knG", [128, 2, NH, 512], BF16)
            vTG = sbt(ph, "vTG", [128, 2, NH, 512], BF16)
            B_qkv = [[Buf() for _ in range(NH)] for _ in range(2)]
            kdec = sbt(ph, "kdec", [128, NH, 4, 128], BF16); vb = sbt(ph, "vb", [128, NH, 4, 128], BF16)
            TT = sbt(ph, "TT", [128, NH, 4, 128], BF16); qkT = sbt(ph, "qkT", [128, NH, 4, 128], BF16)
            qg = sbt(ph, "qg", [128, NH, 4, 128], BF16)
            B_ck = [[Buf() for _ in range(4)] for _ in range(NH)]
            B_TT = [[Buf() for _ in range(4)] for _ in range(NH)]
            oTs = sbt(ph, "oTs", [128, NH, 512], F32); B_oTs = [[Buf() for _ in range(4)] for _ in range(NH)]
            S_f = sbt(ph, "S_f", [128, NH, 128], F32); S_h = sbt(ph, "S_h", [128, NH, 128], BF16); B_S = [Buf() for _ in range(NH)]
            NB = 2
            W4 = 4
            mk = lambda nm, dt_: [[sbt(ph, "%s%d_%d" % (nm, i, j), [128, 128], dt_) for j in range(W4)] for i in range(NB)]
            diagG = mk("diagG", F32); mL = mk("mL", F32); mU = mk("mU", F32); eGb = mk("eGb", F32)
            Pn = [mk("PnA", BF16), mk("PnB", BF16)]; Pt = [mk("PtA", BF16), mk("PtB", BF16)]; XT = [mk("XTA", BF16), mk("XTB", BF16)]
            B_tmp = [[Buf() for _ in range(W4)] for _ in range(NB)]
            rbuf = sbt(ph, "rbuf", [128, NH, 128], BF16); B_r = [Buf() for _ in range(NH)]
            vnew = sbt(ph, "vnew", [128, NH, 128], BF16); B_vn = [Buf() for _ in range(NH)]
            zb = [sbt(ph, "zb%d" % i, [128, 512], BF16) for i in range(2)]; B_zb = [Buf(), Buf()]
            zs = [sbt(ph, "zs%d" % i, [128, 512], F32) for i in range(2)]
            on_ = [sbt(ph, "on%d" % i, [128, 512], F32) for i in range(2)]
            og_ = [sbt(ph, "og%d" % i, [128, 512], BF16) for i in range(2)]; B_og = [Buf(), Buf()]
            P_L = [pst(ph, "P_L%d" % i, [128, 4, 128]) for i in range(3)]
            B_L = [[PB("P_L%d" % i) for _ in range(W4)] for i in range(3)]
            P_T = pst(ph, "P_T", [128, 8, 128], BF16); B_PT = [PB("P_T") for _ in range(8)]
            P_Sa = pst(ph, "P_Sa", [128, 4, 128]); B_Sa = [PB("P_Sa") for _ in range(4)]
            P_Sb = pst(ph, "P_Sb", [128, 4, 128]); B_Sb = [PB("P_Sb") for _ in range(4)]
            P_N = pst(ph, "P_N", [128, 512]); B_PN = PB("P_N")
            cnt = {"sq": 0, "z": 0, "w": 0}
            for h in range(NH):
                ks.op("dve", lambda e, h=h: e.memset(S_f[:, h, :], 0.0), writes=[B_S[h]])
                ks.op("dve", lambda e, h=h: e.memset(S_h[:, h, :], 0.0), writes=[B_S[h]])

            def gprep(h, tg):
                par = tg % 2
                t0 = tg * 512
                srcs = [projT[h], projT[8 + h], projT[16 + h]]
                dsts = [qnG[:, par, h, :], knG[:, par, h, :], vTG[:, par, h, :]]
                Bd = B_qkv[par][h]
                for i in range(3):
                    rb = raw[h % 2][i]; Br = B_raw[h % 2][i]
                    if tg == 0:
                        ks.op("pool", lambda e, rb=rb: e.memset(rb[:, 0:3], 0.0), writes=[Br])
                        ks.dma("sp", rb[:, 3:515], srcs[i][:, 0:512], reads=[B_projT[[h, 8 + h, 16 + h][i]]], writes=[Br])
                    else:
                        ks.dma("sp", rb[:, 0:515], srcs[i][:, t0 - 3:t0 + 512], reads=[B_projT[[h, 8 + h, 16 + h][i]]], writes=[Br])
                    blk = i * 8 + h
                    ca = cacc[i]; Bc = B_cacc[i]
                    ts("dve", ca[:], rb[:, 3:515], cw[:, blk, 3:4], ALU.mult, [Br, B_cw], [Bc])
                    for k in range(3):
                        stt(ca[:], rb[:, k:k + 512], cw[:, blk, k:k + 1], ca[:], ALU.mult, ALU.add, [Br, B_cw, Bc], [Bc])
                    if i == 2:
                        act(dsts[2], ca[:], AF.Silu, [Bc], [Bd])
                    else:
                        act(ca[:], ca[:], AF.Silu, [Bc], [Bc])
                        j = cnt["sq"] % 2; cnt["sq"] += 1
                        tt("pool", sqb[j][:], ca[:], ca[:], ALU.mult, [Bc], [B_sqb[j]])
                        mm(P_N[:], ones_h[:], sqb[j][:], True, True, [B_sqb[j], B_c], [B_PN])
                        act(rsb[j][:], P_N[:], AF.Sqrt, [B_PN, B_c], [B_rsb[j]], bias=ce6, scale=1.0)
                        ks.op("dve", lambda e, j=j: e.reciprocal(rsb[j][:], rsb[j][:]), reads=[B_rsb[j]], writes=[B_rsb[j]])
                        if i == 0:
                            stt(dsts[0], ca[:], 128.0 ** -0.5, rsb[j][:], ALU.mult, ALU.mult, [Bc, B_rsb[j]], [Bd])
                        else:
                            tt("dve", dsts[1], ca[:], rsb[j][:], ALU.mult, [Bc, B_rsb[j]], [Bd])

            def wave(tg, c, hs):
                par = tg % 2; n = tg * 4 + c
                b = cnt["w"] % NB; cnt["w"] += 1
                cs = slice(c * 128, (c + 1) * 128)
                J = list(range(len(hs)))
                knc = lambda j: knG[:, par, hs[j], cs]
                qnc = lambda j: qnG[:, par, hs[j], cs]
                vTc = lambda j: vTG[:, par, hs[j], cs]
                Bq = lambda j: B_qkv[par][hs[j]]
                Bt = lambda j: B_tmp[b][j]
                Bk = lambda j: B_ck[hs[j]][c]
                parts = []

                def p0():
                    for j in J:
                        tr(P_T[:, j, :], knc(j), ident_h[:], [Bq(j), B_ident], [B_PT[j]])
                        tr(P_T[:, 4 + j, :], vTc(j), ident_h[:], [Bq(j), B_ident], [B_PT[4 + j]])
                    for j in J:
                        h = hs[j]
                        ts("dve", kdec[:, h, c, :], P_T[:, j, :], kds[:, h, n:n + 1], ALU.mult, [B_PT[j], B_g], [Bk(j)])
                        ts("dve", vb[:, h, c, :], P_T[:, 4 + j, :], beta[:, h, n:n + 1], ALU.mult, [B_PT[4 + j], B_g], [Bk(j)])
                    for j in J:
                        h = hs[j]
                        ts("pool", diagG[b][j][:], ident_f[:], Gc[:, h, n:n + 1], ALU.mult, [B_ident, B_g], [Bt(j)])
                        mm(P_L[0][:, j, :], ones_f[:], diagG[b][j][:], True, True, [Bt(j), B_c], [B_L[0][j]])
                parts.append(p0)

                def p1():
                    for j in J:
                        h = hs[j]
                        stt(mL[b][j][:], P_L[0][:, j, :], Gc[:, h, n:n + 1], maskL[:], ALU.subtract, ALU.max, [B_L[0][j], B_g, B_c], [Bt(j)])
                        stt(mU[b][j][:], P_L[0][:, j, :], Gc[:, h, n:n + 1], maskU[:], ALU.subtract, ALU.min, [B_L[0][j], B_g, B_c], [Bt(j)])
                    for j in J:
                        act(eGb[b][j][:], P_L[0][:, j, :], AF.Exp, [B_L[0][j]], [Bt(j)])
                    for j in J:
                        act(mL[b][j][:], mL[b][j][:], AF.Exp, [Bt(j)], [Bt(j)], scale=-1.0)
                        act(mU[b][j][:], mU[b][j][:], AF.Exp, [Bt(j)], [Bt(j)])
                    for j in J:
                        tt("pool", qg[:, hs[j], c, :], qnc(j), eGb[b][j][:], ALU.mult, [Bq(j), Bt(j)], [Bk(j)])
                    for j in J:
                        mm(P_L[1][:, j, :], knc(j), knc(j), True, True, [Bq(j)], [B_L[1][j]])
                        mm(P_L[2][:, j, :], knc(j), qnc(j), True, True, [Bq(j)], [B_L[2][j]])
                parts.append(p1)

                def p2():
                    for j in J:
                        h = hs[j]
                        stt(Pn[0][b][j][:], P_L[1][:, j, :], nbeta[:, h, n:n + 1], mL[b][j][:], ALU.mult, ALU.mult, [B_L[1][j], B_g, Bt(j)], [Bt(j)])
                        tt("dve", qkT[:, h, c, :], P_L[2][:, j, :], mU[b][j][:], ALU.mult, [B_L[2][j], Bt(j)], [Bk(j)])
                    for j in J:
                        tr(P_T[:, j, :], Pn[0][b][j][:], ident_h[:], [Bt(j), B_ident], [B_PT[j]])
                    for j in J:
                        cp("act", Pt[0][b][j][:], P_T[:, j, :], [B_PT[j]], [Bt(j)])
                    for j in J:
                        tt("dve", XT[0][b][j][:], P_T[:, j, :], ident_f[:], ALU.add, [B_PT[j], B_ident], [Bt(j)])
                parts.append(p2)

                for k in range(1, 7):
                    def pl(k=k):
                        a = (k - 1) % 2; c_ = k % 2
                        for j in J:
                            mm(P_L[0][:, j, :], Pt[a][b][j][:], Pn[a][b][j][:], True, True, [Bt(j)], [B_L[0][j]])
                        if k < 6:
                            for j in J:
                                mm(P_L[1][:, j, :], Pn[a][b][j][:], Pt[a][b][j][:], True, True, [Bt(j)], [B_L[1][j]])
                        for j in J:
                            cp("act", Pn[c_][b][j][:], P_L[0][:, j, :], [B_L[0][j]], [Bt(j)])
                        if k < 6:
                            for j in J:
                                cp("dve", Pt[c_][b][j][:], P_L[1][:, j, :], [B_L[1][j]], [Bt(j)])
                        for j in J:
                            mm(P_L[2][:, j, :], Pn[c_][b][j][:], XT[a][b][j][:], True, True, [Bt(j)], [B_L[2][j]])
                        for j in J:
                            if k == 6:
                                tt("dve", TT[:, hs[j], c, :], P_L[2][:, j, :], XT[a][b][j][:], ALU.add, [B_L[2][j], Bt(j)], [B_TT[hs[j]][c]])
                            else:
                                tt("dve", XT[c_][b][j][:], P_L[2][:, j, :], XT[a][b][j][:], ALU.add, [B_L[2][j], Bt(j)], [Bt(j)])
                    parts.append(pl)
                return parts

            def sstep(tg, c, hs):
                par = tg % 2; n = tg * 4 + c
                cs = slice(c * 128, (c + 1) * 128)
                J = list(range(len(hs)))
                st_ = []

                def s0():
                    for j in J:
                        h = hs[j]
                        mm(P_Sa[:, j, :], knG[:, par, h, cs], S_h[:, h, :], True, True, [B_qkv[par][h], B_S[h]], [B_Sa[j]])
                    for j in J:
                        h = hs[j]
                        stt(rbuf[:, h, :], P_Sa[:, j, :], nbg[:, h, n:n + 1], vb[:, h, c, :], ALU.mult, ALU.add,
                            [B_Sa[j], B_g, B_ck[h][c]], [B_r[h]])
                st_.append(s0)

                def s1():
                    for j in J:
                        h = hs[j]
                        mm(P_Sa[:, j, :], TT[:, h, c, :], rbuf[:, h, :], True, True, [B_TT[h][c], B_r[h]], [B_Sa[j]])
                    for j in J:
                        h = hs[j]
                        cp("act", vnew[:, h, :], P_Sa[:, j, :], [B_Sa[j]], [B_vn[h]])
                st_.append(s1)

                def s2():
                    for j in J:
                        h = hs[j]
                        mm(P_Sb[:, j, :], S_h[:, h, :], qg[:, h, c, :], True, False, [B_S[h], B_ck[h][c]], [B_Sb[j]])
                        mm(P_Sb[:, j, :], vnew[:, h, :], qkT[:, h, c, :], False, True, [B_vn[h], B_ck[h][c]], [B_Sb[j]])
                    for j in J:
                        h = hs[j]
                        mm(P_Sa[:, j, :], kdec[:, h, c, :], vnew[:, h, :], True, True, [B_ck[h][c], B_vn[h]], [B_Sa[j]])
                    for j in J:
                        h = hs[j]
                        cp("act", oTs[:, h, cs], P_Sb[:, j, :], [B_Sb[j]], [B_oTs[h][c]])
                    for j in J:
                        h = hs[j]
                        ts("dve", S_f[:, h, :], S_f[:, h, :], eGl[:, h, n:n + 1], ALU.mult, [B_S[h], B_g], [B_S[h]])
                        tt("dve", S_f[:, h, :], P_Sa[:, j, :], S_f[:, h, :], ALU.add, [B_Sa[j], B_S[h]], [B_S[h]])
                    for j in J:
                        h = hs[j]
                        cp("act", S_h[:, h, :], S_f[:, h, :], [B_S[h]], [B_S[h]])
                st_.append(s2)
                return st_

            def pnorm(h, tg):
                t0 = tg * 512
                j = cnt["z"] % 2; cnt["z"] += 1
                Bo = [B_oTs[h][k] for k in range(4)]
                ks.dma("sp", zb[j][:], projT[24 + h][:, t0:t0 + 512], reads=[B_projT[24 + h]], writes=[B_zb[j]])
                q = cnt["sq"] % 2; cnt["sq"] += 1
                tt("pool", sqb[q][:], oTs[:, h, :], oTs[:, h, :], ALU.mult, Bo, [B_sqb[q]])
                mm(P_N[:], ones_h[:], sqb[q][:], True, True, [B_sqb[q], B_c], [B_PN])
                act(rsb[q][:], P_N[:], AF.Sqrt, [B_PN, B_c], [B_rsb[q]], bias=ce6, scale=1.0 / 128.0)
                ks.op("dve", lambda e, q=q: e.reciprocal(rsb[q][:], rsb[q][:]), reads=[B_rsb[q]], writes=[B_rsb[q]])
                act(zs[j][:], zb[j][:], AF.Silu, [B_zb[j]], [B_zb[j]])
                tt("dve", on_[j][:], oTs[:, h, :], rsb[q][:], ALU.mult, Bo + [B_rsb[q]], [B_og[j]])
                stt(og_[j][:], on_[j][:], nw[:, 0:1], zs[j][:], ALU.mult, ALU.mult, [B_og[j], B_cw, B_zb[j]], [B_og[j]])
                ks.dma("sp", oT[h][:, t0:t0 + 512], og_[j][:], reads=[B_og[j]], writes=[B_oT[h][tg]])

            def merge(streams):
                tot = max(len(s_) for s_ in streams) if streams else 0
                idx = [0] * len(streams)
                for step in range(tot):
                    for si_, s_ in enumerate(streams):
                        tgt = ((step + 1) * len(s_) + tot - 1) // tot
                        while idx[si_] < tgt:
                            s_[idx[si_]](); idx[si_] += 1

            HW = [[0, 1, 2, 3], [4, 5, 6, 7]]
            for h in range(NH):
                gprep(h, 0)
            pending = []
            for tg in range(NG):
                for c in range(4):
                    streams = [wave(tg, c, HW[0]) + wave(tg, c, HW[1])]
                    if pending:
                        streams.append(pending)
                    merge(streams)
                    if pending and c == 0 and tg > 0:
                        for h in range(NH):
                            pnorm(h, tg - 1)
                    if tg + 1 < NG:
                        gprep(2 * c, tg + 1); gprep(2 * c + 1, tg + 1)
                    pending = sstep(tg, c, HW[0]) + sstep(tg, c, HW[1])
            merge([pending])
            for h in range(NH):
                pnorm(h, NG - 1)
            ks.barrier()
        if stop_after == 2:
            return _finish(nc, ks, [])

        f3d = dscr("f3d", [NH, S], F32); B_f3d = Buf()
        WROW = 24576
        use_moe = (stop_after is None) or (stop_after >= 5)
        B_wsc = Buf("wsc")
        if use_moe:
            w_moe = din("w_moe", [32, 128, WROW])
            wsc = dscr("wsc", [32 * 128, WROW], BF16)
        SCL = 128.0 ** -0.5
        with ExitStack() as ph:
            P_t = pst(ph, "fP_t", [128, 128]); B_PF = PB("fP_t")
            P_F = P_t[0:NT, :]
            f3t = sbt(ph, "f3t", [NT, NH, 128], F32); B_f3t = Buf()
            for h in range(NH):
                tr(P_F, Fc[:, h, :], ident_f[:], [B_g, B_ident], [B_PF])
                cp("dve", f3t[:, h, :], P_F, [B_PF], [B_f3t])
            ks.dma("sp", f3d.rearrange("h (tt p) -> tt h p", p=128), f3t[:], reads=[B_f3t], writes=[B_f3d])

            qT = [sbt(ph, "fqT%d" % i, [128, S], BF16) for i in range(2)]
            kT = [sbt(ph, "fkT%d" % i, [128, S], BF16) for i in range(2)]
            va = [sbt(ph, "fva%d" % i, [128, NT, 128], BF16) for i in range(2)]
            B_in = [Buf(), Buf()]
            Fb = [sbt(ph, "fFb%d" % i, [128, 512], F32) for i in range(2)]; B_Fb = [Buf(), Buf()]
            lgb = [sbt(ph, "flg%d" % i, [128, 512], F32) for i in range(3)]; B_lg = [Buf() for _ in range(3)]
            pT = [sbt(ph, "fpT%d" % i, [128, 512], BF16) for i in range(4)]; B_pT = [Buf() for _ in range(4)]
            rs_ = [sbt(ph, "frs%d" % i, [128, 512], F32) for i in range(2)]; B_rs_ = [Buf(), Buf()]
            oTg = [sbt(ph, "foTg%d" % i, [128, 512], BF16) for i in range(2)]; B_oTg = [Buf(), Buf()]
            P_s = [pst(ph, "fP_s%d" % i, [128, 512]) for i in range(3)]; B_Ps = [PB("fP_s%d" % i) for i in range(3)]
            P_o = [pst(ph, "fP_o%d" % i, [128, 512]) for i in range(2)]; B_Po = [PB("fP_o0"), PB("fP_o1")]
            P_m = [pst(ph, "fP_m%d" % i, [128, 512]) for i in range(2)]; B_Pm = [PB("fP_m0"), PB("fP_m1")]
            it = 0; gi = 0
            if use_moe:
                wst = [sbt(ph, "wst%d" % i, [128, WROW], BF16) for i in range(2)]; B_wst = [Buf(), Buf()]

            def precast(e_):
                i = e_ % 2
                wv = wst[i]
                ks.dma("pool", wv[:], w_moe[e_], writes=[B_wst[i]])
                ks.dma("sp", wsc[e_ * 128:(e_ + 1) * 128, :], wv[:], reads=[B_wst[i]], writes=[B_wsc])

            for h in range(NH):
                hp = h % 2
                ks.dma("sp", qT[hp][:], projT[32 + h], reads=[B_projT[32 + h]], writes=[B_in[hp]])
                ks.dma("sp", kT[hp][:], projT[40 + h], reads=[B_projT[40 + h]], writes=[B_in[hp]])
                ks.dma("sp", va[hp][:], vtok[:, h * 128:(h + 1) * 128].rearrange("(tt p) c -> p tt c", p=128),
                       reads=[B_vtok], writes=[B_in[hp]])
                for qgi in range(NG):
                    q0 = qgi * 512
                    g_ = gi % 2; gi += 1
                    if use_moe and (h * NG + qgi) % 2 == 0:
                        precast((h * NG + qgi) // 2)
                    ks.dma("sp", Fb[g_][:], f3d[h:h + 1, q0:q0 + 512].broadcast_to([128, 512]), reads=[B_f3d], writes=[B_Fb[g_]])
                    nj = 4 * qgi + 4

                    def front(j):
                        nonlocal it
                        bl = max(0, j - 4 * qgi)
                        c_lo = bl * 128
                        ps_ = P_s[it % 3]; Bps = B_Ps[it % 3]
                        lg_ = lgb[it % 3]; Blg = B_lg[it % 3]
                        pt_ = pT[it % 4]; Bpt = B_pT[it % 4]
                        it += 1
                        mm(ps_[:, c_lo:512], kT[hp][:, j * 128:(j + 1) * 128], qT[hp][:, q0 + c_lo:q0 + 512], True, True,
                           [B_in[hp]], [Bps])
                        stt(lg_[:, c_lo:512], ps_[:, c_lo:512], SCL, Fb[g_][:, c_lo:512], ALU.mult, ALU.add, [Bps, B_Fb[g_]], [Blg])
                        act(pt_[:, c_lo:512], lg_[:, c_lo:512], AF.Exp, [Blg, B_g], [Bpt], bias=nF[:, h, j:j + 1], scale=1.0)
                        if j >= 4 * qgi:
                            asel(pt_[:, c_lo:c_lo + 128], pt_[:, c_lo:c_lo + 128], "le", 0.0, [Bpt], [Bpt])
                        return (pt_, Bpt, c_lo)

                    fq = [front(0)]
                    if nj > 1:
                        fq.append(front(1))
                    for j in range(nj):
                        cur = fq.pop(0)
                        if j + 2 < nj:
                            fq.append(front(j + 2))
                        pt_, Bpt, c_lo = cur
                        mm(P_o[g_][:, c_lo:512], va[hp][:, j, :], pt_[:, c_lo:512], j == 0, j == nj - 1, [Bpt, B_in[hp]], [B_Po[g_]])
                        mm(P_m[g_][:, c_lo:512], ones_h[:], pt_[:, c_lo:512], j == 0, j == nj - 1, [Bpt, B_c], [B_Pm[g_]])
                    cp("dve", rs_[g_][:], P_m[g_][:], [B_Pm[g_]], [B_rs_[g_]])
                    ks.op("dve", lambda e, g_=g_: e.reciprocal(rs_[g_][:], rs_[g_][:]), reads=[B_rs_[g_]], writes=[B_rs_[g_]])
                    tt("dve", oTg[g_][:], P_o[g_][:], rs_[g_][:], ALU.mult, [B_Po[g_], B_rs_[g_]], [B_oTg[g_]])
                    ks.dma("sp", oT[8 + h][:, q0:q0 + 512], oTg[g_][:], reads=[B_oTg[g_]], writes=[B_oT[8 + h][qgi]])
            ks.barrier()
        gst_.close()
        if stop_after == 3:
            return _finish(nc, ks, [])

        NBLK = 64
        BLK = 256
        x1d = dscr("x1d", [S, D], F32); B_x1d = [Buf() for _ in range(NT)]
        h2d = dscr("h2d", [S, D], BF16); B_h2d = [Buf() for _ in range(NT)]
        xsd = dscr("xsd", [NBLK * BLK, D], BF16); B_xsd = Buf()
        ybd = dscr("ybd", [NBLK * BLK, D], BF16); B_ybd = Buf()
        mwd = dscr("mwd", [S, 32], F32); B_mwd = Buf()
        rst = ExitStack(); st.enter_context(rst)
        d01 = sbt(rst, "d01", [128, 2, NT], I32)
        w12 = sbt(rst, "w12", [128, 2, NT], F32)
        widx = sbt(rst, "widx", [128, NBLK], I32)
        B_rs = Buf("routing")

        def bcast_load(stack, name, src_row, B, plus1=False):
            t_ = sbt(stack, name, [128, D], F32)
            ks.dma("sp", t_[:], src_row.broadcast_to([128, D]), reads=[B_modrow], writes=[B])
            if plus1:
                ts("pool", t_[:], t_[:], 1.0, ALU.add, [B], [B])
            return t_

        with ExitStack() as ph:
            B_bc = Buf()
            g1p = bcast_load(ph, "g1p", modrow[0:1, 2 * D:3 * D], B_bc, True)
            sh2b = bcast_load(ph, "sh2b", modrow[0:1, 3 * D:4 * D], B_bc)
            sc2p = bcast_load(ph, "sc2p", modrow[0:1, 4 * D:5 * D], B_bc, True)
            l1g = bcast_load(ph, "l1g", ln1_g[0:1, :], B_bc)
            l1b = bcast_load(ph, "l1b", ln1_b[0:1, :], B_bc)
            wo = sbt(ph, "wo", [128, KC, D], BF16); B_wo = Buf()
            w_out_v = w_out.rearrange("(kc p) n -> p kc n", p=128)
            for kc in range(KC):
                ks.dma("pool", wo[:, kc, :], w_out_v[:, kc, :], writes=[B_wo])
            wr = sbt(ph, "wr", [128, KC, 36], F32); brb = sbt(ph, "brb", [128, 36], F32); B_wr = Buf()
            wrh = sbt(ph, "wrh", [128, KC, 36], BF16)
            ks.dma("sp", wr[:], w_r.rearrange("(kc p) n -> p kc n", p=128), writes=[B_wr])
            ks.dma("sp", brb[:], b_r[0:1, :].broadcast_to([128, 36]), writes=[B_wr])
            cp("dve", wrh[:], wr[:], [B_wr], [B_wr])
            ogb = [sbt(ph, "ogb%d" % i, [128, KC, 512], BF16) for i in range(2)]; B_ogb = [Buf(), Buf()]
            xt_ = sbt(ph, "xt0", [128, D], F32); B_xt = Buf()
            vv = sbt(ph, "vv0", [128, D], F32); B_vv = Buf()
            h2 = sbt(ph, "h2_0", [128, D], F32); B_h2 = Buf()
            h2h = [sbt(ph, "h2h%d" % i, [128, D], BF16) for i in range(2)]; B_h2h = [Buf(), Buf()]
            h2Tf = sbt(ph, "h2Tf", [128, KC, 128], BF16); B_h2Tf = Buf()
            st4 = sbt(ph, "st4", [128, 16], F32); B_st = Buf()
            rt = sbt(ph, "rt", [128, 128], F32); B_rt = Buf()
            OH = sbt(ph, "OH", [128, 2, NT, 32], F32); B_OH = Buf()
            junk = sbt(ph, "junk", [128, D], BF16); B_junk = Buf()
            P_y = [pst(ph, "P_y%d" % i, [128, 512]) for i in range(4)]; B_Py = [PB("P_y%d" % i) for i in range(4)]
            P_h = [pst(ph, "P_h%d" % i, [128, 8, 128], BF16) for i in range(2)]; B_Ph = [PB("P_h0"), PB("P_h1")]
            P_r = pst(ph, "P_r", [128, 36]); B_Pr = PB("P_r")
            oT_v = oT.rearrange("h p t -> p h t")
            py = 0; ph_i = 0
            for tg in range(NG):
                gb = tg % 2
                ks.dma("sp", ogb[gb][:], oT_v[:, :, tg * 512:(tg + 1) * 512],
                       reads=[B_oT[h][tg] for h in range(16)], writes=[B_ogb[gb]])
                for ti in range(4):
                    tI = tg * 4 + ti
                    hb_ = tI % 2
                    ks.dma("sp", xt_[:], xtok[tI * 128:(tI + 1) * 128, :], writes=[B_xt])
                    for cg in range(4):
                        p_ = py % 4; py += 1
                        for hh in range(KC):
                            mm(P_y[p_][:], ogb[gb][:, hh, ti * 128:(ti + 1) * 128], wo[:, hh, cg * 512:(cg + 1) * 512],
                               hh == 0, hh == KC - 1, [B_ogb[gb], B_wo], [B_Py[p_]])
                        tt("dve", vv[:, cg * 512:(cg + 1) * 512], P_y[p_][:], g1p[:, cg * 512:(cg + 1) * 512], ALU.mult,
                           [B_Py[p_], B_bc], [B_vv])
                    stt(vv[:], xt_[:], ALPHA, vv[:], ALU.mult, ALU.add, [B_xt, B_vv], [B_vv])
                    ks.op("dve", lambda e: e.reduce_sum(out=st4[:, 0:1], in_=vv[:], axis=AX.X), reads=[B_vv], writes=[B_st])
                    act(junk[:], vv[:], AF.Square, [B_vv], [B_junk, B_st], accum_out=st4[:, 1:2])
                    ts("dve", st4[:, 2:3], st4[:, 0:1], 1.0 / D, ALU.mult, [B_st], [B_st])
                    tt("dve", st4[:, 3:4], st4[:, 2:3], st4[:, 2:3], ALU.mult, [B_st], [B_st])
                    stt(st4[:, 4:5], st4[:, 1:2], 1.0 / D, st4[:, 3:4], ALU.mult, ALU.subtract, [B_st], [B_st])
                    act(st4[:, 5:6], st4[:, 4:5], AF.Sqrt, [B_st, B_c], [B_st], bias=ce5, scale=1.0)
                    ks.op("dve", lambda e: e.reciprocal(st4[:, 6:7], st4[:, 5:6]), reads=[B_st], writes=[B_st])
                    ts("dve", vv[:], vv[:], st4[:, 2:3], ALU.subtract, [B_vv, B_st], [B_vv], s2=st4[:, 6:7], op1=ALU.mult)
                    tt("pool", vv[:], vv[:], l1g[:], ALU.mult, [B_vv, B_bc], [B_vv])
                    tt("dve", vv[:], vv[:], l1b[:], ALU.add, [B_vv, B_bc], [B_vv])
                    ks.dma("sp", x1d[tI * 128:(tI + 1) * 128, :], vv[:], reads=[B_vv], writes=[B_x1d[tI]])
                    tt("pool", h2[:], vv[:], sc2p[:], ALU.mult, [B_vv, B_bc], [B_h2])
                    tt("dve", h2h[hb_][:], h2[:], sh2b[:], ALU.add, [B_h2, B_bc], [B_h2h[hb_]])
                    ks.dma("sp", h2d[tI * 128:(tI + 1) * 128, :], h2h[hb_][:], reads=[B_h2h[hb_]], writes=[B_h2d[tI]])
                    for k8 in range(2):
                        q_ = ph_i % 2; ph_i += 1
                        for kk in range(8):
                            kc = k8 * 8 + kk
                            tr(P_h[q_][:, kk, :], h2h[hb_][:, kc * 128:(kc + 1) * 128], ident_h[:], [B_h2h[hb_], B_ident], [B_Ph[q_]])
                        cp("act", h2Tf[:, k8 * 8:(k8 + 1) * 8, :], P_h[q_][:, :, :], [B_Ph[q_]], [B_h2Tf])
                    for kc in range(KC):
                        mm(P_r[:, :], h2Tf[:, kc, :], wrh[:, kc, :], kc == 0, kc == KC - 1, [B_h2Tf, B_wr], [B_Pr])
                    R_ = [B_rt, B_c]
                    lg = rt[:, 0:36]
                    tt("dve", lg, P_r[:, :], brb[:], ALU.add, [B_Pr, B_wr], [B_rt])
                    gmx = rt[:, 36:37]; ohg = rt[:, 40:44]; gsum = rt[:, 37:38]; gw = rt[:, 38:39]
                    ks.op("dve", lambda e: e.reduce_max(out=gmx, in_=rt[:, 0:4], axis=AX.X), reads=R_, writes=[B_rt])
                    ts("dve", ohg, rt[:, 0:4], gmx, ALU.is_equal, R_, [B_rt])
                    ts("dve", rt[:, 44:48], rt[:, 0:4], gmx, ALU.subtract, R_, [B_rt])
                    act(rt[:, 44:48], rt[:, 44:48], AF.Exp, R_, [B_rt])
                    ks.op("dve", lambda e: e.reduce_sum(out=gsum, in_=rt[:, 44:48], axis=AX.X), reads=R_, writes=[B_rt])
                    ks.op("dve", lambda e: e.reciprocal(gw, gsum), reads=R_, writes=[B_rt])
                    es = rt[:, 48:56]
                    ts("dve", es, rt[:, 4:12], ohg[:, 0:1], ALU.mult, R_, [B_rt])
                    for g_ in range(1, 4):
                        stt(es, rt[:, 4 + 8 * g_:12 + 8 * g_], ohg[:, g_:g_ + 1], es, ALU.mult, ALU.add, R_, [B_rt])
                    m1 = rt[:, 56:57]; m2 = rt[:, 57:58]; oh1 = rt[:, 64:72]; oh2 = rt[:, 72:80]; es2 = rt[:, 80:88]
                    ks.op("dve", lambda e: e.reduce_max(out=m1, in_=es, axis=AX.X), reads=R_, writes=[B_rt])
                    ts("dve", oh1, es, m1, ALU.is_equal, R_, [B_rt])
                    stt(es2, oh1, -1.0e30, es, ALU.mult, ALU.add, R_, [B_rt])
                    ks.op("dve", lambda e: e.reduce_max(out=m2, in_=es2, axis=AX.X), reads=R_, writes=[B_rt])
                    ts("dve", oh2, es2, m2, ALU.is_equal, R_, [B_rt])
                    w1 = rt[:, 58:59]; w2 = rt[:, 59:60]
                    tt("dve", w1, m2, m1, ALU.subtract, R_, [B_rt])
                    act(w1, w1, AF.Exp, R_, [B_rt])
                    ts("dve", w1, w1, 1.0, ALU.add, R_, [B_rt])
                    ks.op("dve", lambda e: e.reciprocal(w1, w1), reads=R_, writes=[B_rt])
                    ts("dve", w2, w1, -1.0, ALU.mult, R_, [B_rt], s2=1.0, op1=ALU.add)
                    tt("dve", w12[:, 0, tI:tI + 1], w1, gw, ALU.mult, R_, [B_rt, B_rs])
                    tt("dve", w12[:, 1, tI:tI + 1], w2, gw, ALU.mult, R_, [B_rt, B_rs])
                    for g_ in range(4):
                        ts("dve", OH[:, 0, tI, g_ * 8:(g_ + 1) * 8], oh1, ohg[:, g_:g_ + 1], ALU.mult, R_, [B_rt, B_OH])
                        ts("dve", OH[:, 1, tI, g_ * 8:(g_ + 1) * 8], oh2, ohg[:, g_:g_ + 1], ALU.mult, R_, [B_rt, B_OH])
            if "mwd" in dbg:
                mwt_ = sbt(ph, "mwt_", [128, NT, 32], F32)
                for tI in range(NT):
                    ts("dve", mwt_[:, tI, :], OH[:, 0, tI, :], w12[:, 0, tI:tI + 1], ALU.mult, [B_OH, B_rs], [B_mwd])
                    stt(mwt_[:, tI, :], OH[:, 1, tI, :], w12[:, 1, tI:tI + 1], mwt_[:, tI, :], ALU.mult, ALU.add, [B_OH, B_rs, B_mwd], [B_mwd])
                ks.dma("sp", mwd.rearrange("(tt p) c -> p tt c", p=128), mwt_[:], reads=[B_mwd], writes=[B_mwd])
            NE = 32
            Cc = sbt(ph, "Cc", [128, NT * NE], F32); B_s = Buf("sort")
            rk = sbt(ph, "rk", [128, NT * NE], F32)
            pf = sbt(ph, "pf", [128, NT * NE], F32)
            sm = sbt(ph, "sm", [128, 8, 64], F32)
            UTs = sbt(ph, "UTs", [128, 128], F32)
            ks.op("pool", lambda e: e.memset(UTs[:], 1.0), writes=[B_s])
            ks.op("pool", lambda e: e.affine_select(out=UTs[:], in_=UTs[:], pattern=[[1, 128]], compare_op=ALU.is_ge,
                                                    fill=0.0, base=-1, channel_multiplier=-1), reads=[B_s], writes=[B_s])
            OHf = lambda k: OH[:, k, :, :].rearrange("p t e -> p (t e)")
            tt("dve", Cc[:], OHf(0), OHf(1), ALU.add, [B_OH], [B_s])
            for half in range(2):
                sl = slice(half * 512, (half + 1) * 512)
                mm(P_y[0][:], UTs[:], Cc[:, sl], True, True, [B_s], [B_Py[0]])
                mm(P_y[1][:], ones_f[:], Cc[:, sl], True, True, [B_s, B_c], [B_Py[1]])
                cp("dve", rk[:, sl], P_y[0][:], [B_Py[0]], [B_s])
                cp("dve", pf[:, sl], P_y[1][:], [B_Py[1]], [B_s])
            pf3 = pf[:, :].rearrange("p (t e) -> p t e", e=NE)
            rk3 = rk[:, :].rearrange("p (t e) -> p t e", e=NE)
            tot = sm[:, 0, 0:NE]; run = sm[:, 1, 0:NE]
            ks.op("dve", lambda e: e.memset(run, 0.0), writes=[B_s])
            for tI in range(NT):
                tt("dve", rk3[:, tI, :], rk3[:, tI, :], run, ALU.add, [B_s], [B_s])
                tt("dve", run, run, pf3[:, tI, :], ALU.add, [B_s], [B_s])
            cp("dve", tot, run, [B_s], [B_s])
            thr = sm[:, 2, 0:32]; nbk = sm[:, 3, 0:NE]; tmp32 = sm[:, 4, 0:32]
            ks.op("pool", lambda e: e.iota(thr, pattern=[[BLK, 32]], base=0, channel_multiplier=0,
                                           allow_small_or_imprecise_dtypes=True), writes=[B_s])
            for e_ in range(NE):
                ts("dve", tmp32, thr, tot[:, e_:e_ + 1], ALU.is_lt, [B_s], [B_s])
                ks.op("dve", lambda e, e_=e_: e.reduce_sum(out=nbk[:, e_:e_ + 1], in_=tmp32, axis=AX.X), reads=[B_s], writes=[B_s])
            pend = sm[:, 5, 0:NE]; pstart = sm[:, 6, 0:NE]
            ks.op("dve", lambda e: e.tensor_tensor_scan(out=pend, data0=ones_f[:, 0:NE], data1=nbk, initial=0.0,
                                                        op0=ALU.mult, op1=ALU.add), reads=[B_s, B_c], writes=[B_s])
            tt("dve", pstart, pend, nbk, ALU.subtract, [B_s], [B_s])
            ts("dve", pstart, pstart, float(BLK), ALU.mult, [B_s], [B_s])
            ts("dve", pend, pend, float(BLK), ALU.mult, [B_s], [B_s])
            for tI in range(NT):
                tt("dve", rk3[:, tI, :], rk3[:, tI, :], pstart, ALU.add, [B_s], [B_s])
            dstf = sm[:, 7, :]
            for k in range(2):
                tt("dve", Cc[:], OHf(k), rk[:], ALU.mult, [B_OH, B_s], [B_s])
                ks.op("dve", lambda e, k=k: e.tensor_reduce(out=dstf[:, k * NT:(k + 1) * NT],
                                                            in_=Cc[:, :].rearrange("p (t e) -> p t e", e=NE),
                                                            axis=AX.X, op=ALU.add), reads=[B_s], writes=[B_s])
            cp("dve", d01[:, :, :].rearrange("p k t -> p (k t)"), dstf, [B_s], [B_rs])
            bthr = sm[:, 2, 0:NBLK]; bacc = sm[:, 3, 0:NBLK]; btmp = sm[:, 4, 0:NBLK]
            ks.op("pool", lambda e: e.iota(bthr, pattern=[[BLK, NBLK]], base=0, channel_multiplier=0,
                                           allow_small_or_imprecise_dtypes=True), reads=[B_s], writes=[B_s])
            ks.op("dve", lambda e: e.memset(bacc, 0.0), reads=[B_s], writes=[B_s])
            for e_ in range(NE):
                ts("dve", btmp, bthr, pend[:, e_:e_ + 1], ALU.is_ge, [B_s], [B_s])
                tt("dve", bacc, bacc, btmp, ALU.add, [B_s], [B_s])
            ts("dve", bacc, bacc, float(NE - 1), ALU.min, [B_s], [B_s], s2=128.0, op1=ALU.mult)
            pidx = sm[:, 0, 32:33]
            ks.op("pool", lambda e: e.iota(pidx, pattern=[[0, 1]], base=0, channel_multiplier=1,
                                           allow_small_or_imprecise_dtypes=True), reads=[B_s], writes=[B_s])
            ts("dve", bacc, bacc, pidx, ALU.add, [B_s], [B_s])
            cp("dve", widx[:], bacc, [B_s], [B_rs])
            for tI in range(NT):
                hb_ = tI % 2
                ks.dma("sp", h2h[hb_][:], h2d[tI * 128:(tI + 1) * 128, :], reads=[B_h2d[tI]], writes=[B_h2h[hb_]])
                for k in range(2):
                    ks.dma("pool", None, None, reads=[B_h2h[hb_], B_rs], writes=[B_xsd],
                           fn=lambda e, k=k, tI=tI, hb_=hb_: e.indirect_dma_start(
                               out=xsd[:, :], out_offset=bass.IndirectOffsetOnAxis(ap=d01[:, k, tI:tI + 1], axis=0),
                               in_=h2h[hb_][:], in_offset=None))
            ks.barrier()
        if stop_after == 4:
            return _finish(nc, ks, [])

        with ExitStack() as ph:
            wblk = [sbt(ph, "wblk%d" % i, [128, WROW], BF16) for i in range(2)]; B_wb = [Buf(), Buf()]
            xsb = [sbt(ph, "xsb%d" % i, [128, 2, D], BF16) for i in range(2)]; B_xsb = [Buf(), Buf()]
            xsT = [sbt(ph, "xsT%d" % i, [128, KC, BLK], BF16) for i in range(2)]; B_xsT = [Buf(), Buf()]
            sg = [sbt(ph, "sg%d" % i, [128, BLK], F32) for i in range(2)]; B_sg = [Buf(), Buf()]
            hid = [sbt(ph, "hid%d" % i, [128, 4, BLK], BF16) for i in range(2)]; B_hid = [Buf(), Buf()]
            yo = [sbt(ph, "yo%d" % i, [128, D], BF16) for i in range(2)]; B_yo = [Buf(), Buf()]
            P_x = [pst(ph, "P_x%d" % i, [128, 8, 128], BF16) for i in range(2)]; B_Px = [PB("P_x0"), PB("P_x1")]
            P_g = [pst(ph, "P_g%d" % i, [128, BLK]) for i in range(2)]; B_Pg = [PB("P_g0"), PB("P_g1")]
            P_u = [pst(ph, "P_u%d" % i, [128, BLK]) for i in range(2)]; B_Pu = [PB("P_u0"), PB("P_u1")]
            P_d = [pst(ph, "P_d%d" % i, [128, 512]) for i in range(2)]; B_Pd = [PB("P_d0"), PB("P_d1")]
            px = 0; pq = 0; pd = 0; sgi = 0; yi = 0
            for b in range(NBLK):
                wb = b % 2
                wv = wblk[wb]
                ks.dma("pool", None, None, reads=[B_rs, B_wsc], writes=[B_wb[wb]],
                       fn=lambda e, b=b, wv=wv: e.indirect_dma_start(
                           out=wv[:], out_offset=None, in_=wsc[:, :],
                           in_offset=bass.IndirectOffsetOnAxis(ap=widx[:, b:b + 1], axis=0)))
                ks.dma("sp", xsb[wb][:], xsd[b * BLK:(b + 1) * BLK, :].rearrange("(t p) d -> p t d", p=128),
                       reads=[B_xsd], writes=[B_xsb[wb]])
                for t in range(2):
                    for k8 in range(2):
                        q_ = px % 2; px += 1
                        for kk in range(8):
                            kc = k8 * 8 + kk
                            tr(P_x[q_][:, kk, :], xsb[wb][:, t, kc * 128:(kc + 1) * 128], ident_h[:], [B_xsb[wb], B_ident], [B_Px[q_]])
                        if (px % 2) == 0:
                            cp("act", xsT[wb][:, k8 * 8:(k8 + 1) * 8, t * 128:(t + 1) * 128], P_x[q_][:, :, :], [B_Px[q_]], [B_xsT[wb]])
                        else:
                            cp("dve", xsT[wb][:, k8 * 8:(k8 + 1) * 8, t * 128:(t + 1) * 128], P_x[q_][:, :, :], [B_Px[q_]], [B_xsT[wb]])
                wgv = wv[:, 0:8192].rearrange("p (kc n) -> p kc n", n=512)
                wuv = wv[:, 8192:16384].rearrange("p (kc n) -> p kc n", n=512)
                wdv = wv[:, 16384:24576].rearrange("p (hc n) -> p hc n", n=D)
                hb = b % 2
                for hc in range(4):
                    q_ = pq % 2; pq += 1
                    for kc in range(KC):
                        mm(P_g[q_][:], wgv[:, kc, hc * 128:(hc + 1) * 128], xsT[wb][:, kc, :], kc == 0, kc == KC - 1,
                           [B_wb[wb], B_xsT[wb]], [B_Pg[q_]])
                    for kc in range(KC):
                        mm(P_u[q_][:], wuv[:, kc, hc * 128:(hc + 1) * 128], xsT[wb][:, kc, :], kc == 0, kc == KC - 1,
                           [B_wb[wb], B_xsT[wb]], [B_Pu[q_]])
                    s_ = sgi % 2; sgi += 1
                    act(sg[s_][:], P_g[q_][:], AF.Silu, [B_Pg[q_]], [B_sg[s_]])
                    tt("dve", hid[hb][:, hc, :], P_u[q_][:], sg[s_][:], ALU.mult, [B_sg[s_], B_Pu[q_]], [B_hid[hb]])
                for t in range(2):
                    y_ = yi % 2; yi += 1
                    for cg in range(4):
                        p_ = pd % 2; pd += 1
                        for hc in range(4):
                            mm(P_d[p_][:], hid[hb][:, hc, t * 128:(t + 1) * 128], wdv[:, hc, cg * 512:(cg + 1) * 512],
                               hc == 0, hc == 3, [B_hid[hb], B_wb[wb]], [B_Pd[p_]])
                        if cg % 2 == 0:
                            cp("act", yo[y_][:, cg * 512:(cg + 1) * 512], P_d[p_][:], [B_Pd[p_]], [B_yo[y_]])
                        else:
                            cp("dve", yo[y_][:, cg * 512:(cg + 1) * 512], P_d[p_][:], [B_Pd[p_]], [B_yo[y_]])
                    r0 = b * BLK + t * 128
                    ks.dma("sp", ybd[r0:r0 + 128, :], yo[y_][:], reads=[B_yo[y_]], writes=[B_ybd])
            ks.barrier()
        if stop_after == 5:
            return _finish(nc, ks, [])
        with ExitStack() as ph:
            B_bc = Buf()
            g2p = bcast_load(ph, "g2p", modrow[0:1, 5 * D:6 * D], B_bc, True)
            l2g = bcast_load(ph, "l2g", ln2_g[0:1, :], B_bc)
            l2b = bcast_load(ph, "l2b", ln2_b[0:1, :], B_bc)
            rr = [[sbt(ph, "rr%d_%d" % (i, k), [128, D], BF16) for k in range(2)] for i in range(2)]
            B_rr = [Buf(), Buf()]
            ya_ = [sbt(ph, "ya%d" % i, [128, D], F32) for i in range(2)]; B_ya = [Buf(), Buf()]
            x1t = [sbt(ph, "x1t%d" % i, [128, D], F32) for i in range(2)]; B_x1t = [Buf(), Buf()]
            st5 = sbt(ph, "st5", [128, 16], F32); B_st5 = Buf()
            junk5 = sbt(ph, "junk5", [128, D], BF16); B_j5 = Buf()
            for tI in range(NT):
                i = tI % 2
                for k in range(2):
                    ks.dma("pool", None, None, reads=[B_ybd, B_rs], writes=[B_rr[i]],
                           fn=lambda e, k=k, tI=tI, i=i: e.indirect_dma_start(
                               out=rr[i][k][:], out_offset=None, in_=ybd[:, :],
                               in_offset=bass.IndirectOffsetOnAxis(ap=d01[:, k, tI:tI + 1], axis=0)))
                ks.dma("sp", x1t[i][:], x1d[tI * 128:(tI + 1) * 128, :], reads=[B_x1d[tI]], writes=[B_x1t[i]])
                ya = ya_[i][:]; By = B_ya[i]
                ts("dve", ya, rr[i][0][:], w12[:, 0, tI:tI + 1], ALU.mult, [B_rr[i], B_rs], [By])
                stt(ya, rr[i][1][:], w12[:, 1, tI:tI + 1], ya, ALU.mult, ALU.add, [B_rr[i], B_rs, By], [By])
                tt("pool", ya, ya, g2p[:], ALU.mult, [By, B_bc], [By])
                stt(ya, x1t[i][:], ALPHA, ya, ALU.mult, ALU.add, [B_x1t[i], By], [By])
                ks.op("dve", lambda e, ya=ya: e.reduce_sum(out=st5[:, 0:1], in_=ya, axis=AX.X), reads=[By], writes=[B_st5])
                act(junk5[:], ya, AF.Square, [By], [B_j5, B_st5], accum_out=st5[:, 1:2])
                ts("dve", st5[:, 2:3], st5[:, 0:1], 1.0 / D, ALU.mult, [B_st5], [B_st5])
                tt("dve", st5[:, 3:4], st5[:, 2:3], st5[:, 2:3], ALU.mult, [B_st5], [B_st5])
                stt(st5[:, 4:5], st5[:, 1:2], 1.0 / D, st5[:, 3:4], ALU.mult, ALU.subtract, [B_st5], [B_st5])
                act(st5[:, 5:6], st5[:, 4:5], AF.Sqrt, [B_st5, B_c], [B_st5], bias=ce5, scale=1.0)
                ks.op("dve", lambda e: e.reciprocal(st5[:, 6:7], st5[:, 5:6]), reads=[B_st5], writes=[B_st5])
                ts("dve", ya, ya, st5[:, 2:3], ALU.subtract, [By, B_st5], [By], s2=st5[:, 6:7], op1=ALU.mult)
                tt("pool", ya, ya, l2g[:], ALU.mult, [By, B_bc], [By])
                tt("dve", ya, ya, l2b[:], ALU.add, [By, B_bc], [By])
                ks.dma("sp", out[tI * 128:(tI + 1) * 128, :], ya, reads=[By], writes=[Buf()])
            ks.barrier()

        return _finish(nc, ks, [])


def _finish(nc, ks, bufs):
    for key, val in ks.cnt.items():
        if key.startswith("d_") and val > 0:
            ks._wait("sp", (key, val))
    print("kernel build: insts=%d waits=%d" % (ks.n_inst, ks.n_wait))
    return nc


def _col_perm():
    idx = list(range(0, 4096)) + list(range(4112, 4112 + 3072)) + list(range(4096, 4112)) + list(range(7184, 7192))
    return np.asarray(idx)


def make_in_maps(inputs, n_cores=8):
    f = lambda a: np.ascontiguousarray(np.asarray(a, dtype=np.float32))
    x = np.asarray(inputs["x"]); c = np.asarray(inputs["c"])
    shared = {
        "w_ada": f(inputs["w_ada"][0]),
        "b_ada": f(inputs["b_ada"][0][None, :]),
        "w_in": f(inputs["w_in"][0][:, _col_perm()]),
        "convw": f(np.asarray(inputs["dn_conv_w"][0]).reshape(4, 24, 128).transpose(2, 1, 0)),
        "a_log": f(inputs["dn_a_log"][0][None, :]),
        "dt_bias": f(inputs["dn_dt_bias"][0][None, :]),
        "f_bias": f(inputs["fox_f_bias"][0][None, :]),
        "norm_w": f(np.asarray(inputs["dn_norm_w"][0])[:, None]),
        "w_out": f(inputs["w_out"][0]),
        "ln1_g": f(inputs["ln1_g"][0][None, :]), "ln1_b": f(inputs["ln1_b"][0][None, :]),
        "ln2_g": f(inputs["ln2_g"][0][None, :]), "ln2_b": f(inputs["ln2_b"][0][None, :]),
        "w_r": f(np.concatenate([np.asarray(inputs["w_router_group"][0])] +
                                [np.asarray(inputs["w_router_expert"][0][g]) for g in range(4)], axis=1)),
        "b_r": f(np.concatenate([np.asarray(inputs["b_router_group"][0])] +
                                [np.asarray(inputs["b_router_expert"][0][g]) for g in range(4)])[None, :]),
    }
    w_gate = np.asarray(inputs["w_gate"][0], dtype=np.float32).reshape(32, KC, 128, 512).transpose(0, 2, 1, 3).reshape(32, 128, KC * 512)
    w_up = np.asarray(inputs["w_up"][0], dtype=np.float32).reshape(32, KC, 128, 512).transpose(0, 2, 1, 3).reshape(32, 128, KC * 512)
    w_down = np.asarray(inputs["w_down"][0], dtype=np.float32).reshape(32, 4, 128, D).transpose(0, 2, 1, 3).reshape(32, 128, 4 * D)
    shared["w_moe"] = np.ascontiguousarray(np.concatenate([w_gate, w_up, w_down], axis=2))
    maps = []
    for b in range(n_cores):
        m = dict(shared)
        m["x"] = f(x[b])
        m["xT"] = f(x[b].T)
        m["ccol"] = f(c[b].reshape(KC, 128).T)
        maps.append(m)
    return maps


def kernel(**inputs):
    nc = build_nc()
    maps = make_in_maps(inputs)
    res = run_bass_kernel_spmd(nc, maps, core_ids=list(range(8)))
    return np.stack([np.asarray(r["out"], dtype=np.float32) for r in res.results], axis=0)
```

```python
import os
import numpy as np
from contextlib import ExitStack
import concourse.bass as bass
import concourse.mybir as mybir
from concourse.bass_utils import run_bass_kernel_spmd

F32 = mybir.dt.float32
BF16 = mybir.dt.bfloat16
I32 = mybir.dt.int32
AF = mybir.ActivationFunctionType
ALU = mybir.AluOpType
AX = mybir.AxisListType

D = 2048
S = 4096
KC = D // 128
NT = S // 128
NG = S // 512
NH = 8
DIN = 7192
ALPHA = 2.0 ** 0.25


class Bank:
    __slots__ = ("acc",)

    def __init__(self):
        self.acc = {}


class Buf:
    __slots__ = ("name", "w", "r", "bank")

    def __init__(self, name="", bank=None):
        self.name = name
        self.w = None
        self.r = {}
        self.bank = bank


class KS:
    ENG = ("pe", "act", "dve", "pool", "sp")

    def __init__(self, nc, stack, n_dsem=12, same_eng_sync=True):
        self.nc = nc
        self.eng = {"pe": nc.tensor, "act": nc.scalar, "dve": nc.vector,
                    "pool": nc.gpsimd, "sp": nc.sync}
        self.same_eng_sync = same_eng_sync
        self.sems = {}
        self.cnt = {}
        for e in self.ENG:
            self.sems[e] = stack.enter_context(nc.semaphore("c_" + e))
            self.cnt[e] = 0
        self.dq = {}
        for q in ("sp", "pool", "act"):
            lst = []
            for i in range(n_dsem):
                key = "d_%s_%d" % (q, i)
                self.sems[key] = stack.enter_context(nc.semaphore(key))
                self.cnt[key] = 0
                lst.append(key)
            self.dq[q] = [lst, 0]
        self.waited = {e: {} for e in self.ENG}
        self.n_wait = 0
        self.n_inst = 0

    def _wait(self, e, dep):
        if dep is None:
            return
        key, val = dep
        if key == e and (e == "pe" or not self.same_eng_sync):
            return
        if self.waited[e].get(key, 0) >= val:
            return
        self.eng[e].wait_ge(self.sems[key], val)
        self.waited[e][key] = val
        self.n_wait += 1

    def _deps(self, e, reads, writes):
        for b in reads:
            self._wait(e, b.w)
        for b in writes:
            self._wait(e, b.w)
            for k, v in b.r.items():
                self._wait(e, (k, v))
        for b in list(reads) + list(writes):
            if b.bank is not None:
                for k, v in b.bank.acc.items():
                    if k != e:
                        self._wait(e, (k, v))

    def _mark(self, tag, reads, writes):
        key, val = tag
        for b in reads:
            if b.r.get(key, 0) < val:
                b.r[key] = val
        for b in writes:
            b.w = tag
            b.r = {}
        for b in list(reads) + list(writes):
            if b.bank is not None:
                b.bank.acc[key] = val

    def op(self, e, fn, reads=(), writes=()):
        self._deps(e, reads, writes)
        inst = fn(self.eng[e])
        self.cnt[e] += 1
        inst.then_inc(self.sems[e], 1)
        self._mark((e, self.cnt[e]), reads, writes)
        self.n_inst += 1
        return inst

    def dma(self, q, out, in_, reads=(), writes=(), fn=None, **kw):
        lst, idx = self.dq[q]
        key = lst[idx % len(lst)]
        self.dq[q][1] = idx + 1
        if self.cnt[key] > 0:
            self._wait(q, (key, self.cnt[key]))
        self._deps(q, reads, writes)
        if fn is not None:
            inst = fn(self.eng[q])
        else:
            inst = self.eng[q].dma_start(out=out, in_=in_, **kw)
        self.cnt[key] += 16
        inst.then_inc(self.sems[key], 16)
        self._mark((key, self.cnt[key]), reads, writes)
        self.n_inst += 1
        return inst

    def wait_all(self, e, bufs):
        for b in bufs:
            self._wait(e, b.w)

    def barrier(self):
        for e in self.ENG:
            for key, val in self.cnt.items():
                if val > 0 and key != e:
                    self._wait(e, (key, val))
            if e != "pe" and self.cnt[e] > 0 and self.same_eng_sync:
                self._wait(e, (e, self.cnt[e]))


def build_nc(debug=(), stop_after=None):
    nc = bass.Bass("TRN2", target_bir_lowering=False)
    dbg = set(debug)

    def din(name, shape, dt=F32):
        return nc.dram_tensor(name, list(shape), dt, kind="ExternalInput").ap()

    def dscr(name, shape, dt=F32):
        kind = "ExternalOutput" if name in dbg else "Internal"
        return nc.dram_tensor(name, list(shape), dt, kind=kind).ap()

    xT = din("xT", [D, S])
    xtok = din("x", [S, D])
    ccol = din("ccol", [128, KC])
    w_ada = din("w_ada", [D, 6 * D])
    b_ada = din("b_ada", [1, 6 * D])
    w_in = din("w_in", [D, DIN])
    convw = din("convw", [128, 24, 4])
    a_log = din("a_log", [1, NH])
    dt_bias = din("dt_bias", [1, NH])
    f_bias = din("f_bias", [1, NH])
    norm_w = din("norm_w", [128, 1])
    w_out = din("w_out", [D, D])
    ln1_g = din("ln1_g", [1, D]); ln1_b = din("ln1_b", [1, D])
    ln2_g = din("ln2_g", [1, D]); ln2_b = din("ln2_b", [1, D])
    w_r = din("w_r", [D, 36]); b_r = din("b_r", [1, 36])
    out = nc.dram_tensor("out", [S, D], F32, kind="ExternalOutput").ap()

    modrow = dscr("modrow", [1, 6 * D])
    projT = dscr("projT", [48, 128, S], BF16)
    vtok = dscr("vtok", [S, 1024], BF16)
    gat = dscr("gat", [S, 24], F32)

    with ExitStack() as st:
        ks = KS(nc, st, same_eng_sync=(os.environ.get("SES", "1") == "1"))
        cst = ExitStack(); st.enter_context(cst)

        def sbt(stack, name, shape, dt):
            return stack.enter_context(nc.sbuf_tensor(name, list(shape), dt))

        BANKS = {}

        def pst(stack, name, shape, dt=F32):
            full = 512 if dt == F32 else 1024
            t_ = stack.enter_context(nc.psum_tensor(name, [128, full], dt))
            BANKS[name] = Bank()
            P = shape[0]
            n = 1
            for d_ in shape[1:]:
                n *= d_
            v = t_[0:P, 0:n]
            if len(shape) == 3:
                v = v.rearrange("p (a b) -> p a b", b=shape[2])
            return v

        def PB(name):
            return Buf(name, BANKS[name])

        ident_f = sbt(cst, "ident_f", [128, 128], F32); B_ident = Buf("ident")
        ident_h = sbt(cst, "ident_h", [128, 128], BF16)
        ks.op("pool", lambda e: e.memset(ident_f[:], 1.0), writes=[B_ident])
        ks.op("pool", lambda e: e.affine_select(out=ident_f[:], in_=ident_f[:], pattern=[[-1, 128]],
                                                compare_op=ALU.is_equal, fill=0.0, base=0, channel_multiplier=1),
              reads=[B_ident], writes=[B_ident])
        ks.op("pool", lambda e: e.tensor_copy(ident_h[:], ident_f[:]), reads=[B_ident], writes=[B_ident])
        sc1c = sbt(cst, "sc1c", [128, KC], F32)
        sh1c = sbt(cst, "sh1c", [128, KC], F32)
        B_mc = Buf("modcols")
        B_modrow = Buf("modrow")

        with ExitStack() as ph:
            cc = sbt(ph, "cc", [128, KC], F32); B_cc = Buf()
            sc = sbt(ph, "sc", [128, KC], F32); B_sc = Buf()
            brow = sbt(ph, "brow", [1, 6 * D], F32); B_brow = Buf()
            mrow = sbt(ph, "mrow", [1, 6 * D], F32); B_mrow = Buf()
            wa = [sbt(ph, "wa%d" % i, [128, KC, 512], F32) for i in range(2)]; B_wa = [Buf(), Buf()]
            pm = [pst(ph, "pm%d" % i, [1, 512]) for i in range(2)]; B_pm = [PB("pm0"), PB("pm1")]
            ks.dma("sp", cc[:], ccol[:, :], writes=[B_cc])
            ks.dma("sp", brow[:], b_ada[:, :], writes=[B_brow])
            ks.op("act", lambda e: e.activation(out=sc[:], in_=cc[:], func=AF.Silu), reads=[B_cc], writes=[B_sc])
            w_ada_v = w_ada.rearrange("(kc p) n -> p kc n", p=128)
            NCG = 6 * D // 512
            for cg in range(NCG):
                i = cg % 2
                ks.dma("sp", wa[i][:], w_ada_v[:, :, cg * 512:(cg + 1) * 512], writes=[B_wa[i]])
                for kc in range(KC):
                    ks.op("pe", lambda e, kc=kc, i=i: e.matmul(pm[i][:], lhsT=sc[:, kc:kc + 1], rhs=wa[i][:, kc, :],
                                                             start=(kc == 0), stop=(kc == KC - 1)),
                          reads=[B_sc, B_wa[i]], writes=[B_pm[i]])
                ks.op("dve", lambda e, cg=cg, i=i: e.tensor_tensor(out=mrow[:, cg * 512:(cg + 1) * 512], in0=pm[i][:],
                                                                   in1=brow[:, cg * 512:(cg + 1) * 512], op=ALU.add),
                      reads=[B_pm[i], B_brow], writes=[B_mrow])
            ks.dma("sp", modrow[:, :], mrow[:], reads=[B_mrow], writes=[B_modrow])
            t16 = sbt(ph, "t16", [KC, 2, 128], F32); B_t16 = Buf()
            ks.dma("sp", t16[:, 0, :], modrow[0, 0:D].rearrange("(kc p) -> kc p", p=128), reads=[B_modrow], writes=[B_t16])
            ks.dma("sp", t16[:, 1, :], modrow[0, D:2 * D].rearrange("(kc p) -> kc p", p=128), reads=[B_modrow], writes=[B_t16])
            pt = pst(ph, "pt", [128, 2, KC]); B_pt = PB("pt")
            for j in range(2):
                ks.op("pe", lambda e, j=j: e.transpose(pt[:, j, :], t16[:, j, :], ident_f[0:KC, 0:KC]),
                      reads=[B_t16, B_ident], writes=[B_pt])
            ks.op("dve", lambda e: e.tensor_copy(sh1c[:], pt[:, 0, :]), reads=[B_pt], writes=[B_mc])
            ks.op("dve", lambda e: e.tensor_scalar(out=sc1c[:], in0=pt[:, 1, :], scalar1=1.0, scalar2=None, op0=ALU.add),
                  reads=[B_pt], writes=[B_mc])
            ks.barrier()
        if stop_after == 0:
            return _finish(nc, ks, [B_modrow])

        B_projT = [Buf("projT%d" % j) for j in range(48)]
        B_vtok = Buf("vtok"); B_gat = Buf("gat")
        with ExitStack() as ph:
            hT = sbt(ph, "hT", [128, KC, S], BF16); B_hT = [Buf() for _ in range(KC)]
            wf = [sbt(ph, "wf%d" % i, [128, KC, 256], F32) for i in range(2)]; B_wf = [Buf(), Buf()]
            xs = [wf[i][:, 0:8, :].rearrange("p a b -> p (a b)") for i in range(2)]; B_xs = B_wf
            xT_v = xT.rearrange("(kc p) t -> p kc t", p=128)
            n = 0
            for kc in range(KC):
                for hf in range(2):
                    i = n % 2; n += 1
                    ks.dma("sp", xs[i], xT_v[:, kc, hf * 2048:(hf + 1) * 2048], writes=[B_xs[i]])
                    ks.op("act", lambda e, kc=kc, hf=hf, i=i: e.activation(
                        out=hT[:, kc, hf * 2048:(hf + 1) * 2048], in_=xs[i], func=AF.Identity,
                        scale=sc1c[:, kc:kc + 1], bias=sh1c[:, kc:kc + 1]),
                        reads=[B_xs[i], B_mc], writes=[B_hT[kc]])
            wh = [sbt(ph, "wh%d" % i, [128, KC, 256], BF16) for i in range(2)]; B_wh = [Buf(), Buf()]
            stg_all = sbt(ph, "stg", [128, 2 * S], BF16)
            stg = [stg_all[:, i * S:(i + 1) * S] for i in range(2)]; B_stg = [Buf(), Buf()]
            vst = stg_all[:, :].rearrange("p (t c) -> p t c", c=256)
            gst = sbt(ph, "gst", [128, NT, 24], F32); B_gst = Buf()
            pp = [pst(ph, "pp%d" % i, [128, 512]) for i in range(4)]; B_pp = [PB("pp%d" % i) for i in range(4)]
            w_in_v = w_in.rearrange("(kc p) n -> p kc n", p=128)
            n_cg = (DIN + 255) // 256
            pi = 0; si = 0; ev = 0
            for cg in range(n_cg):
                i = cg % 2
                c0 = cg * 256
                ncol = min(256, DIN - c0)
                ks.dma("sp", wf[i][:, :, 0:ncol], w_in_v[:, :, c0:c0 + ncol], writes=[B_wf[i]])
                ks.op("pool", lambda e, i=i, ncol=ncol: e.tensor_copy(wh[i][:, :, 0:ncol], wf[i][:, :, 0:ncol]),
                      reads=[B_wf[i]], writes=[B_wh[i]])
                if c0 < 6144:
                    for jb in range(2):
                        j = cg * 2 + jb
                        s_ = si % 2; si += 1
                        for tg in range(NG):
                            p_ = pi % 4; pi += 1
                            for kc in range(KC):
                                ks.op("pe", lambda e, kc=kc, i=i, jb=jb, tg=tg, p_=p_: e.matmul(
                                    pp[p_][:], lhsT=wh[i][:, kc, jb * 128:(jb + 1) * 128],
                                    rhs=hT[:, kc, tg * 512:(tg + 1) * 512], start=(kc == 0), stop=(kc == KC - 1)),
                                    reads=[B_wh[i], B_hT[kc]], writes=[B_pp[p_]])
                            if ev % 2 == 0:
                                ks.op("act", lambda e, s_=s_, tg=tg, p_=p_: e.activation(
                                    out=stg[s_][:, tg * 512:(tg + 1) * 512], in_=pp[p_][:], func=AF.Copy),
                                    reads=[B_pp[p_]], writes=[B_stg[s_]])
                            else:
                                ks.op("dve", lambda e, s_=s_, tg=tg, p_=p_: e.tensor_copy(
                                    stg[s_][:, tg * 512:(tg + 1) * 512], pp[p_][:]),
                                    reads=[B_pp[p_]], writes=[B_stg[s_]])
                            ev += 1
                        ks.dma("sp", projT[j], stg[s_], reads=[B_stg[s_]], writes=[B_projT[j]])
                else:
                    isg = ncol < 256
                    for tt in range(NT):
                        p_ = pi % 4; pi += 1
                        for kc in range(KC):
                            ks.op("pe", lambda e, kc=kc, i=i, tt=tt, p_=p_, ncol=ncol: e.matmul(
                                pp[p_][:, 0:ncol], lhsT=hT[:, kc, tt * 128:(tt + 1) * 128],
                                rhs=wh[i][:, kc, 0:ncol], start=(kc == 0), stop=(kc == KC - 1)),
                                reads=[B_wh[i], B_hT[kc]], writes=[B_pp[p_]])
                        if isg:
                            ks.op("dve", lambda e, tt=tt, p_=p_: e.tensor_copy(gst[:, tt, :], pp[p_][:, 0:24]),
                                  reads=[B_pp[p_]], writes=[B_gst])
                        elif ev % 2 == 0:
                            ks.op("act", lambda e, tt=tt, p_=p_: e.activation(out=vst[:, tt, :], in_=pp[p_][:, 0:256], func=AF.Copy),
                                  reads=[B_pp[p_]], writes=B_stg)
                        else:
                            ks.op("dve", lambda e, tt=tt, p_=p_: e.tensor_copy(vst[:, tt, :], pp[p_][:, 0:256]),
                                  reads=[B_pp[p_]], writes=B_stg)
                        ev += 1
                    if isg:
                        ks.dma("sp", gat.rearrange("(tt p) c -> p tt c", p=128), gst[:], reads=[B_gst], writes=[B_gat])
                    else:
                        v0 = c0 - 6144
                        ks.dma("sp", vtok[:, v0:v0 + 256].rearrange("(tt p) c -> p tt c", p=128), vst,
                               reads=B_stg, writes=[B_vtok])
            ks.barrier()
        if stop_after == 1:
            return _finish(nc, ks, B_projT + [B_vtok, B_gat])

        def act(out, in_, func, R, W, **kw):
            return ks.op("act", lambda e: e.activation(out=out, in_=in_, func=func, **kw), reads=R, writes=W)

        def ts(eng, out, in0, s1, op0, R, W, s2=None, op1=None):
            kw = dict(out=out, in0=in0, scalar1=s1, scalar2=s2, op0=op0)
            if op1 is not None:
                kw["op1"] = op1
            return ks.op(eng, lambda e: e.tensor_scalar(**kw), reads=R, writes=W)

        def tt(eng, out, in0, in1, op, R, W):
            return ks.op(eng, lambda e: e.tensor_tensor(out=out, in0=in0, in1=in1, op=op), reads=R, writes=W)

        def stt(out, in0, scalar, in1, op0, op1, R, W):
            return ks.op("dve", lambda e: e.scalar_tensor_tensor(out=out, in0=in0, scalar=scalar, in1=in1, op0=op0, op1=op1),
                         reads=R, writes=W)

        def mm(out, lhsT, rhs, start, stop, R, W):
            return ks.op("pe", lambda e: e.matmul(out, lhsT=lhsT, rhs=rhs, start=start, stop=stop), reads=R, writes=W)

        def tr(out, in_, idn, R, W):
            return ks.op("pe", lambda e: e.transpose(out, in_, idn), reads=R, writes=W)

        def cp(eng, out, in_, R, W):
            if eng == "act":
                return act(out, in_, AF.Copy, R, W)
            return ks.op(eng, lambda e: e.tensor_copy(out, in_), reads=R, writes=W)

        def asel(out, in_, cmp, fill, R, W):
            if cmp == "le":
                pat, cm, base = [[1, 128]], -1, 0
            else:
                pat, cm, base = [[-1, 128]], 1, -1
            return ks.op("pool", lambda e: e.affine_select(out=out, in_=in_, pattern=pat, compare_op=ALU.is_ge,
                                                           fill=fill, base=base, channel_multiplier=cm), reads=R, writes=W)

        B_c = Buf("consts")
        ones_f = sbt(cst, "ones_f", [128, 128], F32); ones_h = sbt(cst, "ones_h", [128, 128], BF16)
        UT_f = sbt(cst, "UT_f", [128, 128], F32)
        maskL = sbt(cst, "maskL", [128, 128], F32); maskU = sbt(cst, "maskU", [128, 128], F32)
        ccols = sbt(cst, "ccols", [128, 4], F32)
        ks.op("pool", lambda e: e.memset(ones_f[:], 1.0), writes=[B_c])
        ks.op("pool", lambda e: e.memset(ones_h[:], 1.0), writes=[B_c])
        ks.op("pool", lambda e: e.memset(UT_f[:], 1.0), writes=[B_c])
        asel(UT_f[:], UT_f[:], "le", 0.0, [B_c], [B_c])
        ks.op("pool", lambda e: e.memset(maskL[:], 0.0), writes=[B_c])
        asel(maskL[:], maskL[:], "gt", 1.0e4, [B_c], [B_c])
        ks.op("pool", lambda e: e.memset(maskU[:], 0.0), writes=[B_c])
        asel(maskU[:], maskU[:], "le", -1.0e4, [B_c], [B_c])
        ks.op("pool", lambda e: e.memset(ccols[:, 0:1], 0.0), writes=[B_c])
        ks.op("pool", lambda e: e.memset(ccols[:, 1:2], 1.0), writes=[B_c])
        ks.op("pool", lambda e: e.memset(ccols[:, 2:3], 1.0e-6), writes=[B_c])
        ks.op("pool", lambda e: e.memset(ccols[:, 3:4], 1.0e-5), writes=[B_c])
        c0_, c1_, ce6, ce5 = ccols[:, 0:1], ccols[:, 1:2], ccols[:, 2:3], ccols[:, 3:4]
        STOPG = int(os.environ.get("STOPG", "0"))
        if STOPG == 1:
            return _finish(nc, ks, [])

        oT = dscr("oT", [16, 128, S], BF16)
        B_oT = [[Buf() for _ in range(NG)] for _ in range(16)]

        gst_ = ExitStack(); st.enter_context(gst_)
        B_g = Buf("gates")
        gt = sbt(gst_, "gt", [128, NT, 24], F32)
        prm = sbt(gst_, "prm", [128, 3, NH], F32)
        G8 = lambda nm: sbt(gst_, nm, [128, NH, NT], F32)
        beta = G8("beta"); nbeta = G8("nbeta"); gl = G8("gl"); Gc = G8("Gc"); nbg = G8("nbg")
        kds = G8("kds"); eGl = G8("eGl"); tmpg = G8("tmpg"); lf = G8("lf"); Fc = G8("Fc"); nF = G8("nF")
        nea = sbt(gst_, "nea", [128, NH], F32); nfb = sbt(gst_, "nfb", [128, NH], F32)
        ks.dma("sp", gt[:], gat.rearrange("(tt p) c -> p tt c", p=128), reads=[B_gat], writes=[B_g])
        ks.dma("sp", prm[:, 0, :], a_log[0:1, :].broadcast_to([128, NH]), writes=[B_g])
        ks.dma("sp", prm[:, 1, :], dt_bias[0:1, :].broadcast_to([128, NH]), writes=[B_g])
        ks.dma("sp", prm[:, 2, :], f_bias[0:1, :].broadcast_to([128, NH]), writes=[B_g])
        RG = [B_g, B_c]
        act(nea[:], prm[:, 0, :], AF.Exp, RG, [B_g])
        ts("dve", nea[:], nea[:], -1.0, ALU.mult, RG, [B_g])
        ts("dve", nfb[:], prm[:, 2, :], -1.0, ALU.mult, RG, [B_g])
        if STOPG == 2:
            return _finish(nc, ks, [])
        for h in range(NH):
            act(beta[:, h, :], gt[:, :, h], AF.Sigmoid, RG, [B_g])
        for h in range(NH):
            act(tmpg[:, h, :], gt[:, :, 8 + h], AF.Exp, RG, [B_g], bias=prm[:, 1, h:h + 1], scale=1.0)
            act(lf[:, h, :], gt[:, :, 16 + h], AF.Exp, RG, [B_g], bias=nfb[:, h:h + 1], scale=-1.0)
        gflat = lambda t_: t_[:, :, :].rearrange("p h t -> p (h t)")
        act(gflat(tmpg), gflat(tmpg), AF.Ln, RG, [B_g], bias=c1_, scale=1.0)
        act(gflat(lf), gflat(lf), AF.Ln, RG, [B_g], bias=c1_, scale=1.0)
        ts("dve", gflat(lf), gflat(lf), -1.0, ALU.mult, RG, [B_g])
        for h in range(NH):
            ts("dve", gl[:, h, :], tmpg[:, h, :], nea[:, h:h + 1], ALU.mult, RG, [B_g])
        ts("dve", gflat(nbeta), gflat(beta), -1.0, ALU.mult, RG, [B_g])
        if STOPG == 3:
            return _finish(nc, ks, [])
        with ExitStack() as gp:
            pg = pst(gp, "pg", [128, 2, NH * NT]); B_pg = PB("pg")
            mm(pg[:, 0, :], UT_f[:], gflat(gl), True, True, RG, [B_pg])
            mm(pg[:, 1, :], ones_f[:], gflat(gl), True, True, RG, [B_pg])
            cp("dve", gflat(Gc), pg[:, 0, :], [B_pg], [B_g])
            act(gflat(eGl), pg[:, 1, :], AF.Exp, [B_pg, B_c], [B_g])
            tt("dve", gflat(kds), pg[:, 1, :], gflat(Gc), ALU.subtract, [B_pg, B_g], [B_g])
            act(gflat(kds), gflat(kds), AF.Exp, RG, [B_g])
            act(gflat(tmpg), gflat(Gc), AF.Exp, RG, [B_g])
            tt("dve", gflat(nbg), gflat(tmpg), gflat(nbeta), ALU.mult, RG, [B_g])
            if STOPG == 4:
                return _finish(nc, ks, [])
            mm(pg[:, 0, :], UT_f[:], gflat(lf), True, True, RG, [B_pg])
            mm(pg[:, 1, :], ones_f[:], gflat(lf), True, True, RG, [B_pg])
            cp("dve", gflat(Fc), pg[:, 0, :], [B_pg], [B_g])
            cp("dve", gflat(nF), pg[:, 1, :], [B_pg], [B_g])
            for h in range(NH):
                ks.op("dve", lambda e, h=h: e.tensor_tensor_scan(out=tmpg[:, h, :], data0=ones_f[:, 0:NT], data1=nF[:, h, :],
                                                                 initial=0.0, op0=ALU.mult, op1=ALU.add), reads=RG, writes=[B_g])
            if STOPG == 5:
                return _finish(nc, ks, [])
            tt("dve", gflat(tmpg), gflat(tmpg), gflat(nF), ALU.subtract, RG, [B_g])
            tt("dve", gflat(Fc), gflat(Fc), gflat(tmpg), ALU.add, RG, [B_g])
            ts("dve", gflat(nF), gflat(Fc), -1.0, ALU.mult, RG, [B_g])
            if STOPG == 6:
                return _finish(nc, ks, [])
            ks.barrier()

        if stop_after == 1.5:
            return _finish(nc, ks, [])
        with ExitStack() as ph:
            raw = [[sbt(ph, "raw%d_%d" % (p_, i), [128, 515], BF16) for i in range(3)] for p_ in range(2)]
            B_raw = [[Buf() for i in range(3)] for p_ in range(2)]
            cw = sbt(ph, "cw", [128, 24, 4], F32); B_cw = Buf()
            nw = sbt(ph, "nw", [128, 1], F32)
            ks.dma("sp", cw[:], convw[:, :, :], writes=[B_cw])
            ks.dma("sp", nw[:], norm_w[:, :], writes=[B_cw])
            cacc = [sbt(ph, "cacc%d" % i, [128, 512], F32) for i in range(3)]; B_cacc = [Buf() for _ in range(3)]
            sqb = [sbt(ph, "sqb%d" % i, [128, 512], BF16) for i in range(2)]; B_sqb = [Buf(), Buf()]
            rsb = [sbt(ph, "rsb%d" % i, [128, 512], F32) for i in range(2)]; B_rsb = [Buf(), Buf()]
            qnG = sbt(ph, "qnG", [128, 2, NH, 512], BF16); knG = sbt(ph, "knG", [128, 2, NH, 512], BF16)
            vTG = sbt(ph, "vTG", [128, 2, NH, 512], BF16)
            B_qkv = [[Buf() for _ in range(NH)] for _ in range(2)]
            kdec = sbt(ph, "kdec", [128, NH, 4, 128], BF16); vb = sbt(ph, "vb", [128, NH, 4, 128], BF16)
            TT = sbt(ph, "TT", [128, NH, 4, 128], BF16); qkT = sbt(ph, "qkT", [128, NH, 4, 128], BF16)
            qg = sbt(ph, "qg", [128, NH, 4, 128], BF16)
            B_ck = [[Buf() for _ in range(4)] for _ in range(NH)]
            B_TT = [[Buf() for _ in range(4)] for _ in range(NH)]
            oTs = sbt(ph, "oTs", [128, NH, 512], F32); B_oTs = [[Buf() for _ in range(4)] for _ in range(NH)]
            S_f = sbt(ph, "S_f", [128, NH, 128], F32); S_h = sbt(ph, "S_h", [128, NH, 128], BF16); B_S = [Buf() for _ in range(NH)]
            NB = 2
            W4 = 4
            mk = lambda nm, dt_: [[sbt(ph, "%s%d_%d" % (nm, i, j), [128, 128], dt_) for j in range(W4)] for i in range(NB)]
            diagG = mk("diagG", F32); mL = mk("mL", F32); mU = mk("mU", F32); eGb = mk("eGb", F32)
            Pn = [mk("PnA", BF16), mk("PnB", BF16)]; Pt = [mk("PtA", BF16), mk("PtB", BF16)]; XT = [mk("XTA", BF16), mk("XTB", BF16)]
            B_tmp = [[Buf() for _ in range(W4)] for _ in range(NB)]
            rbuf = sbt(ph, "rbuf", [128, NH, 128], BF16); B_r = [Buf() for _ in range(NH)]
            vnew = sbt(ph, "vnew", [128, NH, 128], BF16); B_vn = [Buf() for _ in range(NH)]
            zb = [sbt(ph, "zb%d" % i, [128, 512], BF16) for i in range(2)]; B_zb = [Buf(), Buf()]
            zs = [sbt(ph, "zs%d" % i, [128, 512], F32) for i in range(2)]
            on_ = [sbt(ph, "on%d" % i, [128, 512], F32) for i in range(2)]
            og_ = [sbt(ph, "og%d" % i, [128, 512], BF16) for i in range(2)]; B_og = [Buf(), Buf()]
            P_L = [pst(ph, "P_L%d" % i, [128, 4, 128]) for i in range(3)]
            B_L = [[PB("P_L%d" % i) for _ in range(W4)] for i in range(3)]
            P_T = pst(ph, "P_T", [128, 8, 128], BF16); B_PT = [PB("P_T") for _ in range(8)]
            P_Sa = pst(ph, "P_Sa", [128, 4, 128]); B_Sa = [PB("P_Sa") for _ in range(4)]
            P_Sb = pst(ph, "P_Sb", [128, 4, 128]); B_Sb = [PB("P_Sb") for _ in range(4)]
            P_N = pst(ph, "P_N", [128, 512]); B_PN = PB("P_N")
            cnt = {"sq": 0, "z": 0, "w": 0}
            for h in range(NH):
                ks.op("dve", lambda e, h=h: e.memset(S_f[:, h, :], 0.0), writes=[B_S[h]])
                ks.op("dve", lambda e, h=h: e.memset(S_h[:, h, :], 0.0), writes=[B_S[h]])

            def gprep(h, tg):
                par = tg % 2
                t0 = tg * 512
                srcs = [projT[h], projT[8 + h], projT[16 + h]]
                dsts = [qnG[:, par, h, :], knG[:, par, h, :], vTG[:, par, h, :]]
                Bd = B_qkv[par][h]
                for i in range(3):
                    rb = raw[h % 2][i]; Br = B_raw[h % 2][i]
                    if tg == 0:
                        ks.op("pool", lambda e, rb=rb: e.memset(rb[:, 0:3], 0.0), writes=[Br])
                        ks.dma("sp", rb[:, 3:515], srcs[i][:, 0:512], reads=[B_projT[[h, 8 + h, 16 + h][i]]], writes=[Br])
                    else:
                        ks.dma("sp", rb[:, 0:515], srcs[i][:, t0 - 3:t0 + 512], reads=[B_projT[[h, 8 + h, 16 + h][i]]], writes=[Br])
                    blk = i * 8 + h
                    ca = cacc[i]; Bc = B_cacc[i]
                    ts("dve", ca[:], rb[:, 3:515], cw[:, blk, 3:4], ALU.mult, [Br, B_cw], [Bc])
                    for k in range(3):
                        stt(ca[:], rb[:, k:k + 512], cw[:, blk, k:k + 1], ca[:], ALU.mult, ALU.add, [Br, B_cw, Bc], [Bc])
                    if i == 2:
                        act(dsts[2], ca[:], AF.Silu, [Bc], [Bd])
                    else:
                        act(ca[:], ca[:], AF.Silu, [Bc], [Bc])
                        j = cnt["sq"] % 2; cnt["sq"] += 1
                        tt("pool", sqb[j][:], ca[:], ca[:], ALU.mult, [Bc], [B_sqb[j]])
                        mm(P_N[:], ones_h[:], sqb[j][:], True, True, [B_sqb[j], B_c], [B_PN])
                        act(rsb[j][:], P_N[:], AF.Ln, [B_PN, B_c], [B_rsb[j]], bias=ce6, scale=1.0)
                        act(rsb[j][:], rsb[j][:], AF.Exp, [B_rsb[j]], [B_rsb[j]], scale=-0.5)
                        if i == 0:
                            stt(dsts[0], ca[:], 128.0 ** -0.5, rsb[j][:], ALU.mult, ALU.mult, [Bc, B_rsb[j]], [Bd])
                        else:
                            tt("dve", dsts[1], ca[:], rsb[j][:], ALU.mult, [Bc, B_rsb[j]], [Bd])

            def wave(tg, c, hs):
                par = tg % 2; n = tg * 4 + c
                b = cnt["w"] % NB; cnt["w"] += 1
                cs = slice(c * 128, (c + 1) * 128)
                J = list(range(len(hs)))
                knc = lambda j: knG[:, par, hs[j], cs]
                qnc = lambda j: qnG[:, par, hs[j], cs]
                vTc = lambda j: vTG[:, par, hs[j], cs]
                Bq = lambda j: B_qkv[par][hs[j]]
                Bt = lambda j: B_tmp[b][j]
                Bk = lambda j: B_ck[hs[j]][c]
                parts = []

                def p0():
                    for j in J:
                        tr(P_T[:, j, :], knc(j), ident_h[:], [Bq(j), B_ident], [B_PT[j]])
                        tr(P_T[:, 4 + j, :], vTc(j), ident_h[:], [Bq(j), B_ident], [B_PT[4 + j]])
                    for j in J:
                        h = hs[j]
                        ts("dve", kdec[:, h, c, :], P_T[:, j, :], kds[:, h, n:n + 1], ALU.mult, [B_PT[j], B_g], [Bk(j)])
                        ts("dve", vb[:, h, c, :], P_T[:, 4 + j, :], beta[:, h, n:n + 1], ALU.mult, [B_PT[4 + j], B_g], [Bk(j)])
                    for j in J:
                        h = hs[j]
                        ts("pool", diagG[b][j][:], ident_f[:], Gc[:, h, n:n + 1], ALU.mult, [B_ident, B_g], [Bt(j)])
                        mm(P_L[0][:, j, :], ones_f[:], diagG[b][j][:], True, True, [Bt(j), B_c], [B_L[0][j]])
                parts.append(p0)

                def p1():
                    for j in J:
                        h = hs[j]
                        stt(mL[b][j][:], P_L[0][:, j, :], Gc[:, h, n:n + 1], maskL[:], ALU.subtract, ALU.max, [B_L[0][j], B_g, B_c], [Bt(j)])
                        stt(mU[b][j][:], P_L[0][:, j, :], Gc[:, h, n:n + 1], maskU[:], ALU.subtract, ALU.min, [B_L[0][j], B_g, B_c], [Bt(j)])
                    for j in J:
                        act(eGb[b][j][:], P_L[0][:, j, :], AF.Exp, [B_L[0][j]], [Bt(j)])
                    for j in J:
                        act(mL[b][j][:], mL[b][j][:], AF.Exp, [Bt(j)], [Bt(j)], scale=-1.0)
                        act(mU[b][j][:], mU[b][j][:], AF.Exp, [Bt(j)], [Bt(j)])
                    for j in J:
                        tt("pool", qg[:, hs[j], c, :], qnc(j), eGb[b][j][:], ALU.mult, [Bq(j), Bt(j)], [Bk(j)])
                    for j in J:
                        mm(P_L[1][:, j, :], knc(j), knc(j), True, True, [Bq(j)], [B_L[1][j]])
                        mm(P_L[2][:, j, :], knc(j), qnc(j), True, True, [Bq(j)], [B_L[2][j]])
                parts.append(p1)

                def p2():
                    for j in J:
                        h = hs[j]
                        stt(Pn[0][b][j][:], P_L[1][:, j, :], nbeta[:, h, n:n + 1], mL[b][j][:], ALU.mult, ALU.mult, [B_L[1][j], B_g, Bt(j)], [Bt(j)])
                        tt("dve", qkT[:, h, c, :], P_L[2][:, j, :], mU[b][j][:], ALU.mult, [B_L[2][j], Bt(j)], [Bk(j)])
                    for j in J:
                        tr(P_T[:, j, :], Pn[0][b][j][:], ident_h[:], [Bt(j), B_ident], [B_PT[j]])
                    for j in J:
                        cp("act", Pt[0][b][j][:], P_T[:, j, :], [B_PT[j]], [Bt(j)])
                    for j in J:
                        tt("dve", XT[0][b][j][:], P_T[:, j, :], ident_f[:], ALU.add, [B_PT[j], B_ident], [Bt(j)])
                parts.append(p2)

                for k in range(1, 7):
                    def pl(k=k):
                        a = (k - 1) % 2; c_ = k % 2
                        for j in J:
                            mm(P_L[0][:, j, :], Pt[a][b][j][:], Pn[a][b][j][:], True, True, [Bt(j)], [B_L[0][j]])
                        if k < 6:
                            for j in J:
                                mm(P_L[1][:, j, :], Pn[a][b][j][:], Pt[a][b][j][:], True, True, [Bt(j)], [B_L[1][j]])
                        for j in J:
                            cp("act", Pn[c_][b][j][:], P_L[0][:, j, :], [B_L[0][j]], [Bt(j)])
                        if k < 6:
                            for j in J:
                                cp("dve", Pt[c_][b][j][:], P_L[1][:, j, :], [B_L[1][j]], [Bt(j)])
                        for j in J:
                            mm(P_L[2][:, j, :], Pn[c_][b][j][:], XT[a][b][j][:], True, True, [Bt(j)], [B_L[2][j]])
                        for j in J:
                            if k == 6:
                                tt("dve", TT[:, hs[j], c, :], P_L[2][:, j, :], XT[a][b][j][:], ALU.add, [B_L[2][j], Bt(j)], [B_TT[hs[j]][c]])
                            else:
                                tt("dve", XT[c_][b][j][:], P_L[2][:, j, :], XT[a][b][j][:], ALU.add, [B_L[2][j], Bt(j)], [Bt(j)])
                    parts.append(pl)
                return parts

            def sstep(tg, c, hs):
                par = tg % 2; n = tg * 4 + c
                cs = slice(c * 128, (c + 1) * 128)
                J = list(range(len(hs)))
                st_ = []

                def s0():
                    for j in J:
                        h = hs[j]
                        mm(P_Sa[:, j, :], knG[:, par, h, cs], S_h[:, h, :], True, True, [B_qkv[par][h], B_S[h]], [B_Sa[j]])
                    for j in J:
                        h = hs[j]
                        stt(rbuf[:, h, :], P_Sa[:, j, :], nbg[:, h, n:n + 1], vb[:, h, c, :], ALU.mult, ALU.add,
                            [B_Sa[j], B_g, B_ck[h][c]], [B_r[h]])
                st_.append(s0)

                def s1():
                    for j in J:
                        h = hs[j]
                        mm(P_Sa[:, j, :], TT[:, h, c, :], rbuf[:, h, :], True, True, [B_TT[h][c], B_r[h]], [B_Sa[j]])
                    for j in J:
                        h = hs[j]
                        cp("act", vnew[:, h, :], P_Sa[:, j, :], [B_Sa[j]], [B_vn[h]])
                st_.append(s1)

                def s2():
                    for j in J:
                        h = hs[j]
                        mm(P_Sb[:, j, :], S_h[:, h, :], qg[:, h, c, :], True, False, [B_S[h], B_ck[h][c]], [B_Sb[j]])
                        mm(P_Sb[:, j, :], vnew[:, h, :], qkT[:, h, c, :], False, True, [B_vn[h], B_ck[h][c]], [B_Sb[j]])
                    for j in J:
                        h = hs[j]
                        mm(P_Sa[:, j, :], kdec[:, h, c, :], vnew[:, h, :], True, True, [B_ck[h][c], B_vn[h]], [B_Sa[j]])
                    for j in J:
                        h = hs[j]
                        cp("act", oTs[:, h, cs], P_Sb[:, j, :], [B_Sb[j]], [B_oTs[h][c]])
                    for j in J:
                        h = hs[j]
                        ts("dve", S_f[:, h, :], S_f[:, h, :], eGl[:, h, n:n + 1], ALU.mult, [B_S[h], B_g], [B_S[h]])
                        tt("dve", S_f[:, h, :], P_Sa[:, j, :], S_f[:, h, :], ALU.add, [B_Sa[j], B_S[h]], [B_S[h]])
                    for j in J:
                        h = hs[j]
                        cp("act", S_h[:, h, :], S_f[:, h, :], [B_S[h]], [B_S[h]])
                st_.append(s2)
                return st_

            def pnorm(h, tg):
                t0 = tg * 512
                j = cnt["z"] % 2; cnt["z"] += 1
                Bo = [B_oTs[h][k] for k in range(4)]
                ks.dma("sp", zb[j][:], projT[24 + h][:, t0:t0 + 512], reads=[B_projT[24 + h]], writes=[B_zb[j]])
                q = cnt["sq"] % 2; cnt["sq"] += 1
                tt("pool", sqb[q][:], oTs[:, h, :], oTs[:, h, :], ALU.mult, Bo, [B_sqb[q]])
                mm(P_N[:], ones_h[:], sqb[q][:], True, True, [B_sqb[q], B_c], [B_PN])
                act(rsb[q][:], P_N[:], AF.Ln, [B_PN, B_c], [B_rsb[q]], bias=ce6, scale=1.0 / 128.0)
                act(rsb[q][:], rsb[q][:], AF.Exp, [B_rsb[q]], [B_rsb[q]], scale=-0.5)
                act(zs[j][:], zb[j][:], AF.Silu, [B_zb[j]], [B_zb[j]])
                tt("dve", on_[j][:], oTs[:, h, :], rsb[q][:], ALU.mult, Bo + [B_rsb[q]], [B_og[j]])
                stt(og_[j][:], on_[j][:], nw[:, 0:1], zs[j][:], ALU.mult, ALU.mult, [B_og[j], B_cw, B_zb[j]], [B_og[j]])
                ks.dma("sp", oT[h][:, t0:t0 + 512], og_[j][:], reads=[B_og[j]], writes=[B_oT[h][tg]])

            def merge(streams):
                tot = max(len(s_) for s_ in streams) if streams else 0
                idx = [0] * len(streams)
                for step in range(tot):
                    for si_, s_ in enumerate(streams):
                        tgt = ((step + 1) * len(s_) + tot - 1) // tot
                        while idx[si_] < tgt:
                            s_[idx[si_]](); idx[si_] += 1

            HW = [[0, 1, 2, 3], [4, 5, 6, 7]]
            for h in range(NH):
                gprep(h, 0)
            pending = []
            for tg in range(NG):
                for c in range(4):
                    streams = [wave(tg, c, HW[0]) + wave(tg, c, HW[1])]
                    if pending:
                        streams.append(pending)
                    merge(streams)
                    if pending and c == 0 and tg > 0:
                        for h in range(NH):
                            pnorm(h, tg - 1)
                    if tg + 1 < NG:
                        gprep(2 * c, tg + 1); gprep(2 * c + 1, tg + 1)
                    pending = sstep(tg, c, HW[0]) + sstep(tg, c, HW[1])
            merge([pending])
            for h in range(NH):
                pnorm(h, NG - 1)
            ks.barrier()
        if stop_after == 2:
            return _finish(nc, ks, [])

        f3d = dscr("f3d", [NH, S], F32); B_f3d = Buf()
        WROW = 24576
        use_moe = (stop_after is None) or (stop_after >= 5)
        B_wsc = Buf("wsc")
        if use_moe:
            w_moe = din("w_moe", [32, 128, WROW])
            wsc = dscr("wsc", [32 * 128, WROW], BF16)
        SCL = 128.0 ** -0.5
        with ExitStack() as ph:
            P_t = pst(ph, "fP_t", [128, 128]); B_PF = PB("fP_t")
            P_F = P_t[0:NT, :]
            f3t = sbt(ph, "f3t", [NT, NH, 128], F32); B_f3t = Buf()
            for h in range(NH):
                tr(P_F, Fc[:, h, :], ident_f[:], [B_g, B_ident], [B_PF])
                cp("dve", f3t[:, h, :], P_F, [B_PF], [B_f3t])
            ks.dma("sp", f3d.rearrange("h (tt p) -> tt h p", p=128), f3t[:], reads=[B_f3t], writes=[B_f3d])

            qT = [sbt(ph, "fqT%d" % i, [128, S], BF16) for i in range(2)]
            kT = [sbt(ph, "fkT%d" % i, [128, S], BF16) for i in range(2)]
            va = [sbt(ph, "fva%d" % i, [128, NT, 128], BF16) for i in range(2)]
            B_in = [Buf(), Buf()]
            Fb = [sbt(ph, "fFb%d" % i, [128, 512], F32) for i in range(2)]; B_Fb = [Buf(), Buf()]
            lgb = [sbt(ph, "flg%d" % i, [128, 512], F32) for i in range(3)]; B_lg = [Buf() for _ in range(3)]
            pT = [sbt(ph, "fpT%d" % i, [128, 512], BF16) for i in range(4)]; B_pT = [Buf() for _ in range(4)]
            rs_ = [sbt(ph, "frs%d" % i, [128, 512], F32) for i in range(2)]; B_rs_ = [Buf(), Buf()]
            oTg = [sbt(ph, "foTg%d" % i, [128, 512], BF16) for i in range(2)]; B_oTg = [Buf(), Buf()]
            P_s = [pst(ph, "fP_s%d" % i, [128, 512]) for i in range(3)]; B_Ps = [PB("fP_s%d" % i) for i in range(3)]
            P_o = [pst(ph, "fP_o%d" % i, [128, 512]) for i in range(2)]; B_Po = [PB("fP_o0"), PB("fP_o1")]
            P_m = [pst(ph, "fP_m%d" % i, [128, 512]) for i in range(2)]; B_Pm = [PB("fP_m0"), PB("fP_m1")]
            it = 0; gi = 0
            if use_moe:
                wst = [sbt(ph, "wst%d" % i, [128, WROW], BF16) for i in range(2)]; B_wst = [Buf(), Buf()]

            def precast(e_):
                i = e_ % 2
                wv = wst[i]
                ks.dma("pool", wv[:], w_moe[e_], writes=[B_wst[i]])
                ks.dma("sp", wsc[e_ * 128:(e_ + 1) * 128, :], wv[:], reads=[B_wst[i]], writes=[B_wsc])

            for h in range(NH):
                hp = h % 2
                ks.dma("sp", qT[hp][:], projT[32 + h], reads=[B_projT[32 + h]], writes=[B_in[hp]])
                ks.dma("sp", kT[hp][:], projT[40 + h], reads=[B_projT[40 + h]], writes=[B_in[hp]])
                ks.dma("sp", va[hp][:], vtok[:, h * 128:(h + 1) * 128].rearrange("(tt p) c -> p tt c", p=128),
                       reads=[B_vtok], writes=[B_in[hp]])
                for qgi in range(NG):
                    q0 = qgi * 512
                    g_ = gi % 2; gi += 1
                    if use_moe and (h * NG + qgi) % 2 == 0:
                        precast((h * NG + qgi) // 2)
                    ks.dma("sp", Fb[g_][:], f3d[h:h + 1, q0:q0 + 512].broadcast_to([128, 512]), reads=[B_f3d], writes=[B_Fb[g_]])
                    nj = 4 * qgi + 4

                    def front(j):
                        nonlocal it
                        bl = max(0, j - 4 * qgi)
                        c_lo = bl * 128
                        ps_ = P_s[it % 3]; Bps = B_Ps[it % 3]
                        lg_ = lgb[it % 3]; Blg = B_lg[it % 3]
                        pt_ = pT[it % 4]; Bpt = B_pT[it % 4]
                        it += 1
                        mm(ps_[:, c_lo:512], kT[hp][:, j * 128:(j + 1) * 128], qT[hp][:, q0 + c_lo:q0 + 512], True, True,
                           [B_in[hp]], [Bps])
                        stt(lg_[:, c_lo:512], ps_[:, c_lo:512], SCL, Fb[g_][:, c_lo:512], ALU.mult, ALU.add, [Bps, B_Fb[g_]], [Blg])
                        act(pt_[:, c_lo:512], lg_[:, c_lo:512], AF.Exp, [Blg, B_g], [Bpt], bias=nF[:, h, j:j + 1], scale=1.0)
                        if j >= 4 * qgi:
                            asel(pt_[:, c_lo:c_lo + 128], pt_[:, c_lo:c_lo + 128], "le", 0.0, [Bpt], [Bpt])
                        return (pt_, Bpt, c_lo)

                    fq = [front(0)]
                    if nj > 1:
                        fq.append(front(1))
                    for j in range(nj):
                        cur = fq.pop(0)
                        if j + 2 < nj:
                            fq.append(front(j + 2))
                        pt_, Bpt, c_lo = cur
                        mm(P_o[g_][:, c_lo:512], va[hp][:, j, :], pt_[:, c_lo:512], j == 0, j == nj - 1, [Bpt, B_in[hp]], [B_Po[g_]])
                        mm(P_m[g_][:, c_lo:512], ones_h[:], pt_[:, c_lo:512], j == 0, j == nj - 1, [Bpt, B_c], [B_Pm[g_]])
                    cp("dve", rs_[g_][:], P_m[g_][:], [B_Pm[g_]], [B_rs_[g_]])
                    ks.op("dve", lambda e, g_=g_: e.reciprocal(rs_[g_][:], rs_[g_][:]), reads=[B_rs_[g_]], writes=[B_rs_[g_]])
                    tt("dve", oTg[g_][:], P_o[g_][:], rs_[g_][:], ALU.mult, [B_Po[g_], B_rs_[g_]], [B_oTg[g_]])
                    ks.dma("sp", oT[8 + h][:, q0:q0 + 512], oTg[g_][:], reads=[B_oTg[g_]], writes=[B_oT[8 + h][qgi]])
            ks.barrier()
        gst_.close()
        if stop_after == 3:
            return _finish(nc, ks, [])

        NBLK = 64
        BLK = 256
        x1d = dscr("x1d", [S, D], F32); B_x1d = [Buf() for _ in range(NT)]
        h2d = dscr("h2d", [S, D], BF16); B_h2d = [Buf() for _ in range(NT)]
        xsd = dscr("xsd", [NBLK * BLK, D], BF16); B_xsd = Buf()
        ybd = dscr("ybd", [NBLK * BLK, D], BF16); B_ybd = Buf()
        mwd = dscr("mwd", [S, 32], F32); B_mwd = Buf()
        rst = ExitStack(); st.enter_context(rst)
        d01 = sbt(rst, "d01", [128, 2, NT], I32)
        w12 = sbt(rst, "w12", [128, 2, NT], F32)
        widx = sbt(rst, "widx", [128, NBLK], I32)
        B_rs = Buf("routing")

        def bcast_load(stack, name, src_row, B, plus1=False):
            t_ = sbt(stack, name, [128, D], F32)
            ks.dma("sp", t_[:], src_row.broadcast_to([128, D]), reads=[B_modrow], writes=[B])
            if plus1:
                ts("pool", t_[:], t_[:], 1.0, ALU.add, [B], [B])
            return t_

        with ExitStack() as ph:
            B_bc = Buf()
            g1p = bcast_load(ph, "g1p", modrow[0:1, 2 * D:3 * D], B_bc, True)
            sh2b = bcast_load(ph, "sh2b", modrow[0:1, 3 * D:4 * D], B_bc)
            sc2p = bcast_load(ph, "sc2p", modrow[0:1, 4 * D:5 * D], B_bc, True)
            l1g = bcast_load(ph, "l1g", ln1_g[0:1, :], B_bc)
            l1b = bcast_load(ph, "l1b", ln1_b[0:1, :], B_bc)
            wo = sbt(ph, "wo", [128, KC, D], BF16); B_wo = Buf()
            w_out_v = w_out.rearrange("(kc p) n -> p kc n", p=128)
            for kc in range(KC):
                ks.dma("pool", wo[:, kc, :], w_out_v[:, kc, :], writes=[B_wo])
            wr = sbt(ph, "wr", [128, KC, 36], F32); brb = sbt(ph, "brb", [128, 36], F32); B_wr = Buf()
            wrh = sbt(ph, "wrh", [128, KC, 36], BF16)
            ks.dma("sp", wr[:], w_r.rearrange("(kc p) n -> p kc n", p=128), writes=[B_wr])
            ks.dma("sp", brb[:], b_r[0:1, :].broadcast_to([128, 36]), writes=[B_wr])
            cp("dve", wrh[:], wr[:], [B_wr], [B_wr])
            ogb = [sbt(ph, "ogb%d" % i, [128, KC, 512], BF16) for i in range(2)]; B_ogb = [Buf(), Buf()]
            xt_ = sbt(ph, "xt0", [128, D], F32); B_xt = Buf()
            vv = sbt(ph, "vv0", [128, D], F32); B_vv = Buf()
            h2 = sbt(ph, "h2_0", [128, D], F32); B_h2 = Buf()
            h2h = [sbt(ph, "h2h%d" % i, [128, D], BF16) for i in range(2)]; B_h2h = [Buf(), Buf()]
            h2Tf = sbt(ph, "h2Tf", [128, KC, 128], BF16); B_h2Tf = Buf()
            st4 = sbt(ph, "st4", [128, 16], F32); B_st = Buf()
            rt = sbt(ph, "rt", [128, 128], F32); B_rt = Buf()
            OH = sbt(ph, "OH", [128, 2, NT, 32], F32); B_OH = Buf()
            junk = sbt(ph, "junk", [128, D], BF16); B_junk = Buf()
            P_y = [pst(ph, "P_y%d" % i, [128, 512]) for i in range(4)]; B_Py = [PB("P_y%d" % i) for i in range(4)]
            P_h = [pst(ph, "P_h%d" % i, [128, 8, 128], BF16) for i in range(2)]; B_Ph = [PB("P_h0"), PB("P_h1")]
            P_r = pst(ph, "P_r", [128, 36]); B_Pr = PB("P_r")
            oT_v = oT.rearrange("h p t -> p h t")
            py = 0; ph_i = 0
            for tg in range(NG):
                gb = tg % 2
                ks.dma("sp", ogb[gb][:], oT_v[:, :, tg * 512:(tg + 1) * 512],
                       reads=[B_oT[h][tg] for h in range(16)], writes=[B_ogb[gb]])
                for ti in range(4):
                    tI = tg * 4 + ti
                    hb_ = tI % 2
                    ks.dma("sp", xt_[:], xtok[tI * 128:(tI + 1) * 128, :], writes=[B_xt])
                    for cg in range(4):
                        p_ = py % 4; py += 1
                        for hh in range(KC):
                            mm(P_y[p_][:], ogb[gb][:, hh, ti * 128:(ti + 1) * 128], wo[:, hh, cg * 512:(cg + 1) * 512],
                               hh == 0, hh == KC - 1, [B_ogb[gb], B_wo], [B_Py[p_]])
                        tt("dve", vv[:, cg * 512:(cg + 1) * 512], P_y[p_][:], g1p[:, cg * 512:(cg + 1) * 512], ALU.mult,
                           [B_Py[p_], B_bc], [B_vv])
                    stt(vv[:], xt_[:], ALPHA, vv[:], ALU.mult, ALU.add, [B_xt, B_vv], [B_vv])
                    ks.op("dve", lambda e: e.reduce_sum(out=st4[:, 0:1], in_=vv[:], axis=AX.X), reads=[B_vv], writes=[B_st])
                    act(junk[:], vv[:], AF.Square, [B_vv], [B_junk, B_st], accum_out=st4[:, 1:2])
                    ts("dve", st4[:, 2:3], st4[:, 0:1], 1.0 / D, ALU.mult, [B_st], [B_st])
                    tt("dve", st4[:, 3:4], st4[:, 2:3], st4[:, 2:3], ALU.mult, [B_st], [B_st])
                    stt(st4[:, 4:5], st4[:, 1:2], 1.0 / D, st4[:, 3:4], ALU.mult, ALU.subtract, [B_st], [B_st])
                    act(st4[:, 5:6], st4[:, 4:5], AF.Sqrt, [B_st, B_c], [B_st], bias=ce5, scale=1.0)
                    ks.op("dve", lambda e: e.reciprocal(st4[:, 6:7], st4[:, 5:6]), reads=[B_st], writes=[B_st])
                    ts("dve", vv[:], vv[:], st4[:, 2:3], ALU.subtract, [B_vv, B_st], [B_vv], s2=st4[:, 6:7], op1=ALU.mult)
                    tt("dve", vv[:], vv[:], l1g[:], ALU.mult, [B_vv, B_bc], [B_vv])
                    tt("dve", vv[:], vv[:], l1b[:], ALU.add, [B_vv, B_bc], [B_vv])
                    ks.dma("sp", x1d[tI * 128:(tI + 1) * 128, :], vv[:], reads=[B_vv], writes=[B_x1d[tI]])
                    tt("dve", h2[:], vv[:], sc2p[:], ALU.mult, [B_vv, B_bc], [B_h2])
                    tt("dve", h2h[hb_][:], h2[:], sh2b[:], ALU.add, [B_h2, B_bc], [B_h2h[hb_]])
                    ks.dma("sp", h2d[tI * 128:(tI + 1) * 128, :], h2h[hb_][:], reads=[B_h2h[hb_]], writes=[B_h2d[tI]])
                    for k8 in range(2):
                        q_ = ph_i % 2; ph_i += 1
                        for kk in range(8):
                            kc = k8 * 8 + kk
                            tr(P_h[q_][:, kk, :], h2h[hb_][:, kc * 128:(kc + 1) * 128], ident_h[:], [B_h2h[hb_], B_ident], [B_Ph[q_]])
                        cp("act", h2Tf[:, k8 * 8:(k8 + 1) * 8, :], P_h[q_][:, :, :], [B_Ph[q_]], [B_h2Tf])
                    for kc in range(KC):
                        mm(P_r[:, :], h2Tf[:, kc, :], wrh[:, kc, :], kc == 0, kc == KC - 1, [B_h2Tf, B_wr], [B_Pr])
                    R_ = [B_rt, B_c]
                    lg = rt[:, 0:36]
                    tt("dve", lg, P_r[:, :], brb[:], ALU.add, [B_Pr, B_wr], [B_rt])
                    gmx = rt[:, 36:37]; ohg = rt[:, 40:44]; gsum = rt[:, 37:38]; gw = rt[:, 38:39]
                    ks.op("dve", lambda e: e.reduce_max(out=gmx, in_=rt[:, 0:4], axis=AX.X), reads=R_, writes=[B_rt])
                    ts("dve", ohg, rt[:, 0:4], gmx, ALU.is_equal, R_, [B_rt])
                    ts("dve", rt[:, 44:48], rt[:, 0:4], gmx, ALU.subtract, R_, [B_rt])
                    act(rt[:, 44:48], rt[:, 44:48], AF.Exp, R_, [B_rt])
                    ks.op("dve", lambda e: e.reduce_sum(out=gsum, in_=rt[:, 44:48], axis=AX.X), reads=R_, writes=[B_rt])
                    ks.op("dve", lambda e: e.reciprocal(gw, gsum), reads=R_, writes=[B_rt])
                    es = rt[:, 48:56]
                    ts("dve", es, rt[:, 4:12], ohg[:, 0:1], ALU.mult, R_, [B_rt])
                    for g_ in range(1, 4):
                        stt(es, rt[:, 4 + 8 * g_:12 + 8 * g_], ohg[:, g_:g_ + 1], es, ALU.mult, ALU.add, R_, [B_rt])
                    m1 = rt[:, 56:57]; m2 = rt[:, 57:58]; oh1 = rt[:, 64:72]; oh2 = rt[:, 72:80]; es2 = rt[:, 80:88]
                    ks.op("dve", lambda e: e.reduce_max(out=m1, in_=es, axis=AX.X), reads=R_, writes=[B_rt])
                    ts("dve", oh1, es, m1, ALU.is_equal, R_, [B_rt])
                    stt(es2, oh1, -1.0e30, es, ALU.mult, ALU.add, R_, [B_rt])
                    ks.op("dve", lambda e: e.reduce_max(out=m2, in_=es2, axis=AX.X), reads=R_, writes=[B_rt])
                    ts("dve", oh2, es2, m2, ALU.is_equal, R_, [B_rt])
                    w1 = rt[:, 58:59]; w2 = rt[:, 59:60]
                    tt("dve", w1, m2, m1, ALU.subtract, R_, [B_rt])
                    act(w1, w1, AF.Exp, R_, [B_rt])
                    ts("dve", w1, w1, 1.0, ALU.add, R_, [B_rt])
                    ks.op("dve", lambda e: e.reciprocal(w1, w1), reads=R_, writes=[B_rt])
                    ts("dve", w2, w1, -1.0, ALU.mult, R_, [B_rt], s2=1.0, op1=ALU.add)
                    tt("dve", w12[:, 0, tI:tI + 1], w1, gw, ALU.mult, R_, [B_rt, B_rs])
                    tt("dve", w12[:, 1, tI:tI + 1], w2, gw, ALU.mult, R_, [B_rt, B_rs])
                    for g_ in range(4):
                        ts("dve", OH[:, 0, tI, g_ * 8:(g_ + 1) * 8], oh1, ohg[:, g_:g_ + 1], ALU.mult, R_, [B_rt, B_OH])
                        ts("dve", OH[:, 1, tI, g_ * 8:(g_ + 1) * 8], oh2, ohg[:, g_:g_ + 1], ALU.mult, R_, [B_rt, B_OH])
            if "mwd" in dbg:
                mwt_ = sbt(ph, "mwt_", [128, NT, 32], F32)
                for tI in range(NT):
                    ts("dve", mwt_[:, tI, :], OH[:, 0, tI, :], w12[:, 0, tI:tI + 1], ALU.mult, [B_OH, B_rs], [B_mwd])
                    stt(mwt_[:, tI, :], OH[:, 1, tI, :], w12[:, 1, tI:tI + 1], mwt_[:, tI, :], ALU.mult, ALU.add, [B_OH, B_rs, B_mwd], [B_mwd])
                ks.dma("sp", mwd.rearrange("(tt p) c -> p tt c", p=128), mwt_[:], reads=[B_mwd], writes=[B_mwd])
            NE = 32
            Cc = sbt(ph, "Cc", [128, NT * NE], F32); B_s = Buf("sort")
            rk = sbt(ph, "rk", [128, NT * NE], F32)
            pf = sbt(ph, "pf", [128, NT * NE], F32)
            sm = sbt(ph, "sm", [128, 8, 64], F32)
            UTs = sbt(ph, "UTs", [128, 128], F32)
            ks.op("pool", lambda e: e.memset(UTs[:], 1.0), writes=[B_s])
            ks.op("pool", lambda e: e.affine_select(out=UTs[:], in_=UTs[:], pattern=[[1, 128]], compare_op=ALU.is_ge,
                                                    fill=0.0, base=-1, channel_multiplier=-1), reads=[B_s], writes=[B_s])
            OHf = lambda k: OH[:, k, :, :].rearrange("p t e -> p (t e)")
            tt("dve", Cc[:], OHf(0), OHf(1), ALU.add, [B_OH], [B_s])
            for half in range(2):
                sl = slice(half * 512, (half + 1) * 512)
                mm(P_y[0][:], UTs[:], Cc[:, sl], True, True, [B_s], [B_Py[0]])
                mm(P_y[1][:], ones_f[:], Cc[:, sl], True, True, [B_s, B_c], [B_Py[1]])
                cp("dve", rk[:, sl], P_y[0][:], [B_Py[0]], [B_s])
                cp("dve", pf[:, sl], P_y[1][:], [B_Py[1]], [B_s])
            pf3 = pf[:, :].rearrange("p (t e) -> p t e", e=NE)
            rk3 = rk[:, :].rearrange("p (t e) -> p t e", e=NE)
            tot = sm[:, 0, 0:NE]; run = sm[:, 1, 0:NE]
            ks.op("dve", lambda e: e.memset(run, 0.0), writes=[B_s])
            for tI in range(NT):
                tt("dve", rk3[:, tI, :], rk3[:, tI, :], run, ALU.add, [B_s], [B_s])
                tt("dve", run, run, pf3[:, tI, :], ALU.add, [B_s], [B_s])
            cp("dve", tot, run, [B_s], [B_s])
            thr = sm[:, 2, 0:32]; nbk = sm[:, 3, 0:NE]; tmp32 = sm[:, 4, 0:32]
            ks.op("pool", lambda e: e.iota(thr, pattern=[[BLK, 32]], base=0, channel_multiplier=0,
                                           allow_small_or_imprecise_dtypes=True), writes=[B_s])
            for e_ in range(NE):
                ts("dve", tmp32, thr, tot[:, e_:e_ + 1], ALU.is_lt, [B_s], [B_s])
                ks.op("dve", lambda e, e_=e_: e.reduce_sum(out=nbk[:, e_:e_ + 1], in_=tmp32, axis=AX.X), reads=[B_s], writes=[B_s])
            pend = sm[:, 5, 0:NE]; pstart = sm[:, 6, 0:NE]
            ks.op("dve", lambda e: e.tensor_tensor_scan(out=pend, data0=ones_f[:, 0:NE], data1=nbk, initial=0.0,
                                                        op0=ALU.mult, op1=ALU.add), reads=[B_s, B_c], writes=[B_s])
            tt("dve", pstart, pend, nbk, ALU.subtract, [B_s], [B_s])
            ts("dve", pstart, pstart, float(BLK), ALU.mult, [B_s], [B_s])
            ts("dve", pend, pend, float(BLK), ALU.mult, [B_s], [B_s])
            for tI in range(NT):
                tt("dve", rk3[:, tI, :], rk3[:, tI, :], pstart, ALU.add, [B_s], [B_s])
            dstf = sm[:, 7, :]
            for k in range(2):
                tt("dve", Cc[:], OHf(k), rk[:], ALU.mult, [B_OH, B_s], [B_s])
                ks.op("dve", lambda e, k=k: e.tensor_reduce(out=dstf[:, k * NT:(k + 1) * NT],
                                                            in_=Cc[:, :].rearrange("p (t e) -> p t e", e=NE),
                                                            axis=AX.X, op=ALU.add), reads=[B_s], writes=[B_s])
            cp("dve", d01[:, :, :].rearrange("p k t -> p (k t)"), dstf, [B_s], [B_rs])
            bthr = sm[:, 2, 0:NBLK]; bacc = sm[:, 3, 0:NBLK]; btmp = sm[:, 4, 0:NBLK]
            ks.op("pool", lambda e: e.iota(bthr, pattern=[[BLK, NBLK]], base=0, channel_multiplier=0,
                                           allow_small_or_imprecise_dtypes=True), reads=[B_s], writes=[B_s])
            ks.op("dve", lambda e: e.memset(bacc, 0.0), reads=[B_s], writes=[B_s])
            for e_ in range(NE):
                ts("dve", btmp, bthr, pend[:, e_:e_ + 1], ALU.is_ge, [B_s], [B_s])
                tt("dve", bacc, bacc, btmp, ALU.add, [B_s], [B_s])
            ts("dve", bacc, bacc, float(NE - 1), ALU.min, [B_s], [B_s], s2=128.0, op1=ALU.mult)
            pidx = sm[:, 0, 32:33]
            ks.op("pool", lambda e: e.iota(pidx, pattern=[[0, 1]], base=0, channel_multiplier=1,
                                           allow_small_or_imprecise_dtypes=True), reads=[B_s], writes=[B_s])
            ts("dve", bacc, bacc, pidx, ALU.add, [B_s], [B_s])
            cp("dve", widx[:], bacc, [B_s], [B_rs])
            for tI in range(NT):
                hb_ = tI % 2
                ks.dma("sp", h2h[hb_][:], h2d[tI * 128:(tI + 1) * 128, :], reads=[B_h2d[tI]], writes=[B_h2h[hb_]])
                for k in range(2):
                    ks.dma("pool", None, None, reads=[B_h2h[hb_], B_rs], writes=[B_xsd],
                           fn=lambda e, k=k, tI=tI, hb_=hb_: e.indirect_dma_start(
                               out=xsd[:, :], out_offset=bass.IndirectOffsetOnAxis(ap=d01[:, k, tI:tI + 1], axis=0),
                               in_=h2h[hb_][:], in_offset=None))
            ks.barrier()
        if stop_after == 4:
            return _finish(nc, ks, [])

        with ExitStack() as ph:
            wblk = [sbt(ph, "wblk%d" % i, [128, WROW], BF16) for i in range(2)]; B_wb = [Buf(), Buf()]
            xsb = [sbt(ph, "xsb%d" % i, [128, 2, D], BF16) for i in range(2)]; B_xsb = [Buf(), Buf()]
            xsT = [sbt(ph, "xsT%d" % i, [128, KC, BLK], BF16) for i in range(2)]; B_xsT = [Buf(), Buf()]
            sg = [sbt(ph, "sg%d" % i, [128, BLK], F32) for i in range(2)]; B_sg = [Buf(), Buf()]
            hid = [sbt(ph, "hid%d" % i, [128, 4, BLK], BF16) for i in range(2)]; B_hid = [Buf(), Buf()]
            yo = [sbt(ph, "yo%d" % i, [128, D], BF16) for i in range(2)]; B_yo = [Buf(), Buf()]
            P_x = [pst(ph, "P_x%d" % i, [128, 8, 128], BF16) for i in range(2)]; B_Px = [PB("P_x0"), PB("P_x1")]
            P_g = [pst(ph, "P_g%d" % i, [128, BLK]) for i in range(2)]; B_Pg = [PB("P_g0"), PB("P_g1")]
            P_u = [pst(ph, "P_u%d" % i, [128, BLK]) for i in range(2)]; B_Pu = [PB("P_u0"), PB("P_u1")]
            P_d = [pst(ph, "P_d%d" % i, [128, 512]) for i in range(2)]; B_Pd = [PB("P_d0"), PB("P_d1")]
            px = 0; pq = 0; pd = 0; sgi = 0; yi = 0
            for b in range(NBLK):
                wb = b % 2
                wv = wblk[wb]
                ks.dma("pool", None, None, reads=[B_rs, B_wsc], writes=[B_wb[wb]],
                       fn=lambda e, b=b, wv=wv: e.indirect_dma_start(
                           out=wv[:], out_offset=None, in_=wsc[:, :],
                           in_offset=bass.IndirectOffsetOnAxis(ap=widx[:, b:b + 1], axis=0)))
                ks.dma("sp", xsb[wb][:], xsd[b * BLK:(b + 1) * BLK, :].rearrange("(t p) d -> p t d", p=128),
                       reads=[B_xsd], writes=[B_xsb[wb]])
                for t in range(2):
                    for k8 in range(2):
                        q_ = px % 2; px += 1
                        for kk in range(8):
                            kc = k8 * 8 + kk
                            tr(P_x[q_][:, kk, :], xsb[wb][:, t, kc * 128:(kc + 1) * 128], ident_h[:], [B_xsb[wb], B_ident], [B_Px[q_]])
                        if (px % 2) == 0:
                            cp("act", xsT[wb][:, k8 * 8:(k8 + 1) * 8, t * 128:(t + 1) * 128], P_x[q_][:, :, :], [B_Px[q_]], [B_xsT[wb]])
                        else:
                            cp("dve", xsT[wb][:, k8 * 8:(k8 + 1) * 8, t * 128:(t + 1) * 128], P_x[q_][:, :, :], [B_Px[q_]], [B_xsT[wb]])
                wgv = wv[:, 0:8192].rearrange("p (kc n) -> p kc n", n=512)
                wuv = wv[:, 8192:16384].rearrange("p (kc n) -> p kc n", n=512)
                wdv = wv[:, 16384:24576].rearrange("p (hc n) -> p hc n", n=D)
                hb = b % 2
                for hc in range(4):
                    q_ = pq % 2; pq += 1
                    for kc in range(KC):
                        mm(P_g[q_][:], wgv[:, kc, hc * 128:(hc + 1) * 128], xsT[wb][:, kc, :], kc == 0, kc == KC - 1,
                           [B_wb[wb], B_xsT[wb]], [B_Pg[q_]])
                    for kc in range(KC):
                        mm(P_u[q_][:], wuv[:, kc, hc * 128:(hc + 1) * 128], xsT[wb][:, kc, :], kc == 0, kc == KC - 1,
                           [B_wb[wb], B_xsT[wb]], [B_Pu[q_]])
                    s_ = sgi % 2; sgi += 1
                    act(sg[s_][:], P_g[q_][:], AF.Silu, [B_Pg[q_]], [B_sg[s_]])
                    tt("dve", hid[hb][:, hc, :], P_u[q_][:], sg[s_][:], ALU.mult, [B_sg[s_], B_Pu[q_]], [B_hid[hb]])
                for t in range(2):
                    y_ = yi % 2; yi += 1
                    for cg in range(4):
                        p_ = pd % 2; pd += 1
                        for hc in range(4):
                            mm(P_d[p_][:], hid[hb][:, hc, t * 128:(t + 1) * 128], wdv[:, hc, cg * 512:(cg + 1) * 512],
                               hc == 0, hc == 3, [B_hid[hb], B_wb[wb]], [B_Pd[p_]])
                        if cg % 2 == 0:
                            cp("act", yo[y_][:, cg * 512:(cg + 1) * 512], P_d[p_][:], [B_Pd[p_]], [B_yo[y_]])
                        else:
                            cp("dve", yo[y_][:, cg * 512:(cg + 1) * 512], P_d[p_][:], [B_Pd[p_]], [B_yo[y_]])
                    r0 = b * BLK + t * 128
                    ks.dma("sp", ybd[r0:r0 + 128, :], yo[y_][:], reads=[B_yo[y_]], writes=[B_ybd])
            ks.barrier()
        if stop_after == 5:
            return _finish(nc, ks, [])
        with ExitStack() as ph:
            B_bc = Buf()
            g2p = bcast_load(ph, "g2p", modrow[0:1, 5 * D:6 * D], B_bc, True)
            l2g = bcast_load(ph, "l2g", ln2_g[0:1, :], B_bc)
            l2b = bcast_load(ph, "l2b", ln2_b[0:1, :], B_bc)
            rr = [[sbt(ph, "rr%d_%d" % (i, k), [128, D], BF16) for k in range(2)] for i in range(2)]
            B_rr = [Buf(), Buf()]
            ya_ = [sbt(ph, "ya%d" % i, [128, D], F32) for i in range(2)]; B_ya = [Buf(), Buf()]
            x1t = [sbt(ph, "x1t%d" % i, [128, D], F32) for i in range(2)]; B_x1t = [Buf(), Buf()]
            st5 = sbt(ph, "st5", [128, 16], F32); B_st5 = Buf()
            junk5 = sbt(ph, "junk5", [128, D], BF16); B_j5 = Buf()
            for tI in range(NT):
                i = tI % 2
                for k in range(2):
                    ks.dma("pool", None, None, reads=[B_ybd, B_rs], writes=[B_rr[i]],
                           fn=lambda e, k=k, tI=tI, i=i: e.indirect_dma_start(
                               out=rr[i][k][:], out_offset=None, in_=ybd[:, :],
                               in_offset=bass.IndirectOffsetOnAxis(ap=d01[:, k, tI:tI + 1], axis=0)))
                ks.dma("sp", x1t[i][:], x1d[tI * 128:(tI + 1) * 128, :], reads=[B_x1d[tI]], writes=[B_x1t[i]])
                ya = ya_[i][:]; By = B_ya[i]
                ts("dve", ya, rr[i][0][:], w12[:, 0, tI:tI + 1], ALU.mult, [B_rr[i], B_rs], [By])
                stt(ya, rr[i][1][:], w12[:, 1, tI:tI + 1], ya, ALU.mult, ALU.add, [B_rr[i], B_rs, By], [By])
                tt("dve", ya, ya, g2p[:], ALU.mult, [By, B_bc], [By])
                stt(ya, x1t[i][:], ALPHA, ya, ALU.mult, ALU.add, [B_x1t[i], By], [By])
                ks.op("dve", lambda e, ya=ya: e.reduce_sum(out=st5[:, 0:1], in_=ya, axis=AX.X), reads=[By], writes=[B_st5])
                act(junk5[:], ya, AF.Square, [By], [B_j5, B_st5], accum_out=st5[:, 1:2])
                ts("dve", st5[:, 2:3], st5[:, 0:1], 1.0 / D, ALU.mult, [B_st5], [B_st5])
                tt("dve", st5[:, 3:4], st5[:, 2:3], st5[:, 2:3], ALU.mult, [B_st5], [B_st5])
                stt(st5[:, 4:5], st5[:, 1:2], 1.0 / D, st5[:, 3:4], ALU.mult, ALU.subtract, [B_st5], [B_st5])
                act(st5[:, 5:6], st5[:, 4:5], AF.Sqrt, [B_st5, B_c], [B_st5], bias=ce5, scale=1.0)
                ks.op("dve", lambda e: e.reciprocal(st5[:, 6:7], st5[:, 5:6]), reads=[B_st5], writes=[B_st5])
                ts("dve", ya, ya, st5[:, 2:3], ALU.subtract, [By, B_st5], [By], s2=st5[:, 6:7], op1=ALU.mult)
                tt("dve", ya, ya, l2g[:], ALU.mult, [By, B_bc], [By])
                tt("dve", ya, ya, l2b[:], ALU.add, [By, B_bc], [By])
                ks.dma("sp", out[tI * 128:(tI + 1) * 128, :], ya, reads=[By], writes=[Buf()])
            ks.barrier()

        return _finish(nc, ks, [])


def _finish(nc, ks, bufs):
    for key, val in ks.cnt.items():
        if key.startswith("d_") and val > 0:
            ks._wait("sp", (key, val))
    print("kernel build: insts=%d waits=%d" % (ks.n_inst, ks.n_wait))
    return nc


def _col_perm():
    idx = list(range(0, 4096)) + list(range(4112, 4112 + 3072)) + list(range(4096, 4112)) + list(range(7184, 7192))
    return np.asarray(idx)


def make_in_maps(inputs, n_cores=8):
    f = lambda a: np.ascontiguousarray(np.asarray(a, dtype=np.float32))
    x = np.asarray(inputs["x"]); c = np.asarray(inputs["c"])
    shared = {
        "w_ada": f(inputs["w_ada"][0]),
        "b_ada": f(inputs["b_ada"][0][None, :]),
        "w_in": f(inputs["w_in"][0][:, _col_perm()]),
        "convw": f(np.asarray(inputs["dn_conv_w"][0]).reshape(4, 24, 128).transpose(2, 1, 0)),
        "a_log": f(inputs["dn_a_log"][0][None, :]),
        "dt_bias": f(inputs["dn_dt_bias"][0][None, :]),
        "f_bias": f(inputs["fox_f_bias"][0][None, :]),
        "norm_w": f(np.asarray(inputs["dn_norm_w"][0])[:, None]),
        "w_out": f(inputs["w_out"][0]),
        "ln1_g": f(inputs["ln1_g"][0][None, :]), "ln1_b": f(inputs["ln1_b"][0][None, :]),
        "ln2_g": f(inputs["ln2_g"][0][None, :]), "ln2_b": f(inputs["ln2_b"][0][None, :]),
        "w_r": f(np.concatenate([np.asarray(inputs["w_router_group"][0])] +
                                [np.asarray(inputs["w_router_expert"][0][g]) for g in range(4)], axis=1)),
        "b_r": f(np.concatenate([np.asarray(inputs["b_router_group"][0])] +
                                [np.asarray(inputs["b_router_expert"][0][g]) for g in range(4)])[None, :]),
    }
    w_gate = np.asarray(inputs["w_gate"][0], dtype=np.float32).reshape(32, KC, 128, 512).transpose(0, 2, 1, 3).reshape(32, 128, KC * 512)
    w_up = np.asarray(inputs["w_up"][0], dtype=np.float32).reshape(32, KC, 128, 512).transpose(0, 2, 1, 3).reshape(32, 128, KC * 512)
    w_down = np.asarray(inputs["w_down"][0], dtype=np.float32).reshape(32, 4, 128, D).transpose(0, 2, 1, 3).reshape(32, 128, 4 * D)
    shared["w_moe"] = np.ascontiguousarray(np.concatenate([w_gate, w_up, w_down], axis=2))
    maps = []
    for b in range(n_cores):
        m = dict(shared)
        m["x"] = f(x[b])
        m["xT"] = f(x[b].T)
        m["ccol"] = f(c[b].reshape(KC, 128).T)
        maps.append(m)
    return maps


def kernel(**inputs):
    nc = build_nc()
    maps = make_in_maps(inputs)
    res = run_bass_kernel_spmd(nc, maps, core_ids=list(range(8)))
    return np.stack([np.asarray(r["out"], dtype=np.float32) for r in res.results], axis=0)
```

```python
import os
import numpy as np
from contextlib import ExitStack
import concourse.bass as bass
import concourse.mybir as mybir
from concourse.bass_utils import run_bass_kernel_spmd

F32 = mybir.dt.float32
BF16 = mybir.dt.bfloat16
I32 = mybir.dt.int32
AF = mybir.ActivationFunctionType
ALU = mybir.AluOpType
AX = mybir.AxisListType

D = 2048
S = 4096
KC = D // 128
NT = S // 128
NG = S // 512
NH = 8
DIN = 7192
ALPHA = 2.0 ** 0.25


class Bank:
    __slots__ = ("acc",)

    def __init__(self):
        self.acc = {}


class Buf:
    __slots__ = ("name", "w", "r", "bank")

    def __init__(self, name="", bank=None):
        self.name = name
        self.w = None
        self.r = {}
        self.bank = bank


class KS:
    ENG = ("pe", "act", "dve", "pool", "sp")

    def __init__(self, nc, stack, n_dsem=12, same_eng_sync=True):
        self.nc = nc
        self.eng = {"pe": nc.tensor, "act": nc.scalar, "dve": nc.vector,
                    "pool": nc.gpsimd, "sp": nc.sync}
        self.same_eng_sync = same_eng_sync
        self.sems = {}
        self.cnt = {}
        for e in self.ENG:
            self.sems[e] = stack.enter_context(nc.semaphore("c_" + e))
            self.cnt[e] = 0
        self.dq = {}
        for q in ("sp", "pool", "act"):
            lst = []
            for i in range(n_dsem):
                key = "d_%s_%d" % (q, i)
                self.sems[key] = stack.enter_context(nc.semaphore(key))
                self.cnt[key] = 0
                lst.append(key)
            self.dq[q] = [lst, 0]
        self.waited = {e: {} for e in self.ENG}
        self.n_wait = 0
        self.n_inst = 0

    def _wait(self, e, dep):
        if dep is None:
            return
        key, val = dep
        if key == e and (e == "pe" or not self.same_eng_sync):
            return
        if self.waited[e].get(key, 0) >= val:
            return
        self.eng[e].wait_ge(self.sems[key], val)
        self.waited[e][key] = val
        self.n_wait += 1

    def _deps(self, e, reads, writes):
        for b in reads:
            self._wait(e, b.w)
        for b in writes:
            self._wait(e, b.w)
            for k, v in b.r.items():
                self._wait(e, (k, v))
        for b in list(reads) + list(writes):
            if b.bank is not None:
                for k, v in b.bank.acc.items():
                    if k != e:
                        self._wait(e, (k, v))

    def _mark(self, tag, reads, writes):
        key, val = tag
        for b in reads:
            if b.r.get(key, 0) < val:
                b.r[key] = val
        for b in writes:
            b.w = tag
            b.r = {}
        for b in list(reads) + list(writes):
            if b.bank is not None:
                b.bank.acc[key] = val

    def op(self, e, fn, reads=(), writes=()):
        self._deps(e, reads, writes)
        inst = fn(self.eng[e])
        self.cnt[e] += 1
        inst.then_inc(self.sems[e], 1)
        self._mark((e, self.cnt[e]), reads, writes)
        self.n_inst += 1
        return inst

    def dma(self, q, out, in_, reads=(), writes=(), fn=None, **kw):
        lst, idx = self.dq[q]
        key = lst[idx % len(lst)]
        self.dq[q][1] = idx + 1
        if self.cnt[key] > 0:
            self._wait(q, (key, self.cnt[key]))
        self._deps(q, reads, writes)
        if fn is not None:
            inst = fn(self.eng[q])
        else:
            inst = self.eng[q].dma_start(out=out, in_=in_, **kw)
        self.cnt[key] += 16
        inst.then_inc(self.sems[key], 16)
        self._mark((key, self.cnt[key]), reads, writes)
        self.n_inst += 1
        return inst

    def wait_all(self, e, bufs):
        for b in bufs:
            self._wait(e, b.w)

    def barrier(self):
        for e in self.ENG:
            for key, val in self.cnt.items():
                if val > 0 and key != e:
                    self._wait(e, (key, val))
            if e != "pe" and self.cnt[e] > 0 and self.same_eng_sync:
                self._wait(e, (e, self.cnt[e]))


def build_nc(debug=(), stop_after=None):
    nc = bass.Bass("TRN2", target_bir_lowering=False)
    dbg = set(debug)

    def din(name, shape, dt=F32):
        return nc.dram_tensor(name, list(shape), dt, kind="ExternalInput").ap()

    def dscr(name, shape, dt=F32):
        kind = "ExternalOutput" if name in dbg else "Internal"
        return nc.dram_tensor(name, list(shape), dt, kind=kind).ap()

    xT = din("xT", [D, S])
    xtok = din("x", [S, D])
    ccol = din("ccol", [128, KC])
    w_ada = din("w_ada", [D, 6 * D])
    b_ada = din("b_ada", [1, 6 * D])
    w_in = din("w_in", [D, DIN])
    convw = din("convw", [128, 24, 4])
    a_log = din("a_log", [1, NH])
    dt_bias = din("dt_bias", [1, NH])
    f_bias = din("f_bias", [1, NH])
    norm_w = din("norm_w", [128, 1])
    w_out = din("w_out", [D, D])
    ln1_g = din("ln1_g", [1, D]); ln1_b = din("ln1_b", [1, D])
    ln2_g = din("ln2_g", [1, D]); ln2_b = din("ln2_b", [1, D])
    w_r = din("w_r", [D, 36]); b_r = din("b_r", [1, 36])
    out = nc.dram_tensor("out", [S, D], F32, kind="ExternalOutput").ap()

    modrow = dscr("modrow", [1, 6 * D])
    projT = dscr("projT", [48, 128, S], BF16)
    vtok = dscr("vtok", [S, 1024], BF16)
    gat = dscr("gat", [S, 24], F32)

    with ExitStack() as st:
        ks = KS(nc, st, same_eng_sync=(os.environ.get("SES", "1") == "1"))
        cst = ExitStack(); st.enter_context(cst)

        def sbt(stack, name, shape, dt):
            return stack.enter_context(nc.sbuf_tensor(name, list(shape), dt))

        BANKS = {}

        def pst(stack, name, shape, dt=F32):
            full = 512 if dt == F32 else 1024
            t_ = stack.enter_context(nc.psum_tensor(name, [128, full], dt))
            BANKS[name] = Bank()
            P = shape[0]
            n = 1
            for d_ in shape[1:]:
                n *= d_
            v = t_[0:P, 0:n]
            if len(shape) == 3:
                v = v.rearrange("p (a b) -> p a b", b=shape[2])
            return v

        def PB(name):
            return Buf(name, BANKS[name])

        ident_f = sbt(cst, "ident_f", [128, 128], F32); B_ident = Buf("ident")
        ident_h = sbt(cst, "ident_h", [128, 128], BF16)
        ks.op("pool", lambda e: e.memset(ident_f[:], 1.0), writes=[B_ident])
        ks.op("pool", lambda e: e.affine_select(out=ident_f[:], in_=ident_f[:], pattern=[[-1, 128]],
                                                compare_op=ALU.is_equal, fill=0.0, base=0, channel_multiplier=1),
              reads=[B_ident], writes=[B_ident])
        ks.op("pool", lambda e: e.tensor_copy(ident_h[:], ident_f[:]), reads=[B_ident], writes=[B_ident])
        sc1c = sbt(cst, "sc1c", [128, KC], F32)
        sh1c = sbt(cst, "sh1c", [128, KC], F32)
        B_mc = Buf("modcols")
        B_modrow = Buf("modrow")

        with ExitStack() as ph:
            cc = sbt(ph, "cc", [128, KC], F32); B_cc = Buf()
            sc = sbt(ph, "sc", [128, KC], F32); B_sc = Buf()
            brow = sbt(ph, "brow", [1, 6 * D], F32); B_brow = Buf()
            mrow = sbt(ph, "mrow", [1, 6 * D], F32); B_mrow = Buf()
            wa = [sbt(ph, "wa%d" % i, [128, KC, 512], F32) for i in range(2)]; B_wa = [Buf(), Buf()]
            pm = [pst(ph, "pm%d" % i, [1, 512]) for i in range(2)]; B_pm = [PB("pm0"), PB("pm1")]
            ks.dma("sp", cc[:], ccol[:, :], writes=[B_cc])
            ks.dma("sp", brow[:], b_ada[:, :], writes=[B_brow])
            ks.op("act", lambda e: e.activation(out=sc[:], in_=cc[:], func=AF.Silu), reads=[B_cc], writes=[B_sc])
            w_ada_v = w_ada.rearrange("(kc p) n -> p kc n", p=128)
            NCG = 6 * D // 512
            for cg in range(NCG):
                i = cg % 2
                ks.dma("sp", wa[i][:], w_ada_v[:, :, cg * 512:(cg + 1) * 512], writes=[B_wa[i]])
                for kc in range(KC):
                    ks.op("pe", lambda e, kc=kc, i=i: e.matmul(pm[i][:], lhsT=sc[:, kc:kc + 1], rhs=wa[i][:, kc, :],
                                                             start=(kc == 0), stop=(kc == KC - 1)),
                          reads=[B_sc, B_wa[i]], writes=[B_pm[i]])
                ks.op("dve", lambda e, cg=cg, i=i: e.tensor_tensor(out=mrow[:, cg * 512:(cg + 1) * 512], in0=pm[i][:],
                                                                   in1=brow[:, cg * 512:(cg + 1) * 512], op=ALU.add),
                      reads=[B_pm[i], B_brow], writes=[B_mrow])
            ks.dma("sp", modrow[:, :], mrow[:], reads=[B_mrow], writes=[B_modrow])
            t16 = sbt(ph, "t16", [KC, 2, 128], F32); B_t16 = Buf()
            ks.dma("sp", t16[:, 0, :], modrow[0, 0:D].rearrange("(kc p) -> kc p", p=128), reads=[B_modrow], writes=[B_t16])
            ks.dma("sp", t16[:, 1, :], modrow[0, D:2 * D].rearrange("(kc p) -> kc p", p=128), reads=[B_modrow], writes=[B_t16])
            pt = pst(ph, "pt", [128, 2, KC]); B_pt = PB("pt")
            for j in range(2):
                ks.op("pe", lambda e, j=j: e.transpose(pt[:, j, :], t16[:, j, :], ident_f[0:KC, 0:KC]),
                      reads=[B_t16, B_ident], writes=[B_pt])
            ks.op("dve", lambda e: e.tensor_copy(sh1c[:], pt[:, 0, :]), reads=[B_pt], writes=[B_mc])
            ks.op("dve", lambda e: e.tensor_scalar(out=sc1c[:], in0=pt[:, 1, :], scalar1=1.0, scalar2=None, op0=ALU.add),
                  reads=[B_pt], writes=[B_mc])
            ks.barrier()
        if stop_after == 0:
            return _finish(nc, ks, [B_modrow])

        B_projT = [Buf("projT%d" % j) for j in range(48)]
        B_vtok = Buf("vtok"); B_gat = Buf("gat")
        with ExitStack() as ph:
            hT = sbt(ph, "hT", [128, KC, S], BF16); B_hT = [Buf() for _ in range(KC)]
            wf = [sbt(ph, "wf%d" % i, [128, KC, 256], F32) for i in range(2)]; B_wf = [Buf(), Buf()]
            wh = [sbt(ph, "wh%d" % i, [128, KC, 256], BF16) for i in range(2)]; B_wh = [Buf(), Buf()]
            w_in_v = w_in.rearrange("(kc p) n -> p kc n", p=128)
            ks.dma("sp", wf[0][:, :, 0:256], w_in_v[:, :, 0:256], writes=[B_wf[0]])
            ks.op("pool", lambda e: e.tensor_copy(wh[0][:, :, 0:256], wf[0][:, :, 0:256]), reads=[B_wf[0]], writes=[B_wh[0]])
            xsl = [wf[1][:, 2 * i:2 * i + 2, :].rearrange("p a b -> p (a b)") for i in range(8)]
            B_xsl = [Buf() for _ in range(8)]
            B_hTg = [[Buf() for _ in range(NG)] for _ in range(KC)]
            xT_v = xT.rearrange("(kc p) t -> p kc t", p=128)
            n = 0
            for tg in range(NG):
                for kc in range(KC):
                    i = n % 8; n += 1
                    ks.dma("sp", xsl[i], xT_v[:, kc, tg * 512:(tg + 1) * 512], writes=[B_xsl[i]])
                    ks.op("act", lambda e, kc=kc, tg=tg, i=i: e.activation(
                        out=hT[:, kc, tg * 512:(tg + 1) * 512], in_=xsl[i], func=AF.Identity,
                        scale=sc1c[:, kc:kc + 1], bias=sh1c[:, kc:kc + 1]),
                        reads=[B_xsl[i], B_mc], writes=[B_hTg[kc][tg]])
            stg_all = sbt(ph, "stg", [128, 2 * S], BF16)
            stg = [stg_all[:, i * S:(i + 1) * S] for i in range(2)]; B_stg = [Buf(), Buf()]
            vst = stg_all[:, :].rearrange("p (t c) -> p t c", c=256)
            gst = sbt(ph, "gst", [128, NT, 24], F32); B_gst = Buf()
            pp = [pst(ph, "pp%d" % i, [128, 512]) for i in range(4)]; B_pp = [PB("pp%d" % i) for i in range(4)]
            w_in_v = w_in.rearrange("(kc p) n -> p kc n", p=128)
            n_cg = (DIN + 255) // 256
            pi = 0; si = 0; ev = 0
            for cg in range(n_cg):
                i = cg % 2
                c0 = cg * 256
                ncol = min(256, DIN - c0)
                if cg > 0:
                    ks.dma("sp", wf[i][:, :, 0:ncol], w_in_v[:, :, c0:c0 + ncol], writes=[B_wf[i]] + (B_xsl if cg == 1 else []))
                    ks.op("pool", lambda e, i=i, ncol=ncol: e.tensor_copy(wh[i][:, :, 0:ncol], wf[i][:, :, 0:ncol]),
                          reads=[B_wf[i]], writes=[B_wh[i]])
                if c0 < 6144:
                    for jb in range(2):
                        j = cg * 2 + jb
                        s_ = si % 2; si += 1
                        for tg in range(NG):
                            p_ = pi % 4; pi += 1
                            for kc in range(KC):
                                ks.op("pe", lambda e, kc=kc, i=i, jb=jb, tg=tg, p_=p_: e.matmul(
                                    pp[p_][:], lhsT=wh[i][:, kc, jb * 128:(jb + 1) * 128],
                                    rhs=hT[:, kc, tg * 512:(tg + 1) * 512], start=(kc == 0), stop=(kc == KC - 1)),
                                    reads=[B_wh[i], B_hTg[kc][tg]], writes=[B_pp[p_]])
                            if ev % 2 == 0:
                                ks.op("act", lambda e, s_=s_, tg=tg, p_=p_: e.activation(
                                    out=stg[s_][:, tg * 512:(tg + 1) * 512], in_=pp[p_][:], func=AF.Copy),
                                    reads=[B_pp[p_]], writes=[B_stg[s_]])
                            else:
                                ks.op("dve", lambda e, s_=s_, tg=tg, p_=p_: e.tensor_copy(
                                    stg[s_][:, tg * 512:(tg + 1) * 512], pp[p_][:]),
                                    reads=[B_pp[p_]], writes=[B_stg[s_]])
                            ev += 1
                        ks.dma("sp", projT[j], stg[s_], reads=[B_stg[s_]], writes=[B_projT[j]])
                else:
                    isg = ncol < 256
                    for tt in range(NT):
                        p_ = pi % 4; pi += 1
                        for kc in range(KC):
                            ks.op("pe", lambda e, kc=kc, i=i, tt=tt, p_=p_, ncol=ncol: e.matmul(
                                pp[p_][:, 0:ncol], lhsT=hT[:, kc, tt * 128:(tt + 1) * 128],
                                rhs=wh[i][:, kc, 0:ncol], start=(kc == 0), stop=(kc == KC - 1)),
                                reads=[B_wh[i], B_hTg[kc][tt // 4]], writes=[B_pp[p_]])
                        if isg:
                            ks.op("dve", lambda e, tt=tt, p_=p_: e.tensor_copy(gst[:, tt, :], pp[p_][:, 0:24]),
                                  reads=[B_pp[p_]], writes=[B_gst])
                        elif ev % 2 == 0:
                            ks.op("act", lambda e, tt=tt, p_=p_: e.activation(out=vst[:, tt, :], in_=pp[p_][:, 0:256], func=AF.Copy),
                                  reads=[B_pp[p_]], writes=B_stg)
                        else:
                            ks.op("dve", lambda e, tt=tt, p_=p_: e.tensor_copy(vst[:, tt, :], pp[p_][:, 0:256]),
                                  reads=[B_pp[p_]], writes=B_stg)
                        ev += 1
                    if isg:
                        ks.dma("sp", gat.rearrange("(tt p) c -> p tt c", p=128), gst[:], reads=[B_gst], writes=[B_gat])
                    else:
                        v0 = c0 - 6144
                        ks.dma("sp", vtok[:, v0:v0 + 256].rearrange("(tt p) c -> p tt c", p=128), vst,
                               reads=B_stg, writes=[B_vtok])
            ks.barrier()
        if stop_after == 1:
            return _finish(nc, ks, B_projT + [B_vtok, B_gat])

        def act(out, in_, func, R, W, **kw):
            return ks.op("act", lambda e: e.activation(out=out, in_=in_, func=func, **kw), reads=R, writes=W)

        def ts(eng, out, in0, s1, op0, R, W, s2=None, op1=None):
            kw = dict(out=out, in0=in0, scalar1=s1, scalar2=s2, op0=op0)
            if op1 is not None:
                kw["op1"] = op1
            return ks.op(eng, lambda e: e.tensor_scalar(**kw), reads=R, writes=W)

        def tt(eng, out, in0, in1, op, R, W):
            return ks.op(eng, lambda e: e.tensor_tensor(out=out, in0=in0, in1=in1, op=op), reads=R, writes=W)

        def stt(out, in0, scalar, in1, op0, op1, R, W):
            return ks.op("dve", lambda e: e.scalar_tensor_tensor(out=out, in0=in0, scalar=scalar, in1=in1, op0=op0, op1=op1),
                         reads=R, writes=W)

        def mm(out, lhsT, rhs, start, stop, R, W):
            return ks.op("pe", lambda e: e.matmul(out, lhsT=lhsT, rhs=rhs, start=start, stop=stop), reads=R, writes=W)

        def tr(out, in_, idn, R, W):
            return ks.op("pe", lambda e: e.transpose(out, in_, idn), reads=R, writes=W)

        def cp(eng, out, in_, R, W):
            if eng == "act":
                return act(out, in_, AF.Copy, R, W)
            return ks.op(eng, lambda e: e.tensor_copy(out, in_), reads=R, writes=W)

        def asel(out, in_, cmp, fill, R, W):
            if cmp == "le":
                pat, cm, base = [[1, 128]], -1, 0
            else:
                pat, cm, base = [[-1, 128]], 1, -1
            return ks.op("pool", lambda e: e.affine_select(out=out, in_=in_, pattern=pat, compare_op=ALU.is_ge,
                                                           fill=fill, base=base, channel_multiplier=cm), reads=R, writes=W)

        B_c = Buf("consts")
        ones_f = sbt(cst, "ones_f", [128, 128], F32); ones_h = sbt(cst, "ones_h", [128, 128], BF16)
        UT_f = sbt(cst, "UT_f", [128, 128], F32)
        maskL = sbt(cst, "maskL", [128, 128], F32); maskU = sbt(cst, "maskU", [128, 128], F32)
        ccols = sbt(cst, "ccols", [128, 4], F32)
        ks.op("pool", lambda e: e.memset(ones_f[:], 1.0), writes=[B_c])
        ks.op("pool", lambda e: e.memset(ones_h[:], 1.0), writes=[B_c])
        ks.op("pool", lambda e: e.memset(UT_f[:], 1.0), writes=[B_c])
        asel(UT_f[:], UT_f[:], "le", 0.0, [B_c], [B_c])
        ks.op("pool", lambda e: e.memset(maskL[:], 0.0), writes=[B_c])
        asel(maskL[:], maskL[:], "gt", 1.0e4, [B_c], [B_c])
        ks.op("pool", lambda e: e.memset(maskU[:], 0.0), writes=[B_c])
        asel(maskU[:], maskU[:], "le", -1.0e4, [B_c], [B_c])
        ks.op("pool", lambda e: e.memset(ccols[:, 0:1], 0.0), writes=[B_c])
        ks.op("pool", lambda e: e.memset(ccols[:, 1:2], 1.0), writes=[B_c])
        ks.op("pool", lambda e: e.memset(ccols[:, 2:3], 1.0e-6), writes=[B_c])
        ks.op("pool", lambda e: e.memset(ccols[:, 3:4], 1.0e-5), writes=[B_c])
        c0_, c1_, ce6, ce5 = ccols[:, 0:1], ccols[:, 1:2], ccols[:, 2:3], ccols[:, 3:4]
        STOPG = int(os.environ.get("STOPG", "0"))
        if STOPG == 1:
            return _finish(nc, ks, [])

        oT = dscr("oT", [16, 128, S], BF16)
        B_oT = [[Buf() for _ in range(NG)] for _ in range(16)]

        gst_ = ExitStack(); st.enter_context(gst_)
        B_g = Buf("gates")
        gt = sbt(gst_, "gt", [128, NT, 24], F32)
        prm = sbt(gst_, "prm", [128, 3, NH], F32)
        G8 = lambda nm: sbt(gst_, nm, [128, NH, NT], F32)
        beta = G8("beta"); nbeta = G8("nbeta"); gl = G8("gl"); Gc = G8("Gc"); nbg = G8("nbg")
        kds = G8("kds"); eGl = G8("eGl"); tmpg = G8("tmpg"); lf = G8("lf"); Fc = G8("Fc"); nF = G8("nF")
        nea = sbt(gst_, "nea", [128, NH], F32); nfb = sbt(gst_, "nfb", [128, NH], F32)
        ks.dma("sp", gt[:], gat.rearrange("(tt p) c -> p tt c", p=128), reads=[B_gat], writes=[B_g])
        ks.dma("sp", prm[:, 0, :], a_log[0:1, :].broadcast_to([128, NH]), writes=[B_g])
        ks.dma("sp", prm[:, 1, :], dt_bias[0:1, :].broadcast_to([128, NH]), writes=[B_g])
        ks.dma("sp", prm[:, 2, :], f_bias[0:1, :].broadcast_to([128, NH]), writes=[B_g])
        RG = [B_g, B_c]
        act(nea[:], prm[:, 0, :], AF.Exp, RG, [B_g])
        ts("dve", nea[:], nea[:], -1.0, ALU.mult, RG, [B_g])
        ts("dve", nfb[:], prm[:, 2, :], -1.0, ALU.mult, RG, [B_g])
        if STOPG == 2:
            return _finish(nc, ks, [])
        for h in range(NH):
            act(beta[:, h, :], gt[:, :, h], AF.Sigmoid, RG, [B_g])
        for h in range(NH):
            act(tmpg[:, h, :], gt[:, :, 8 + h], AF.Exp, RG, [B_g], bias=prm[:, 1, h:h + 1], scale=1.0)
            act(lf[:, h, :], gt[:, :, 16 + h], AF.Exp, RG, [B_g], bias=nfb[:, h:h + 1], scale=-1.0)
        gflat = lambda t_: t_[:, :, :].rearrange("p h t -> p (h t)")
        act(gflat(tmpg), gflat(tmpg), AF.Ln, RG, [B_g], bias=c1_, scale=1.0)
        act(gflat(lf), gflat(lf), AF.Ln, RG, [B_g], bias=c1_, scale=1.0)
        ts("dve", gflat(lf), gflat(lf), -1.0, ALU.mult, RG, [B_g])
        for h in range(NH):
            ts("dve", gl[:, h, :], tmpg[:, h, :], nea[:, h:h + 1], ALU.mult, RG, [B_g])
        ts("dve", gflat(nbeta), gflat(beta), -1.0, ALU.mult, RG, [B_g])
        if STOPG == 3:
            return _finish(nc, ks, [])
        with ExitStack() as gp:
            pg = pst(gp, "pg", [128, 2, NH * NT]); B_pg = PB("pg")
            mm(pg[:, 0, :], UT_f[:], gflat(gl), True, True, RG, [B_pg])
            mm(pg[:, 1, :], ones_f[:], gflat(gl), True, True, RG, [B_pg])
            cp("dve", gflat(Gc), pg[:, 0, :], [B_pg], [B_g])
            act(gflat(eGl), pg[:, 1, :], AF.Exp, [B_pg, B_c], [B_g])
            tt("dve", gflat(kds), pg[:, 1, :], gflat(Gc), ALU.subtract, [B_pg, B_g], [B_g])
            act(gflat(kds), gflat(kds), AF.Exp, RG, [B_g])
            act(gflat(tmpg), gflat(Gc), AF.Exp, RG, [B_g])
            tt("dve", gflat(nbg), gflat(tmpg), gflat(nbeta), ALU.mult, RG, [B_g])
            if STOPG == 4:
                return _finish(nc, ks, [])
            mm(pg[:, 0, :], UT_f[:], gflat(lf), True, True, RG, [B_pg])
            mm(pg[:, 1, :], ones_f[:], gflat(lf), True, True, RG, [B_pg])
            cp("dve", gflat(Fc), pg[:, 0, :], [B_pg], [B_g])
            cp("dve", gflat(nF), pg[:, 1, :], [B_pg], [B_g])
            for h in range(NH):
                ks.op("dve", lambda e, h=h: e.tensor_tensor_scan(out=tmpg[:, h, :], data0=ones_f[:, 0:NT], data1=nF[:, h, :],
                                                                 initial=0.0, op0=ALU.mult, op1=ALU.add), reads=RG, writes=[B_g])
            if STOPG == 5:
                return _finish(nc, ks, [])
            tt("dve", gflat(tmpg), gflat(tmpg), gflat(nF), ALU.subtract, RG, [B_g])
            tt("dve", gflat(Fc), gflat(Fc), gflat(tmpg), ALU.add, RG, [B_g])
            ts("dve", gflat(nF), gflat(Fc), -1.0, ALU.mult, RG, [B_g])
            if STOPG == 6:
                return _finish(nc, ks, [])
            ks.barrier()

        if stop_after == 1.5:
            return _finish(nc, ks, [])
        with ExitStack() as ph:
            raw = [[sbt(ph, "raw%d_%d" % (p_, i), [128, 515], BF16) for i in range(3)] for p_ in range(2)]
            B_raw = [[Buf() for i in range(3)] for p_ in range(2)]
            cw = sbt(ph, "cw", [128, 24, 4], F32); B_cw = Buf()
            nw = sbt(ph, "nw", [128, 1], F32)
            ks.dma("sp", cw[:], convw[:, :, :], writes=[B_cw])
            ks.dma("sp", nw[:], norm_w[:, :], writes=[B_cw])
            cacc = [sbt(ph, "cacc%d" % i, [128, 512], F32) for i in range(3)]; B_cacc = [Buf() for _ in range(3)]
            sqb = [sbt(ph, "sqb%d" % i, [128, 512], BF16) for i in range(2)]; B_sqb = [Buf(), Buf()]
            rsb = [sbt(ph, "rsb%d" % i, [128, 512], F32) for i in range(2)]; B_rsb = [Buf(), Buf()]
            qnG = sbt(ph, "qnG", [128, 2, NH, 512], BF16); knG = sbt(ph, "knG", [128, 2, NH, 512], BF16)
            vTG = sbt(ph, "vTG", [128, 2, NH, 512], BF16)
            B_qkv = [[Buf() for _ in range(NH)] for _ in range(2)]
            kdec = sbt(ph, "kdec", [128, NH, 4, 128], BF16); vb = sbt(ph, "vb", [128, NH, 4, 128], BF16)
            TT = sbt(ph, "TT", [128, NH, 4, 128], BF16); qkT = sbt(ph, "qkT", [128, NH, 4, 128], BF16)
            qg = sbt(ph, "qg", [128, NH, 4, 128], BF16)
            B_ck = [[Buf() for _ in range(4)] for _ in range(NH)]
            B_TT = [[Buf() for _ in range(4)] for _ in range(NH)]
            oTs = sbt(ph, "oTs", [128, NH, 512], F32); B_oTs = [[Buf() for _ in range(4)] for _ in range(NH)]
            S_f = sbt(ph, "S_f", [128, NH, 128], F32); S_h = sbt(ph, "S_h", [128, NH, 128], BF16); B_S = [Buf() for _ in range(NH)]
            NB = 2
            W4 = 4
            mk = lambda nm, dt_: [[sbt(ph, "%s%d_%d" % (nm, i, j), [128, 128], dt_) for j in range(W4)] for i in range(NB)]
            diagG4 = [sbt(ph, "diagG4_%d" % i, [128, W4, 128], F32) for i in range(NB)]
            diagG = [[diagG4[i][:, j, :] for j in range(W4)] for i in range(NB)]
            mL = mk("mL", F32); mU = mk("mU", F32); eGb = mk("eGb", F32)
            Pn = [mk("PnA", BF16), mk("PnB", BF16)]; Pt = [mk("PtA", BF16), mk("PtB", BF16)]; XT = [mk("XTA", BF16), mk("XTB", BF16)]
            B_tmp = [[Buf() for _ in range(W4)] for _ in range(NB)]
            rbuf = sbt(ph, "rbuf", [128, NH, 128], BF16); B_r = [Buf() for _ in range(NH)]
            vnew = sbt(ph, "vnew", [128, NH, 128], BF16); B_vn = [Buf() for _ in range(NH)]
            zb = [sbt(ph, "zb%d" % i, [128, 512], BF16) for i in range(2)]; B_zb = [Buf(), Buf()]
            zs = [sbt(ph, "zs%d" % i, [128, 512], F32) for i in range(2)]
            on_ = [sbt(ph, "on%d" % i, [128, 512], F32) for i in range(2)]
            og_ = [sbt(ph, "og%d" % i, [128, 512], BF16) for i in range(2)]; B_og = [Buf(), Buf()]
            P_L = [pst(ph, "P_L%d" % i, [128, 4, 128]) for i in range(3)]
            B_L = [[PB("P_L%d" % i) for _ in range(W4)] for i in range(3)]
            P_T = pst(ph, "P_T", [128, 8, 128], BF16); B_PT = [PB("P_T") for _ in range(8)]
            P_Sa = pst(ph, "P_Sa", [128, 4, 128]); B_Sa = [PB("P_Sa") for _ in range(4)]
            P_Sb = pst(ph, "P_Sb", [128, 4, 128]); B_Sb = [PB("P_Sb") for _ in range(4)]
            P_N = pst(ph, "P_N", [128, 512]); B_PN = PB("P_N")
            cnt = {"sq": 0, "z": 0, "w": 0}
            for h in range(NH):
                ks.op("dve", lambda e, h=h: e.memset(S_f[:, h, :], 0.0), writes=[B_S[h]])
                ks.op("dve", lambda e, h=h: e.memset(S_h[:, h, :], 0.0), writes=[B_S[h]])

            def gprep(h, tg):
                par = tg % 2
                t0 = tg * 512
                srcs = [projT[h], projT[8 + h], projT[16 + h]]
                dsts = [qnG[:, par, h, :], knG[:, par, h, :], vTG[:, par, h, :]]
                Bd = B_qkv[par][h]
                for i in range(3):
                    rb = raw[h % 2][i]; Br = B_raw[h % 2][i]
                    if tg == 0:
                        ks.op("pool", lambda e, rb=rb: e.memset(rb[:, 0:3], 0.0), writes=[Br])
                        ks.dma("sp", rb[:, 3:515], srcs[i][:, 0:512], reads=[B_projT[[h, 8 + h, 16 + h][i]]], writes=[Br])
                    else:
                        ks.dma("sp", rb[:, 0:515], srcs[i][:, t0 - 3:t0 + 512], reads=[B_projT[[h, 8 + h, 16 + h][i]]], writes=[Br])
                    blk = i * 8 + h
                    ca = cacc[i]; Bc = B_cacc[i]
                    ts("dve", ca[:], rb[:, 3:515], cw[:, blk, 3:4], ALU.mult, [Br, B_cw], [Bc])
                    for k in range(3):
                        stt(ca[:], rb[:, k:k + 512], cw[:, blk, k:k + 1], ca[:], ALU.mult, ALU.add, [Br, B_cw, Bc], [Bc])
                    if i == 2:
                        act(dsts[2], ca[:], AF.Silu, [Bc], [Bd])
                    else:
                        act(ca[:], ca[:], AF.Silu, [Bc], [Bc])
                        j = cnt["sq"] % 2; cnt["sq"] += 1
                        tt("pool", sqb[j][:], ca[:], ca[:], ALU.mult, [Bc], [B_sqb[j]])
                        mm(P_N[:], ones_h[:], sqb[j][:], True, True, [B_sqb[j], B_c], [B_PN])
                        act(rsb[j][:], P_N[:], AF.Ln, [B_PN, B_c], [B_rsb[j]], bias=ce6, scale=1.0)
                        act(rsb[j][:], rsb[j][:], AF.Exp, [B_rsb[j]], [B_rsb[j]], scale=-0.5)
                        if i == 0:
                            stt(dsts[0], ca[:], 128.0 ** -0.5, rsb[j][:], ALU.mult, ALU.mult, [Bc, B_rsb[j]], [Bd])
                        else:
                            tt("dve", dsts[1], ca[:], rsb[j][:], ALU.mult, [Bc, B_rsb[j]], [Bd])

            def wave(tg, c, hs):
                par = tg % 2; n = tg * 4 + c
                b = cnt["w"] % NB; cnt["w"] += 1
                cs = slice(c * 128, (c + 1) * 128)
                J = list(range(len(hs)))
                knc = lambda j: knG[:, par, hs[j], cs]
                qnc = lambda j: qnG[:, par, hs[j], cs]
                vTc = lambda j: vTG[:, par, hs[j], cs]
                Bq = lambda j: B_qkv[par][hs[j]]
                Bt = lambda j: B_tmp[b][j]
                Bk = lambda j: B_ck[hs[j]][c]
                parts = []

                def p0():
                    for j in J:
                        tr(P_T[:, j, :], knc(j), ident_h[:], [Bq(j), B_ident], [B_PT[j]])
                        tr(P_T[:, 4 + j, :], vTc(j), ident_h[:], [Bq(j), B_ident], [B_PT[4 + j]])
                    for j in J:
                        h = hs[j]
                        ts("dve", kdec[:, h, c, :], P_T[:, j, :], kds[:, h, n:n + 1], ALU.mult, [B_PT[j], B_g], [Bk(j)])
                        ts("dve", vb[:, h, c, :], P_T[:, 4 + j, :], beta[:, h, n:n + 1], ALU.mult, [B_PT[4 + j], B_g], [Bk(j)])
                    for j in J:
                        h = hs[j]
                        ts("pool", diagG[b][j], ident_f[:], Gc[:, h, n:n + 1], ALU.mult, [B_ident, B_g], [Bt(j)])
                    mm(P_L[0][:, :, :].rearrange("p a b -> p (a b)"), ones_f[:], diagG4[b][:, :, :].rearrange("p a b -> p (a b)"),
                       True, True, [Bt(j) for j in J] + [B_c], [B_L[0][j] for j in J])
                parts.append(p0)

                def p1():
                    for j in J:
                        h = hs[j]
                        stt(mL[b][j][:], P_L[0][:, j, :], Gc[:, h, n:n + 1], maskL[:], ALU.subtract, ALU.max, [B_L[0][j], B_g, B_c], [Bt(j)])
                        stt(mU[b][j][:], P_L[0][:, j, :], Gc[:, h, n:n + 1], maskU[:], ALU.subtract, ALU.min, [B_L[0][j], B_g, B_c], [Bt(j)])
                    for j in J:
                        act(eGb[b][j][:], P_L[0][:, j, :], AF.Exp, [B_L[0][j]], [Bt(j)])
                    for j in J:
                        act(mL[b][j][:], mL[b][j][:], AF.Exp, [Bt(j)], [Bt(j)], scale=-1.0)
                        act(mU[b][j][:], mU[b][j][:], AF.Exp, [Bt(j)], [Bt(j)])
                    for j in J:
                        tt("pool", qg[:, hs[j], c, :], qnc(j), eGb[b][j][:], ALU.mult, [Bq(j), Bt(j)], [Bk(j)])
                    for j in J:
                        mm(P_L[1][:, j, :], knc(j), knc(j), True, True, [Bq(j)], [B_L[1][j]])
                        mm(P_L[2][:, j, :], knc(j), qnc(j), True, True, [Bq(j)], [B_L[2][j]])
                parts.append(p1)

                def p2():
                    for j in J:
                        h = hs[j]
                        stt(Pn[0][b][j][:], P_L[1][:, j, :], nbeta[:, h, n:n + 1], mL[b][j][:], ALU.mult, ALU.mult, [B_L[1][j], B_g, Bt(j)], [Bt(j)])
                        tt("dve", qkT[:, h, c, :], P_L[2][:, j, :], mU[b][j][:], ALU.mult, [B_L[2][j], Bt(j)], [Bk(j)])
                    for j in J:
                        tr(P_T[:, j, :], Pn[0][b][j][:], ident_h[:], [Bt(j), B_ident], [B_PT[j]])
                    for j in J:
                        cp("act", Pt[0][b][j][:], P_T[:, j, :], [B_PT[j]], [Bt(j)])
                    for j in J:
                        tt("dve", XT[0][b][j][:], P_T[:, j, :], ident_f[:], ALU.add, [B_PT[j], B_ident], [Bt(j)])
                parts.append(p2)

                for k in range(1, 7):
                    def pl(k=k):
                        a = (k - 1) % 2; c_ = k % 2
                        for j in J:
                            mm(P_L[0][:, j, :], Pt[a][b][j][:], Pn[a][b][j][:], True, True, [Bt(j)], [B_L[0][j]])
                        if k < 6:
                            for j in J:
                                mm(P_L[1][:, j, :], Pn[a][b][j][:], Pt[a][b][j][:], True, True, [Bt(j)], [B_L[1][j]])
                        for j in J:
                            cp("act", Pn[c_][b][j][:], P_L[0][:, j, :], [B_L[0][j]], [Bt(j)])
                        if k < 6:
                            for j in J:
                                cp("dve", Pt[c_][b][j][:], P_L[1][:, j, :], [B_L[1][j]], [Bt(j)])
                        for j in J:
                            mm(P_L[2][:, j, :], Pn[c_][b][j][:], XT[a][b][j][:], True, True, [Bt(j)], [B_L[2][j]])
                        for j in J:
                            if k == 6:
                                tt("dve", TT[:, hs[j], c, :], P_L[2][:, j, :], XT[a][b][j][:], ALU.add, [B_L[2][j], Bt(j)], [B_TT[hs[j]][c]])
                            else:
                                tt("dve", XT[c_][b][j][:], P_L[2][:, j, :], XT[a][b][j][:], ALU.add, [B_L[2][j], Bt(j)], [Bt(j)])
                    parts.append(pl)
                return parts

            def sstep(tg, c, hs):
                par = tg % 2; n = tg * 4 + c
                cs = slice(c * 128, (c + 1) * 128)
                J = list(range(len(hs)))
                st_ = []

                def s0():
                    for j in J:
                        h = hs[j]
                        mm(P_Sa[:, j, :], knG[:, par, h, cs], S_h[:, h, :], True, True, [B_qkv[par][h], B_S[h]], [B_Sa[j]])
                    for j in J:
                        h = hs[j]
                        stt(rbuf[:, h, :], P_Sa[:, j, :], nbg[:, h, n:n + 1], vb[:, h, c, :], ALU.mult, ALU.add,
                            [B_Sa[j], B_g, B_ck[h][c]], [B_r[h]])
                st_.append(s0)

                def s1():
                    for j in J:
                        h = hs[j]
                        mm(P_Sa[:, j, :], TT[:, h, c, :], rbuf[:, h, :], True, True, [B_TT[h][c], B_r[h]], [B_Sa[j]])
                    for j in J:
                        h = hs[j]
                        cp("act", vnew[:, h, :], P_Sa[:, j, :], [B_Sa[j]], [B_vn[h]])
                st_.append(s1)

                def s2():
                    for j in J:
                        h = hs[j]
                        mm(P_Sb[:, j, :], S_h[:, h, :], qg[:, h, c, :], True, False, [B_S[h], B_ck[h][c]], [B_Sb[j]])
                        mm(P_Sb[:, j, :], vnew[:, h, :], qkT[:, h, c, :], False, True, [B_vn[h], B_ck[h][c]], [B_Sb[j]])
                    for j in J:
                        h = hs[j]
                        mm(P_Sa[:, j, :], kdec[:, h, c, :], vnew[:, h, :], True, True, [B_ck[h][c], B_vn[h]], [B_Sa[j]])
                    for j in J:
                        h = hs[j]
                        cp("act", oTs[:, h, cs], P_Sb[:, j, :], [B_Sb[j]], [B_oTs[h][c]])
                    for j in J:
                        h = hs[j]
                        ts("dve", S_f[:, h, :], S_f[:, h, :], eGl[:, h, n:n + 1], ALU.mult, [B_S[h], B_g], [B_S[h]])
                        tt("dve", S_f[:, h, :], P_Sa[:, j, :], S_f[:, h, :], ALU.add, [B_Sa[j], B_S[h]], [B_S[h]])
                    for j in J:
                        h = hs[j]
                        cp("act", S_h[:, h, :], S_f[:, h, :], [B_S[h]], [B_S[h]])
                st_.append(s2)
                return st_

            def pnorm(h, tg):
                t0 = tg * 512
                j = cnt["z"] % 2; cnt["z"] += 1
                Bo = [B_oTs[h][k] for k in range(4)]
                ks.dma("sp", zb[j][:], projT[24 + h][:, t0:t0 + 512], reads=[B_projT[24 + h]], writes=[B_zb[j]])
                q = cnt["sq"] % 2; cnt["sq"] += 1
                tt("pool", sqb[q][:], oTs[:, h, :], oTs[:, h, :], ALU.mult, Bo, [B_sqb[q]])
                mm(P_N[:], ones_h[:], sqb[q][:], True, True, [B_sqb[q], B_c], [B_PN])
                act(rsb[q][:], P_N[:], AF.Ln, [B_PN, B_c], [B_rsb[q]], bias=ce6, scale=1.0 / 128.0)
                act(rsb[q][:], rsb[q][:], AF.Exp, [B_rsb[q]], [B_rsb[q]], scale=-0.5)
                act(zs[j][:], zb[j][:], AF.Silu, [B_zb[j]], [B_zb[j]])
                tt("dve", on_[j][:], oTs[:, h, :], rsb[q][:], ALU.mult, Bo + [B_rsb[q]], [B_og[j]])
                stt(og_[j][:], on_[j][:], nw[:, 0:1], zs[j][:], ALU.mult, ALU.mult, [B_og[j], B_cw, B_zb[j]], [B_og[j]])
                ks.dma("sp", oT[h][:, t0:t0 + 512], og_[j][:], reads=[B_og[j]], writes=[B_oT[h][tg]])

            def merge(streams):
                tot = max(len(s_) for s_ in streams) if streams else 0
                idx = [0] * len(streams)
                for step in range(tot):
                    for si_, s_ in enumerate(streams):
                        tgt = ((step + 1) * len(s_) + tot - 1) // tot
                        while idx[si_] < tgt:
                            s_[idx[si_]](); idx[si_] += 1

            HW = [[0, 1, 2, 3], [4, 5, 6, 7]]
            for h in range(NH):
                gprep(h, 0)
            pending = []
            for tg in range(NG):
                for c in range(4):
                    streams = [wave(tg, c, HW[0]) + wave(tg, c, HW[1])]
                    if pending:
                        streams.append(pending)
                    merge(streams)
                    if pending and c == 0 and tg > 0:
                        for h in range(NH):
                            pnorm(h, tg - 1)
                    if tg + 1 < NG:
                        gprep(2 * c, tg + 1); gprep(2 * c + 1, tg + 1)
                    pending = sstep(tg, c, HW[0]) + sstep(tg, c, HW[1])
            merge([pending])
            for h in range(NH):
                pnorm(h, NG - 1)
            ks.barrier()
        if stop_after == 2:
            return _finish(nc, ks, [])

        f3d = dscr("f3d", [NH, S], F32); B_f3d = Buf()
        WROW = 24576
        use_moe = (stop_after is None) or (stop_after >= 5)
        B_wsc = Buf("wsc")
        if use_moe:
            w_moe = din("w_moe", [32, 128, WROW])
            wsc = dscr("wsc", [32 * 128, WROW], BF16)
        SCL = 128.0 ** -0.5
        with ExitStack() as ph:
            P_t = pst(ph, "fP_t", [128, 128]); B_PF = PB("fP_t")
            P_F = P_t[0:NT, :]
            f3t = sbt(ph, "f3t", [NT, NH, 128], F32); B_f3t = Buf()
            for h in range(NH):
                tr(P_F, Fc[:, h, :], ident_f[:], [B_g, B_ident], [B_PF])
                cp("dve", f3t[:, h, :], P_F, [B_PF], [B_f3t])
            ks.dma("sp", f3d.rearrange("h (tt p) -> tt h p", p=128), f3t[:], reads=[B_f3t], writes=[B_f3d])

            qT = [sbt(ph, "fqT%d" % i, [128, S], BF16) for i in range(2)]
            kT = [sbt(ph, "fkT%d" % i, [128, S], BF16) for i in range(2)]
            va = [sbt(ph, "fva%d" % i, [128, NT, 128], BF16) for i in range(2)]
            B_in = [Buf(), Buf()]
            Fb = [sbt(ph, "fFb%d" % i, [128, 512], F32) for i in range(2)]; B_Fb = [Buf(), Buf()]
            lgb = [sbt(ph, "flg%d" % i, [128, 512], F32) for i in range(3)]; B_lg = [Buf() for _ in range(3)]
            pT = [sbt(ph, "fpT%d" % i, [128, 512], BF16) for i in range(4)]; B_pT = [Buf() for _ in range(4)]
            rs_ = [sbt(ph, "frs%d" % i, [128, 512], F32) for i in range(2)]; B_rs_ = [Buf(), Buf()]
            oTg = [sbt(ph, "foTg%d" % i, [128, 512], BF16) for i in range(2)]; B_oTg = [Buf(), Buf()]
            P_s = [pst(ph, "fP_s%d" % i, [128, 512]) for i in range(3)]; B_Ps = [PB("fP_s%d" % i) for i in range(3)]
            P_o = [pst(ph, "fP_o%d" % i, [128, 512]) for i in range(2)]; B_Po = [PB("fP_o0"), PB("fP_o1")]
            P_m = [pst(ph, "fP_m%d" % i, [128, 512]) for i in range(2)]; B_Pm = [PB("fP_m0"), PB("fP_m1")]
            it = 0; gi = 0
            if use_moe:
                wst = [sbt(ph, "wst%d" % i, [128, WROW], BF16) for i in range(2)]; B_wst = [Buf(), Buf()]

            def precast(e_):
                i = e_ % 2
                wv = wst[i]
                ks.dma("pool", wv[:], w_moe[e_], writes=[B_wst[i]])
                ks.dma("sp", wsc[e_ * 128:(e_ + 1) * 128, :], wv[:], reads=[B_wst[i]], writes=[B_wsc])

            for h in range(NH):
                hp = h % 2
                ks.dma("sp", qT[hp][:], projT[32 + h], reads=[B_projT[32 + h]], writes=[B_in[hp]])
                ks.dma("sp", kT[hp][:], projT[40 + h], reads=[B_projT[40 + h]], writes=[B_in[hp]])
                ks.dma("sp", va[hp][:], vtok[:, h * 128:(h + 1) * 128].rearrange("(tt p) c -> p tt c", p=128),
                       reads=[B_vtok], writes=[B_in[hp]])
                for qgi in range(NG):
                    q0 = qgi * 512
                    g_ = gi % 2; gi += 1
                    if use_moe and (h * NG + qgi) % 2 == 0:
                        precast((h * NG + qgi) // 2)
                    ks.dma("sp", Fb[g_][:], f3d[h:h + 1, q0:q0 + 512].broadcast_to([128, 512]), reads=[B_f3d], writes=[B_Fb[g_]])
                    nj = 4 * qgi + 4

                    def front(j):
                        nonlocal it
                        bl = max(0, j - 4 * qgi)
                        c_lo = bl * 128
                        ps_ = P_s[it % 3]; Bps = B_Ps[it % 3]
                        lg_ = lgb[it % 3]; Blg = B_lg[it % 3]
                        pt_ = pT[it % 4]; Bpt = B_pT[it % 4]
                        it += 1
                        mm(ps_[:, c_lo:512], kT[hp][:, j * 128:(j + 1) * 128], qT[hp][:, q0 + c_lo:q0 + 512], True, True,
                           [B_in[hp]], [Bps])
                        stt(lg_[:, c_lo:512], ps_[:, c_lo:512], SCL, Fb[g_][:, c_lo:512], ALU.mult, ALU.add, [Bps, B_Fb[g_]], [Blg])
                        act(pt_[:, c_lo:512], lg_[:, c_lo:512], AF.Exp, [Blg, B_g], [Bpt], bias=nF[:, h, j:j + 1], scale=1.0)
                        if j >= 4 * qgi:
                            asel(pt_[:, c_lo:c_lo + 128], pt_[:, c_lo:c_lo + 128], "le", 0.0, [Bpt], [Bpt])
                        return (pt_, Bpt, c_lo)

                    fq = [front(0)]
                    if nj > 1:
                        fq.append(front(1))
                    for j in range(nj):
                        cur = fq.pop(0)
                        if j + 2 < nj:
                            fq.append(front(j + 2))
                        pt_, Bpt, c_lo = cur
                        mm(P_o[g_][:, c_lo:512], va[hp][:, j, :], pt_[:, c_lo:512], j == 0, j == nj - 1, [Bpt, B_in[hp]], [B_Po[g_]])
                        mm(P_m[g_][:, c_lo:512], ones_h[:], pt_[:, c_lo:512], j == 0, j == nj - 1, [Bpt, B_c], [B_Pm[g_]])
                    cp("dve", rs_[g_][:], P_m[g_][:], [B_Pm[g_]], [B_rs_[g_]])
                    ks.op("dve", lambda e, g_=g_: e.reciprocal(rs_[g_][:], rs_[g_][:]), reads=[B_rs_[g_]], writes=[B_rs_[g_]])
                    tt("dve", oTg[g_][:], P_o[g_][:], rs_[g_][:], ALU.mult, [B_Po[g_], B_rs_[g_]], [B_oTg[g_]])
                    ks.dma("sp", oT[8 + h][:, q0:q0 + 512], oTg[g_][:], reads=[B_oTg[g_]], writes=[B_oT[8 + h][qgi]])
            ks.barrier()
        gst_.close()
        if stop_after == 3:
            return _finish(nc, ks, [])

        NBLK = 64
        BLK = 256
        x1d = dscr("x1d", [S, D], F32); B_x1d = [Buf() for _ in range(NT)]
        h2d = dscr("h2d", [S, D], BF16); B_h2d = [Buf() for _ in range(NT)]
        xsd = dscr("xsd", [NBLK * BLK, D], BF16); B_xsd = Buf()
        ybd = dscr("ybd", [NBLK * BLK, D], BF16); B_ybd = Buf()
        mwd = dscr("mwd", [S, 32], F32); B_mwd = Buf()
        rst = ExitStack(); st.enter_context(rst)
        d01 = sbt(rst, "d01", [128, 2, NT], I32)
        w12 = sbt(rst, "w12", [128, 2, NT], F32)
        widx = sbt(rst, "widx", [128, NBLK], I32)
        B_rs = Buf("routing")

        def bcast_load(stack, name, src_row, B, plus1=False):
            t_ = sbt(stack, name, [128, D], F32)
            ks.dma("sp", t_[:], src_row.broadcast_to([128, D]), reads=[B_modrow], writes=[B])
            if plus1:
                ts("pool", t_[:], t_[:], 1.0, ALU.add, [B], [B])
            return t_

        with ExitStack() as ph:
            B_bc = Buf()
            g1p = bcast_load(ph, "g1p", modrow[0:1, 2 * D:3 * D], B_bc, True)
            sh2b = bcast_load(ph, "sh2b", modrow[0:1, 3 * D:4 * D], B_bc)
            sc2p = bcast_load(ph, "sc2p", modrow[0:1, 4 * D:5 * D], B_bc, True)
            l1g = bcast_load(ph, "l1g", ln1_g[0:1, :], B_bc)
            l1b = bcast_load(ph, "l1b", ln1_b[0:1, :], B_bc)
            wo = sbt(ph, "wo", [128, KC, D], BF16); B_wo = Buf()
            w_out_v = w_out.rearrange("(kc p) n -> p kc n", p=128)
            for kc in range(KC):
                ks.dma("pool", wo[:, kc, :], w_out_v[:, kc, :], writes=[B_wo])
            wr = sbt(ph, "wr", [128, KC, 36], F32); brb = sbt(ph, "brb", [128, 36], F32); B_wr = Buf()
            wrh = sbt(ph, "wrh", [128, KC, 36], BF16)
            ks.dma("sp", wr[:], w_r.rearrange("(kc p) n -> p kc n", p=128), writes=[B_wr])
            ks.dma("sp", brb[:], b_r[0:1, :].broadcast_to([128, 36]), writes=[B_wr])
            cp("dve", wrh[:], wr[:], [B_wr], [B_wr])
            ogb = [sbt(ph, "ogb%d" % i, [128, KC, 512], BF16) for i in range(2)]; B_ogb = [Buf(), Buf()]
            xt_ = sbt(ph, "xt0", [128, D], F32); B_xt = Buf()
            vv = sbt(ph, "vv0", [128, D], F32); B_vv = Buf()
            h2 = sbt(ph, "h2_0", [128, D], F32); B_h2 = Buf()
            h2h = [sbt(ph, "h2h%d" % i, [128, D], BF16) for i in range(2)]; B_h2h = [Buf(), Buf()]
            h2Tf = sbt(ph, "h2Tf", [128, KC, 128], BF16); B_h2Tf = Buf()
            st4 = sbt(ph, "st4", [128, 16], F32); B_st = Buf()
            rt = sbt(ph, "rt", [128, 128], F32); B_rt = Buf()
            OH = sbt(ph, "OH", [128, 2, NT, 32], F32); B_OH = Buf()
            junk = sbt(ph, "junk", [128, D], BF16); B_junk = Buf()
            P_y = [pst(ph, "P_y%d" % i, [128, 512]) for i in range(4)]; B_Py = [PB("P_y%d" % i) for i in range(4)]
            P_h = [pst(ph, "P_h%d" % i, [128, 8, 128], BF16) for i in range(2)]; B_Ph = [PB("P_h0"), PB("P_h1")]
            P_r = pst(ph, "P_r", [128, 36]); B_Pr = PB("P_r")
            oT_v = oT.rearrange("h p t -> p h t")
            py = 0; ph_i = 0
            for tg in range(NG):
                gb = tg % 2
                ks.dma("sp", ogb[gb][:], oT_v[:, :, tg * 512:(tg + 1) * 512],
                       reads=[B_oT[h][tg] for h in range(16)], writes=[B_ogb[gb]])
                for ti in range(4):
                    tI = tg * 4 + ti
                    hb_ = tI % 2
                    ks.dma("sp", xt_[:], xtok[tI * 128:(tI + 1) * 128, :], writes=[B_xt])
                    for cg in range(4):
                        p_ = py % 4; py += 1
                        for hh in range(KC):
                            mm(P_y[p_][:], ogb[gb][:, hh, ti * 128:(ti + 1) * 128], wo[:, hh, cg * 512:(cg + 1) * 512],
                               hh == 0, hh == KC - 1, [B_ogb[gb], B_wo], [B_Py[p_]])
                        tt("dve", vv[:, cg * 512:(cg + 1) * 512], P_y[p_][:], g1p[:, cg * 512:(cg + 1) * 512], ALU.mult,
                           [B_Py[p_], B_bc], [B_vv])
                    stt(vv[:], xt_[:], ALPHA, vv[:], ALU.mult, ALU.add, [B_xt, B_vv], [B_vv])
                    ks.op("dve", lambda e: e.reduce_sum(out=st4[:, 0:1], in_=vv[:], axis=AX.X), reads=[B_vv], writes=[B_st])
                    act(junk[:], vv[:], AF.Square, [B_vv], [B_junk, B_st], accum_out=st4[:, 1:2])
                    ts("dve", st4[:, 2:3], st4[:, 0:1], 1.0 / D, ALU.mult, [B_st], [B_st])
                    tt("dve", st4[:, 3:4], st4[:, 2:3], st4[:, 2:3], ALU.mult, [B_st], [B_st])
                    stt(st4[:, 4:5], st4[:, 1:2], 1.0 / D, st4[:, 3:4], ALU.mult, ALU.subtract, [B_st], [B_st])
                    act(st4[:, 5:6], st4[:, 4:5], AF.Sqrt, [B_st, B_c], [B_st], bias=ce5, scale=1.0)
                    ks.op("dve", lambda e: e.reciprocal(st4[:, 6:7], st4[:, 5:6]), reads=[B_st], writes=[B_st])
                    ts("dve", vv[:], vv[:], st4[:, 2:3], ALU.subtract, [B_vv, B_st], [B_vv], s2=st4[:, 6:7], op1=ALU.mult)
                    tt("dve", vv[:], vv[:], l1g[:], ALU.mult, [B_vv, B_bc], [B_vv])
                    tt("dve", vv[:], vv[:], l1b[:], ALU.add, [B_vv, B_bc], [B_vv])
                    ks.dma("sp", x1d[tI * 128:(tI + 1) * 128, :], vv[:], reads=[B_vv], writes=[B_x1d[tI]])
                    tt("dve", h2[:], vv[:], sc2p[:], ALU.mult, [B_vv, B_bc], [B_h2])
                    tt("dve", h2h[hb_][:], h2[:], sh2b[:], ALU.add, [B_h2, B_bc], [B_h2h[hb_]])
                    ks.dma("sp", h2d[tI * 128:(tI + 1) * 128, :], h2h[hb_][:], reads=[B_h2h[hb_]], writes=[B_h2d[tI]])
                    for k8 in range(2):
                        q_ = ph_i % 2; ph_i += 1
                        for kk in range(8):
                            kc = k8 * 8 + kk
                            tr(P_h[q_][:, kk, :], h2h[hb_][:, kc * 128:(kc + 1) * 128], ident_h[:], [B_h2h[hb_], B_ident], [B_Ph[q_]])
                        cp("act", h2Tf[:, k8 * 8:(k8 + 1) * 8, :], P_h[q_][:, :, :], [B_Ph[q_]], [B_h2Tf])
                    for kc in range(KC):
                        mm(P_r[:, :], h2Tf[:, kc, :], wrh[:, kc, :], kc == 0, kc == KC - 1, [B_h2Tf, B_wr], [B_Pr])
                    R_ = [B_rt, B_c]
                    lg = rt[:, 0:36]
                    tt("dve", lg, P_r[:, :], brb[:], ALU.add, [B_Pr, B_wr], [B_rt])
                    gmx = rt[:, 36:37]; ohg = rt[:, 40:44]; gsum = rt[:, 37:38]; gw = rt[:, 38:39]
                    ks.op("dve", lambda e: e.reduce_max(out=gmx, in_=rt[:, 0:4], axis=AX.X), reads=R_, writes=[B_rt])
                    ts("dve", ohg, rt[:, 0:4], gmx, ALU.is_equal, R_, [B_rt])
                    ts("dve", rt[:, 44:48], rt[:, 0:4], gmx, ALU.subtract, R_, [B_rt])
                    act(rt[:, 44:48], rt[:, 44:48], AF.Exp, R_, [B_rt])
                    ks.op("dve", lambda e: e.reduce_sum(out=gsum, in_=rt[:, 44:48], axis=AX.X), reads=R_, writes=[B_rt])
                    ks.op("dve", lambda e: e.reciprocal(gw, gsum), reads=R_, writes=[B_rt])
                    es = rt[:, 48:56]
                    ts("dve", es, rt[:, 4:12], ohg[:, 0:1], ALU.mult, R_, [B_rt])
                    for g_ in range(1, 4):
                        stt(es, rt[:, 4 + 8 * g_:12 + 8 * g_], ohg[:, g_:g_ + 1], es, ALU.mult, ALU.add, R_, [B_rt])
                    m1 = rt[:, 56:57]; m2 = rt[:, 57:58]; oh1 = rt[:, 64:72]; oh2 = rt[:, 72:80]; es2 = rt[:, 80:88]
                    ks.op("dve", lambda e: e.reduce_max(out=m1, in_=es, axis=AX.X), reads=R_, writes=[B_rt])
                    ts("dve", oh1, es, m1, ALU.is_equal, R_, [B_rt])
                    stt(es2, oh1, -1.0e30, es, ALU.mult, ALU.add, R_, [B_rt])
                    ks.op("dve", lambda e: e.reduce_max(out=m2, in_=es2, axis=AX.X), reads=R_, writes=[B_rt])
                    ts("dve", oh2, es2, m2, ALU.is_equal, R_, [B_rt])
                    w1 = rt[:, 58:59]; w2 = rt[:, 59:60]
                    tt("dve", w1, m2, m1, ALU.subtract, R_, [B_rt])
                    act(w1, w1, AF.Exp, R_, [B_rt])
                    ts("dve", w1, w1, 1.0, ALU.add, R_, [B_rt])
                    ks.op("dve", lambda e: e.reciprocal(w1, w1), reads=R_, writes=[B_rt])
                    ts("dve", w2, w1, -1.0, ALU.mult, R_, [B_rt], s2=1.0, op1=ALU.add)
                    tt("dve", w12[:, 0, tI:tI + 1], w1, gw, ALU.mult, R_, [B_rt, B_rs])
                    tt("dve", w12[:, 1, tI:tI + 1], w2, gw, ALU.mult, R_, [B_rt, B_rs])
                    for g_ in range(4):
                        ts("dve", OH[:, 0, tI, g_ * 8:(g_ + 1) * 8], oh1, ohg[:, g_:g_ + 1], ALU.mult, R_, [B_rt, B_OH])
                        ts("dve", OH[:, 1, tI, g_ * 8:(g_ + 1) * 8], oh2, ohg[:, g_:g_ + 1], ALU.mult, R_, [B_rt, B_OH])
            if "mwd" in dbg:
                mwt_ = sbt(ph, "mwt_", [128, NT, 32], F32)
                for tI in range(NT):
                    ts("dve", mwt_[:, tI, :], OH[:, 0, tI, :], w12[:, 0, tI:tI + 1], ALU.mult, [B_OH, B_rs], [B_mwd])
                    stt(mwt_[:, tI, :], OH[:, 1, tI, :], w12[:, 1, tI:tI + 1], mwt_[:, tI, :], ALU.mult, ALU.add, [B_OH, B_rs, B_mwd], [B_mwd])
                ks.dma("sp", mwd.rearrange("(tt p) c -> p tt c", p=128), mwt_[:], reads=[B_mwd], writes=[B_mwd])
            NE = 32
            Cc = sbt(ph, "Cc", [128, NT * NE], F32); B_s = Buf("sort")
            rk = sbt(ph, "rk", [128, NT * NE], F32)
            pf = sbt(ph, "pf", [128, NT * NE], F32)
            sm = sbt(ph, "sm", [128, 8, 64], F32)
            UTs = sbt(ph, "UTs", [128, 128], F32)
            ks.op("pool", lambda e: e.memset(UTs[:], 1.0), writes=[B_s])
            ks.op("pool", lambda e: e.affine_select(out=UTs[:], in_=UTs[:], pattern=[[1, 128]], compare_op=ALU.is_ge,
                                                    fill=0.0, base=-1, channel_multiplier=-1), reads=[B_s], writes=[B_s])
            OHf = lambda k: OH[:, k, :, :].rearrange("p t e -> p (t e)")
            tt("dve", Cc[:], OHf(0), OHf(1), ALU.add, [B_OH], [B_s])
            for half in range(2):
                sl = slice(half * 512, (half + 1) * 512)
                mm(P_y[0][:], UTs[:], Cc[:, sl], True, True, [B_s], [B_Py[0]])
                mm(P_y[1][:], ones_f[:], Cc[:, sl], True, True, [B_s, B_c], [B_Py[1]])
                cp("dve", rk[:, sl], P_y[0][:], [B_Py[0]], [B_s])
                cp("dve", pf[:, sl], P_y[1][:], [B_Py[1]], [B_s])
            pf3 = pf[:, :].rearrange("p (t e) -> p t e", e=NE)
            rk3 = rk[:, :].rearrange("p (t e) -> p t e", e=NE)
            tot = sm[:, 0, 0:NE]; run = sm[:, 1, 0:NE]
            ks.op("dve", lambda e: e.memset(run, 0.0), writes=[B_s])
            for tI in range(NT):
                tt("dve", rk3[:, tI, :], rk3[:, tI, :], run, ALU.add, [B_s], [B_s])
                tt("dve", run, run, pf3[:, tI, :], ALU.add, [B_s], [B_s])
            cp("dve", tot, run, [B_s], [B_s])
            thr = sm[:, 2, 0:32]; nbk = sm[:, 3, 0:NE]; tmp32 = sm[:, 4, 0:32]
            ks.op("pool", lambda e: e.iota(thr, pattern=[[BLK, 32]], base=0, channel_multiplier=0,
                                           allow_small_or_imprecise_dtypes=True), writes=[B_s])
            for e_ in range(NE):
                ts("dve", tmp32, thr, tot[:, e_:e_ + 1], ALU.is_lt, [B_s], [B_s])
                ks.op("dve", lambda e, e_=e_: e.reduce_sum(out=nbk[:, e_:e_ + 1], in_=tmp32, axis=AX.X), reads=[B_s], writes=[B_s])
            pend = sm[:, 5, 0:NE]; pstart = sm[:, 6, 0:NE]
            ks.op("dve", lambda e: e.tensor_tensor_scan(out=pend, data0=ones_f[:, 0:NE], data1=nbk, initial=0.0,
                                                        op0=ALU.mult, op1=ALU.add), reads=[B_s, B_c], writes=[B_s])
            tt("dve", pstart, pend, nbk, ALU.subtract, [B_s], [B_s])
            ts("dve", pstart, pstart, float(BLK), ALU.mult, [B_s], [B_s])
            ts("dve", pend, pend, float(BLK), ALU.mult, [B_s], [B_s])
            for tI in range(NT):
                tt("dve", rk3[:, tI, :], rk3[:, tI, :], pstart, ALU.add, [B_s], [B_s])
            dstf = sm[:, 7, :]
            for k in range(2):
                tt("dve", Cc[:], OHf(k), rk[:], ALU.mult, [B_OH, B_s], [B_s])
                ks.op("dve", lambda e, k=k: e.tensor_reduce(out=dstf[:, k * NT:(k + 1) * NT],
                                                            in_=Cc[:, :].rearrange("p (t e) -> p t e", e=NE),
                                                            axis=AX.X, op=ALU.add), reads=[B_s], writes=[B_s])
            cp("dve", d01[:, :, :].rearrange("p k t -> p (k t)"), dstf, [B_s], [B_rs])
            bthr = sm[:, 2, 0:NBLK]; bacc = sm[:, 3, 0:NBLK]; btmp = sm[:, 4, 0:NBLK]
            ks.op("pool", lambda e: e.iota(bthr, pattern=[[BLK, NBLK]], base=0, channel_multiplier=0,
                                           allow_small_or_imprecise_dtypes=True), reads=[B_s], writes=[B_s])
            ks.op("dve", lambda e: e.memset(bacc, 0.0), reads=[B_s], writes=[B_s])
            for e_ in range(NE):
                ts("dve", btmp, bthr, pend[:, e_:e_ + 1], ALU.is_ge, [B_s], [B_s])
                tt("dve", bacc, bacc, btmp, ALU.add, [B_s], [B_s])
            ts("dve", bacc, bacc, float(NE - 1), ALU.min, [B_s], [B_s], s2=128.0, op1=ALU.mult)
            pidx = sm[:, 0, 32:33]
            ks.op("pool", lambda e: e.iota(pidx, pattern=[[0, 1]], base=0, channel_multiplier=1,
                                           allow_small_or_imprecise_dtypes=True), reads=[B_s], writes=[B_s])
            ts("dve", bacc, bacc, pidx, ALU.add, [B_s], [B_s])
            cp("dve", widx[:], bacc, [B_s], [B_rs])
            for tI in range(NT):
                hb_ = tI % 2
                ks.dma("sp", h2h[hb_][:], h2d[tI * 128:(tI + 1) * 128, :], reads=[B_h2d[tI]], writes=[B_h2h[hb_]])
                for k in range(2):
                    ks.dma("pool", None, None, reads=[B_h2h[hb_], B_rs], writes=[B_xsd],
                           fn=lambda e, k=k, tI=tI, hb_=hb_: e.indirect_dma_start(
                               out=xsd[:, :], out_offset=bass.IndirectOffsetOnAxis(ap=d01[:, k, tI:tI + 1], axis=0),
                               in_=h2h[hb_][:], in_offset=None))
            ks.barrier()
        if stop_after == 4:
            return _finish(nc, ks, [])

        with ExitStack() as ph:
            wblk = [sbt(ph, "wblk%d" % i, [128, WROW], BF16) for i in range(2)]; B_wb = [Buf(), Buf()]
            xsb = [sbt(ph, "xsb%d" % i, [128, 2, D], BF16) for i in range(2)]; B_xsb = [Buf(), Buf()]
            xsT = [sbt(ph, "xsT%d" % i, [128, KC, BLK], BF16) for i in range(2)]; B_xsT = [Buf(), Buf()]
            sg = [sbt(ph, "sg%d" % i, [128, BLK], F32) for i in range(2)]; B_sg = [Buf(), Buf()]
            hid = [sbt(ph, "hid%d" % i, [128, 4, BLK], BF16) for i in range(2)]; B_hid = [Buf(), Buf()]
            yo = [sbt(ph, "yo%d" % i, [128, D], BF16) for i in range(2)]; B_yo = [Buf(), Buf()]
            P_x = [pst(ph, "P_x%d" % i, [128, 8, 128], BF16) for i in range(2)]; B_Px = [PB("P_x0"), PB("P_x1")]
            P_g = [pst(ph, "P_g%d" % i, [128, BLK]) for i in range(2)]; B_Pg = [PB("P_g0"), PB("P_g1")]
            P_u = [pst(ph, "P_u%d" % i, [128, BLK]) for i in range(2)]; B_Pu = [PB("P_u0"), PB("P_u1")]
            P_d = [pst(ph, "P_d%d" % i, [128, 512]) for i in range(2)]; B_Pd = [PB("P_d0"), PB("P_d1")]
            px = 0; pq = 0; pd = 0; sgi = 0; yi = 0
            for b in range(NBLK):
                wb = b % 2
                wv = wblk[wb]
                ks.dma("pool", None, None, reads=[B_rs, B_wsc], writes=[B_wb[wb]],
                       fn=lambda e, b=b, wv=wv: e.indirect_dma_start(
                           out=wv[:], out_offset=None, in_=wsc[:, :],
                           in_offset=bass.IndirectOffsetOnAxis(ap=widx[:, b:b + 1], axis=0)))
                ks.dma("sp", xsb[wb][:], xsd[b * BLK:(b + 1) * BLK, :].rearrange("(t p) d -> p t d", p=128),
                       reads=[B_xsd], writes=[B_xsb[wb]])
                for t in range(2):
                    for k8 in range(2):
                        q_ = px % 2; px += 1
                        for kk in range(8):
                            kc = k8 * 8 + kk
                            tr(P_x[q_][:, kk, :], xsb[wb][:, t, kc * 128:(kc + 1) * 128], ident_h[:], [B_xsb[wb], B_ident], [B_Px[q_]])
                        if (px % 2) == 0:
                            cp("act", xsT[wb][:, k8 * 8:(k8 + 1) * 8, t * 128:(t + 1) * 128], P_x[q_][:, :, :], [B_Px[q_]], [B_xsT[wb]])
                        else:
                            cp("dve", xsT[wb][:, k8 * 8:(k8 + 1) * 8, t * 128:(t + 1) * 128], P_x[q_][:, :, :], [B_Px[q_]], [B_xsT[wb]])
                wgv = wv[:, 0:8192].rearrange("p (kc n) -> p kc n", n=512)
                wuv = wv[:, 8192:16384].rearrange("p (kc n) -> p kc n", n=512)
                wdv = wv[:, 16384:24576].rearrange("p (hc n) -> p hc n", n=D)
                hb = b % 2
                for hc in range(4):
                    q_ = pq % 2; pq += 1
                    for kc in range(KC):
                        mm(P_g[q_][:], wgv[:, kc, hc * 128:(hc + 1) * 128], xsT[wb][:, kc, :], kc == 0, kc == KC - 1,
                           [B_wb[wb], B_xsT[wb]], [B_Pg[q_]])
                    for kc in range(KC):
                        mm(P_u[q_][:], wuv[:, kc, hc * 128:(hc + 1) * 128], xsT[wb][:, kc, :], kc == 0, kc == KC - 1,
                           [B_wb[wb], B_xsT[wb]], [B_Pu[q_]])
                    s_ = sgi % 2; sgi += 1
                    act(sg[s_][:], P_g[q_][:], AF.Silu, [B_Pg[q_]], [B_sg[s_]])
                    tt("dve", hid[hb][:, hc, :], P_u[q_][:], sg[s_][:], ALU.mult, [B_sg[s_], B_Pu[q_]], [B_hid[hb]])
                for t in range(2):
                    y_ = yi % 2; yi += 1
                    for cg in range(4):
                        p_ = pd % 2; pd += 1
                        for hc in range(4):
                            mm(P_d[p_][:], hid[hb][:, hc, t * 128:(t + 1) * 128], wdv[:, hc, cg * 512:(cg + 1) * 512],
                               hc == 0, hc == 3, [B_hid[hb], B_wb[wb]], [B_Pd[p_]])
                        if cg % 2 == 0:
                            cp("act", yo[y_][:, cg * 512:(cg + 1) * 512], P_d[p_][:], [B_Pd[p_]], [B_yo[y_]])
                        else:
                            cp("dve", yo[y_][:, cg * 512:(cg + 1) * 512], P_d[p_][:], [B_Pd[p_]], [B_yo[y_]])
                    r0 = b * BLK + t * 128
                    ks.dma("sp", ybd[r0:r0 + 128, :], yo[y_][:], reads=[B_yo[y_]], writes=[B_ybd])
            ks.barrier()
        if stop_after == 5:
            return _finish(nc, ks, [])
        with ExitStack() as ph:
            B_bc = Buf()
            g2p = bcast_load(ph, "g2p", modrow[0:1, 5 * D:6 * D], B_bc, True)
            l2g = bcast_load(ph, "l2g", ln2_g[0:1, :], B_bc)
            l2b = bcast_load(ph, "l2b", ln2_b[0:1, :], B_bc)
            rr = [[sbt(ph, "rr%d_%d" % (i, k), [128, D], BF16) for k in range(2)] for i in range(2)]
            B_rr = [Buf(), Buf()]
            ya_ = [sbt(ph, "ya%d" % i, [128, D], F32) for i in range(2)]; B_ya = [Buf(), Buf()]
            x1t = [sbt(ph, "x1t%d" % i, [128, D], F32) for i in range(2)]; B_x1t = [Buf(), Buf()]
            st5 = sbt(ph, "st5", [128, 16], F32); B_st5 = Buf()
            junk5 = sbt(ph, "junk5", [128, D], BF16); B_j5 = Buf()
            for tI in range(NT):
                i = tI % 2
                for k in range(2):
                    ks.dma("pool", None, None, reads=[B_ybd, B_rs], writes=[B_rr[i]],
                           fn=lambda e, k=k, tI=tI, i=i: e.indirect_dma_start(
                               out=rr[i][k][:], out_offset=None, in_=ybd[:, :],
                               in_offset=bass.IndirectOffsetOnAxis(ap=d01[:, k, tI:tI + 1], axis=0)))
                ks.dma("sp", x1t[i][:], x1d[tI * 128:(tI + 1) * 128, :], reads=[B_x1d[tI]], writes=[B_x1t[i]])
                ya = ya_[i][:]; By = B_ya[i]
                ts("dve", ya, rr[i][0][:], w12[:, 0, tI:tI + 1], ALU.mult, [B_rr[i], B_rs], [By])
                stt(ya, rr[i][1][:], w12[:, 1, tI:tI + 1], ya, ALU.mult, ALU.add, [B_rr[i], B_rs, By], [By])
                tt("dve", ya, ya, g2p[:], ALU.mult, [By, B_bc], [By])
                stt(ya, x1t[i][:], ALPHA, ya, ALU.mult, ALU.add, [B_x1t[i], By], [By])
                ks.op("dve", lambda e, ya=ya: e.reduce_sum(out=st5[:, 0:1], in_=ya, axis=AX.X), reads=[By], writes=[B_st5])
                act(junk5[:], ya, AF.Square, [By], [B_j5, B_st5], accum_out=st5[:, 1:2])
                ts("dve", st5[:, 2:3], st5[:, 0:1], 1.0 / D, ALU.mult, [B_st5], [B_st5])
                tt("dve", st5[:, 3:4], st5[:, 2:3], st5[:, 2:3], ALU.mult, [B_st5], [B_st5])
                stt(st5[:, 4:5], st5[:, 1:2], 1.0 / D, st5[:, 3:4], ALU.mult, ALU.subtract, [B_st5], [B_st5])
                act(st5[:, 5:6], st5[:, 4:5], AF.Sqrt, [B_st5, B_c], [B_st5], bias=ce5, scale=1.0)
                ks.op("dve", lambda e: e.reciprocal(st5[:, 6:7], st5[:, 5:6]), reads=[B_st5], writes=[B_st5])
                ts("dve", ya, ya, st5[:, 2:3], ALU.subtract, [By, B_st5], [By], s2=st5[:, 6:7], op1=ALU.mult)
                tt("dve", ya, ya, l2g[:], ALU.mult, [By, B_bc], [By])
                tt("dve", ya, ya, l2b[:], ALU.add, [By, B_bc], [By])
                ks.dma("sp", out[tI * 128:(tI + 1) * 128, :], ya, reads=[By], writes=[Buf()])
            ks.barrier()

        return _finish(nc, ks, [])


def _finish(nc, ks, bufs):
    for key, val in ks.cnt.items():
        if key.startswith("d_") and val > 0:
            ks._wait("sp", (key, val))
    print("kernel build: insts=%d waits=%d" % (ks.n_inst, ks.n_wait))
    return nc


def _col_perm():
    idx = list(range(0, 4096)) + list(range(4112, 4112 + 3072)) + list(range(4096, 4112)) + list(range(7184, 7192))
    return np.asarray(idx)


def make_in_maps(inputs, n_cores=8):
    f = lambda a: np.ascontiguousarray(np.asarray(a, dtype=np.float32))
    x = np.asarray(inputs["x"]); c = np.asarray(inputs["c"])
    shared = {
        "w_ada": f(inputs["w_ada"][0]),
        "b_ada": f(inputs["b_ada"][0][None, :]),
        "w_in": f(inputs["w_in"][0][:, _col_perm()]),
        "convw": f(np.asarray(inputs["dn_conv_w"][0]).reshape(4, 24, 128).transpose(2, 1, 0)),
        "a_log": f(inputs["dn_a_log"][0][None, :]),
        "dt_bias": f(inputs["dn_dt_bias"][0][None, :]),
        "f_bias": f(inputs["fox_f_bias"][0][None, :]),
        "norm_w": f(np.asarray(inputs["dn_norm_w"][0])[:, None]),
        "w_out": f(inputs["w_out"][0]),
        "ln1_g": f(inputs["ln1_g"][0][None, :]), "ln1_b": f(inputs["ln1_b"][0][None, :]),
        "ln2_g": f(inputs["ln2_g"][0][None, :]), "ln2_b": f(inputs["ln2_b"][0][None, :]),
        "w_r": f(np.concatenate([np.asarray(inputs["w_router_group"][0])] +
                                [np.asarray(inputs["w_router_expert"][0][g]) for g in range(4)], axis=1)),
        "b_r": f(np.concatenate([np.asarray(inputs["b_router_group"][0])] +
                                [np.asarray(inputs["b_router_expert"][0][g]) for g in range(4)])[None, :]),
    }
    w_gate = np.asarray(inputs["w_gate"][0], dtype=np.float32).reshape(32, KC, 128, 512).transpose(0, 2, 1, 3).reshape(32, 128, KC * 512)
    w_up = np.asarray(inputs["w_up"][0], dtype=np.float32).reshape(32, KC, 128, 512).transpose(0, 2, 1, 3).reshape(32, 128, KC * 512)
    w_down = np.asarray(inputs["w_down"][0], dtype=np.float32).reshape(32, 4, 128, D).transpose(0, 2, 1, 3).reshape(32, 128, 4 * D)
    shared["w_moe"] = np.ascontiguousarray(np.concatenate([w_gate, w_up, w_down], axis=2))
    maps = []
    for b in range(n_cores):
        m = dict(shared)
        m["x"] = f(x[b])
        m["xT"] = f(x[b].T)
        m["ccol"] = f(c[b].reshape(KC, 128).T)
        maps.append(m)
    return maps


def kernel(**inputs):
    nc = build_nc()
    maps = make_in_maps(inputs)
    res = run_bass_kernel_spmd(nc, maps, core_ids=list(range(8)))
    return np.stack([np.asarray(r["out"], dtype=np.float32) for r in res.results], axis=0)
```

```python
import os
import numpy as np
from contextlib import ExitStack
import concourse.bass as bass
import concourse.mybir as mybir
from concourse.bass_utils import run_bass_kernel_spmd

F32 = mybir.dt.float32
BF16 = mybir.dt.bfloat16
I32 = mybir.dt.int32
AF = mybir.ActivationFunctionType
ALU = mybir.AluOpType
AX = mybir.AxisListType

D = 2048
S = 4096
KC = D // 128
NT = S // 128
NG = S // 512
NH = 8
DIN = 7192
ALPHA = 2.0 ** 0.25


class Bank:
    __slots__ = ("acc",)

    def __init__(self):
        self.acc = {}


class Buf:
    __slots__ = ("name", "w", "r", "bank")

    def __init__(self, name="", bank=None):
        self.name = name
        self.w = None
        self.r = {}
        self.bank = bank


class KS:
    ENG = ("pe", "act", "dve", "pool", "sp")

    def __init__(self, nc, stack, n_dsem=12, same_eng_sync=True):
        self.nc = nc
        self.eng = {"pe": nc.tensor, "act": nc.scalar, "dve": nc.vector,
                    "pool": nc.gpsimd, "sp": nc.sync}
        self.same_eng_sync = same_eng_sync
        self.sems = {}
        self.cnt = {}
        for e in self.ENG:
            self.sems[e] = stack.enter_context(nc.semaphore("c_" + e))
            self.cnt[e] = 0
        self.dq = {}
        for q in ("sp", "pool", "act"):
            lst = []
            for i in range(n_dsem):
                key = "d_%s_%d" % (q, i)
                self.sems[key] = stack.enter_context(nc.semaphore(key))
                self.cnt[key] = 0
                lst.append(key)
            self.dq[q] = [lst, 0]
        self.waited = {e: {} for e in self.ENG}
        self.n_wait = 0
        self.n_inst = 0

    def _wait(self, e, dep):
        if dep is None:
            return
        key, val = dep
        if key == e and (e == "pe" or not self.same_eng_sync):
            return
        if self.waited[e].get(key, 0) >= val:
            return
        self.eng[e].wait_ge(self.sems[key], val)
        self.waited[e][key] = val
        self.n_wait += 1

    def _deps(self, e, reads, writes):
        for b in reads:
            self._wait(e, b.w)
        for b in writes:
            self._wait(e, b.w)
            for k, v in b.r.items():
                self._wait(e, (k, v))
        for b in list(reads) + list(writes):
            if b.bank is not None:
                for k, v in b.bank.acc.items():
                    if k != e:
                        self._wait(e, (k, v))

    def _mark(self, tag, reads, writes):
        key, val = tag
        for b in reads:
            if b.r.get(key, 0) < val:
                b.r[key] = val
        for b in writes:
            b.w = tag
            b.r = {}
        for b in list(reads) + list(writes):
            if b.bank is not None:
                b.bank.acc[key] = val

    def op(self, e, fn, reads=(), writes=()):
        self._deps(e, reads, writes)
        inst = fn(self.eng[e])
        self.cnt[e] += 1
        inst.then_inc(self.sems[e], 1)
        self._mark((e, self.cnt[e]), reads, writes)
        self.n_inst += 1
        return inst

    def dma(self, q, out, in_, reads=(), writes=(), fn=None, **kw):
        lst, idx = self.dq[q]
        key = lst[idx % len(lst)]
        self.dq[q][1] = idx + 1
        if self.cnt[key] > 0:
            self._wait(q, (key, self.cnt[key]))
        self._deps(q, reads, writes)
        if fn is not None:
            inst = fn(self.eng[q])
        else:
            inst = self.eng[q].dma_start(out=out, in_=in_, **kw)
        self.cnt[key] += 16
        inst.then_inc(self.sems[key], 16)
        self._mark((key, self.cnt[key]), reads, writes)
        self.n_inst += 1
        return inst

    def wait_all(self, e, bufs):
        for b in bufs:
            self._wait(e, b.w)

    def barrier(self):
        for e in self.ENG:
            for key, val in self.cnt.items():
                if val > 0 and key != e:
                    self._wait(e, (key, val))
            if e != "pe" and self.cnt[e] > 0 and self.same_eng_sync:
                self._wait(e, (e, self.cnt[e]))


def build_nc(debug=(), stop_after=None):
    nc = bass.Bass("TRN2", target_bir_lowering=False)
    dbg = set(debug)

    def din(name, shape, dt=F32):
        return nc.dram_tensor(name, list(shape), dt, kind="ExternalInput").ap()

    def dscr(name, shape, dt=F32):
        kind = "ExternalOutput" if name in dbg else "Internal"
        return nc.dram_tensor(name, list(shape), dt, kind=kind).ap()

    xT = din("xT", [D, S])
    xtok = din("x", [S, D])
    ccol = din("ccol", [128, KC])
    w_ada = din("w_ada", [D, 6 * D])
    b_ada = din("b_ada", [1, 6 * D])
    w_in = din("w_in", [D, DIN])
    convw = din("convw", [128, 24, 4])
    a_log = din("a_log", [1, NH])
    dt_bias = din("dt_bias", [1, NH])
    f_bias = din("f_bias", [1, NH])
    norm_w = din("norm_w", [128, 1])
    w_out = din("w_out", [D, D])
    ln1_g = din("ln1_g", [1, D]); ln1_b = din("ln1_b", [1, D])
    ln2_g = din("ln2_g", [1, D]); ln2_b = din("ln2_b", [1, D])
    w_r = din("w_r", [D, 36]); b_r = din("b_r", [1, 36])
    out = nc.dram_tensor("out", [S, D], F32, kind="ExternalOutput").ap()

    modrow = dscr("modrow", [1, 6 * D])
    projT = dscr("projT", [48, 128, S], BF16)
    vtok = dscr("vtok", [S, 1024], BF16)
    gat = dscr("gat", [S, 24], F32)

    with ExitStack() as st:
        ks = KS(nc, st, same_eng_sync=(os.environ.get("SES", "1") == "1"))
        cst = ExitStack(); st.enter_context(cst)

        def sbt(stack, name, shape, dt):
            return stack.enter_context(nc.sbuf_tensor(name, list(shape), dt))

        BANKS = {}

        def pst(stack, name, shape, dt=F32):
            full = 512 if dt == F32 else 1024
            t_ = stack.enter_context(nc.psum_tensor(name, [128, full], dt))
            BANKS[name] = Bank()
            P = shape[0]
            n = 1
            for d_ in shape[1:]:
                n *= d_
            v = t_[0:P, 0:n]
            if len(shape) == 3:
                v = v.rearrange("p (a b) -> p a b", b=shape[2])
            return v

        def PB(name):
            return Buf(name, BANKS[name])

        ident_f = sbt(cst, "ident_f", [128, 128], F32); B_ident = Buf("ident")
        ident_h = sbt(cst, "ident_h", [128, 128], BF16)
        ks.op("pool", lambda e: e.memset(ident_f[:], 1.0), writes=[B_ident])
        ks.op("pool", lambda e: e.affine_select(out=ident_f[:], in_=ident_f[:], pattern=[[-1, 128]],
                                                compare_op=ALU.is_equal, fill=0.0, base=0, channel_multiplier=1),
              reads=[B_ident], writes=[B_ident])
        ks.op("pool", lambda e: e.tensor_copy(ident_h[:], ident_f[:]), reads=[B_ident], writes=[B_ident])
        sc1c = sbt(cst, "sc1c", [128, KC], F32)
        sh1c = sbt(cst, "sh1c", [128, KC], F32)
        B_mc = Buf("modcols")
        B_modrow = Buf("modrow")

        with ExitStack() as ph:
            cc = sbt(ph, "cc", [128, KC], F32); B_cc = Buf()
            sc = sbt(ph, "sc", [128, KC], F32); B_sc = Buf()
            brow = sbt(ph, "brow", [1, 6 * D], F32); B_brow = Buf()
            mrow = sbt(ph, "mrow", [1, 6 * D], F32); B_mrow = Buf()
            wa = [sbt(ph, "wa%d" % i, [128, KC, 512], F32) for i in range(2)]; B_wa = [Buf(), Buf()]
            pm = [pst(ph, "pm%d" % i, [1, 512]) for i in range(2)]; B_pm = [PB("pm0"), PB("pm1")]
            ks.dma("sp", cc[:], ccol[:, :], writes=[B_cc])
            ks.dma("sp", brow[:], b_ada[:, :], writes=[B_brow])
            ks.op("act", lambda e: e.activation(out=sc[:], in_=cc[:], func=AF.Silu), reads=[B_cc], writes=[B_sc])
            w_ada_v = w_ada.rearrange("(kc p) n -> p kc n", p=128)
            NCG = 6 * D // 512
            for cg in range(NCG):
                i = cg % 2
                ks.dma("sp", wa[i][:], w_ada_v[:, :, cg * 512:(cg + 1) * 512], writes=[B_wa[i]])
                for kc in range(KC):
                    ks.op("pe", lambda e, kc=kc, i=i: e.matmul(pm[i][:], lhsT=sc[:, kc:kc + 1], rhs=wa[i][:, kc, :],
                                                             start=(kc == 0), stop=(kc == KC - 1)),
                          reads=[B_sc, B_wa[i]], writes=[B_pm[i]])
                ks.op("dve", lambda e, cg=cg, i=i: e.tensor_tensor(out=mrow[:, cg * 512:(cg + 1) * 512], in0=pm[i][:],
                                                                   in1=brow[:, cg * 512:(cg + 1) * 512], op=ALU.add),
                      reads=[B_pm[i], B_brow], writes=[B_mrow])
            ks.dma("sp", modrow[:, :], mrow[:], reads=[B_mrow], writes=[B_modrow])
            t16 = sbt(ph, "t16", [KC, 2, 128], F32); B_t16 = Buf()
            ks.dma("sp", t16[:, 0, :], modrow[0, 0:D].rearrange("(kc p) -> kc p", p=128), reads=[B_modrow], writes=[B_t16])
            ks.dma("sp", t16[:, 1, :], modrow[0, D:2 * D].rearrange("(kc p) -> kc p", p=128), reads=[B_modrow], writes=[B_t16])
            pt = pst(ph, "pt", [128, 2, KC]); B_pt = PB("pt")
            for j in range(2):
                ks.op("pe", lambda e, j=j: e.transpose(pt[:, j, :], t16[:, j, :], ident_f[0:KC, 0:KC]),
                      reads=[B_t16, B_ident], writes=[B_pt])
            ks.op("dve", lambda e: e.tensor_copy(sh1c[:], pt[:, 0, :]), reads=[B_pt], writes=[B_mc])
            ks.op("dve", lambda e: e.tensor_scalar(out=sc1c[:], in0=pt[:, 1, :], scalar1=1.0, scalar2=None, op0=ALU.add),
                  reads=[B_pt], writes=[B_mc])
            ks.barrier()
        if stop_after == 0:
            return _finish(nc, ks, [B_modrow])

        B_projT = [Buf("projT%d" % j) for j in range(48)]
        B_vtok = Buf("vtok"); B_gat = Buf("gat")
        with ExitStack() as ph:
            hT = sbt(ph, "hT", [128, KC, S], BF16); B_hT = [Buf() for _ in range(KC)]
            wf = [sbt(ph, "wf%d" % i, [128, KC, 256], F32) for i in range(2)]; B_wf = [Buf(), Buf()]
            wh = [sbt(ph, "wh%d" % i, [128, KC, 256], BF16) for i in range(2)]; B_wh = [Buf(), Buf()]
            w_in_v = w_in.rearrange("(kc p) n -> p kc n", p=128)
            ks.dma("sp", wf[0][:, :, 0:256], w_in_v[:, :, 0:256], writes=[B_wf[0]])
            ks.op("pool", lambda e: e.tensor_copy(wh[0][:, :, 0:256], wf[0][:, :, 0:256]), reads=[B_wf[0]], writes=[B_wh[0]])
            xsl = [wf[1][:, 2 * i:2 * i + 2, :].rearrange("p a b -> p (a b)") for i in range(8)]
            B_xsl = [Buf() for _ in range(8)]
            B_hTg = [[Buf() for _ in range(NG)] for _ in range(KC)]
            xT_v = xT.rearrange("(kc p) t -> p kc t", p=128)
            n = 0
            for tg in range(NG):
                for kc in range(KC):
                    i = n % 8; n += 1
                    ks.dma("sp", xsl[i], xT_v[:, kc, tg * 512:(tg + 1) * 512], writes=[B_xsl[i]])
                    ks.op("act", lambda e, kc=kc, tg=tg, i=i: e.activation(
                        out=hT[:, kc, tg * 512:(tg + 1) * 512], in_=xsl[i], func=AF.Identity,
                        scale=sc1c[:, kc:kc + 1], bias=sh1c[:, kc:kc + 1]),
                        reads=[B_xsl[i], B_mc], writes=[B_hTg[kc][tg]])
            stg_all = sbt(ph, "stg", [128, 2 * S], BF16)
            stg = [stg_all[:, i * S:(i + 1) * S] for i in range(2)]; B_stg = [Buf(), Buf()]
            vst = stg_all[:, :].rearrange("p (t c) -> p t c", c=256)
            gst = sbt(ph, "gst", [128, NT, 24], F32); B_gst = Buf()
            pp = [pst(ph, "pp%d" % i, [128, 512]) for i in range(4)]; B_pp = [PB("pp%d" % i) for i in range(4)]
            w_in_v = w_in.rearrange("(kc p) n -> p kc n", p=128)
            n_cg = (DIN + 255) // 256
            pi = 0; si = 0; ev = 0
            for cg in range(n_cg):
                i = cg % 2
                c0 = cg * 256
                ncol = min(256, DIN - c0)
                if cg + 1 < n_cg:
                    i1 = (cg + 1) % 2
                    c1 = (cg + 1) * 256
                    ncol1 = min(256, DIN - c1)
                    ks.dma("sp", wf[i1][:, :, 0:ncol1], w_in_v[:, :, c1:c1 + ncol1], writes=[B_wf[i1]] + (B_xsl if cg == 0 else []))
                    ks.op("pool", lambda e, i1=i1, ncol1=ncol1: e.tensor_copy(wh[i1][:, :, 0:ncol1], wf[i1][:, :, 0:ncol1]),
                          reads=[B_wf[i1]], writes=[B_wh[i1]])
                if c0 < 6144:
                    for jb in range(2):
                        j = cg * 2 + jb
                        s_ = si % 2; si += 1
                        for tg in range(NG):
                            p_ = pi % 4; pi += 1
                            for kc in range(KC):
                                ks.op("pe", lambda e, kc=kc, i=i, jb=jb, tg=tg, p_=p_: e.matmul(
                                    pp[p_][:], lhsT=wh[i][:, kc, jb * 128:(jb + 1) * 128],
                                    rhs=hT[:, kc, tg * 512:(tg + 1) * 512], start=(kc == 0), stop=(kc == KC - 1)),
                                    reads=[B_wh[i], B_hTg[kc][tg]], writes=[B_pp[p_]])
                            if ev % 2 == 0:
                                ks.op("act", lambda e, s_=s_, tg=tg, p_=p_: e.activation(
                                    out=stg[s_][:, tg * 512:(tg + 1) * 512], in_=pp[p_][:], func=AF.Copy),
                                    reads=[B_pp[p_]], writes=[B_stg[s_]])
                            else:
                                ks.op("dve", lambda e, s_=s_, tg=tg, p_=p_: e.tensor_copy(
                                    stg[s_][:, tg * 512:(tg + 1) * 512], pp[p_][:]),
                                    reads=[B_pp[p_]], writes=[B_stg[s_]])
                            ev += 1
                        ks.dma("sp", projT[j], stg[s_], reads=[B_stg[s_]], writes=[B_projT[j]])
                else:
                    isg = ncol < 256
                    for tt in range(NT):
                        p_ = pi % 4; pi += 1
                        for kc in range(KC):
                            ks.op("pe", lambda e, kc=kc, i=i, tt=tt, p_=p_, ncol=ncol: e.matmul(
                                pp[p_][:, 0:ncol], lhsT=hT[:, kc, tt * 128:(tt + 1) * 128],
                                rhs=wh[i][:, kc, 0:ncol], start=(kc == 0), stop=(kc == KC - 1)),
                                reads=[B_wh[i], B_hTg[kc][tt // 4]], writes=[B_pp[p_]])
                        if isg:
                            ks.op("dve", lambda e, tt=tt, p_=p_: e.tensor_copy(gst[:, tt, :], pp[p_][:, 0:24]),
                                  reads=[B_pp[p_]], writes=[B_gst])
                        elif ev % 2 == 0:
                            ks.op("act", lambda e, tt=tt, p_=p_: e.activation(out=vst[:, tt, :], in_=pp[p_][:, 0:256], func=AF.Copy),
                                  reads=[B_pp[p_]], writes=B_stg)
                        else:
                            ks.op("dve", lambda e, tt=tt, p_=p_: e.tensor_copy(vst[:, tt, :], pp[p_][:, 0:256]),
                                  reads=[B_pp[p_]], writes=B_stg)
                        ev += 1
                    if isg:
                        ks.dma("sp", gat.rearrange("(tt p) c -> p tt c", p=128), gst[:], reads=[B_gst], writes=[B_gat])
                    else:
                        v0 = c0 - 6144
                        ks.dma("sp", vtok[:, v0:v0 + 256].rearrange("(tt p) c -> p tt c", p=128), vst,
                               reads=B_stg, writes=[B_vtok])
            ks.barrier()
        if stop_after == 1:
            return _finish(nc, ks, B_projT + [B_vtok, B_gat])

        def act(out, in_, func, R, W, **kw):
            return ks.op("act", lambda e: e.activation(out=out, in_=in_, func=func, **kw), reads=R, writes=W)

        def ts(eng, out, in0, s1, op0, R, W, s2=None, op1=None):
            kw = dict(out=out, in0=in0, scalar1=s1, scalar2=s2, op0=op0)
            if op1 is not None:
                kw["op1"] = op1
            return ks.op(eng, lambda e: e.tensor_scalar(**kw), reads=R, writes=W)

        def tt(eng, out, in0, in1, op, R, W):
            return ks.op(eng, lambda e: e.tensor_tensor(out=out, in0=in0, in1=in1, op=op), reads=R, writes=W)

        def stt(out, in0, scalar, in1, op0, op1, R, W):
            return ks.op("dve", lambda e: e.scalar_tensor_tensor(out=out, in0=in0, scalar=scalar, in1=in1, op0=op0, op1=op1),
                         reads=R, writes=W)

        def mm(out, lhsT, rhs, start, stop, R, W):
            return ks.op("pe", lambda e: e.matmul(out, lhsT=lhsT, rhs=rhs, start=start, stop=stop), reads=R, writes=W)

        def tr(out, in_, idn, R, W):
            return ks.op("pe", lambda e: e.transpose(out, in_, idn), reads=R, writes=W)

        def cp(eng, out, in_, R, W):
            if eng == "act":
                return act(out, in_, AF.Copy, R, W)
            return ks.op(eng, lambda e: e.tensor_copy(out, in_), reads=R, writes=W)

        def asel(out, in_, cmp, fill, R, W):
            if cmp == "le":
                pat, cm, base = [[1, 128]], -1, 0
            else:
                pat, cm, base = [[-1, 128]], 1, -1
            return ks.op("pool", lambda e: e.affine_select(out=out, in_=in_, pattern=pat, compare_op=ALU.is_ge,
                                                           fill=fill, base=base, channel_multiplier=cm), reads=R, writes=W)

        B_c = Buf("consts")
        ones_f = sbt(cst, "ones_f", [128, 128], F32); ones_h = sbt(cst, "ones_h", [128, 128], BF16)
        UT_f = sbt(cst, "UT_f", [128, 128], F32)
        maskL = sbt(cst, "maskL", [128, 128], F32); maskU = sbt(cst, "maskU", [128, 128], F32)
        ccols = sbt(cst, "ccols", [128, 4], F32)
        ks.op("pool", lambda e: e.memset(ones_f[:], 1.0), writes=[B_c])
        ks.op("pool", lambda e: e.memset(ones_h[:], 1.0), writes=[B_c])
        ks.op("pool", lambda e: e.memset(UT_f[:], 1.0), writes=[B_c])
        asel(UT_f[:], UT_f[:], "le", 0.0, [B_c], [B_c])
        ks.op("pool", lambda e: e.memset(maskL[:], 0.0), writes=[B_c])
        asel(maskL[:], maskL[:], "gt", 1.0e4, [B_c], [B_c])
        ks.op("pool", lambda e: e.memset(maskU[:], 0.0), writes=[B_c])
        asel(maskU[:], maskU[:], "le", -1.0e4, [B_c], [B_c])
        ks.op("pool", lambda e: e.memset(ccols[:, 0:1], 0.0), writes=[B_c])
        ks.op("pool", lambda e: e.memset(ccols[:, 1:2], 1.0), writes=[B_c])
        ks.op("pool", lambda e: e.memset(ccols[:, 2:3], 1.0e-6), writes=[B_c])
        ks.op("pool", lambda e: e.memset(ccols[:, 3:4], 1.0e-5), writes=[B_c])
        c0_, c1_, ce6, ce5 = ccols[:, 0:1], ccols[:, 1:2], ccols[:, 2:3], ccols[:, 3:4]
        STOPG = int(os.environ.get("STOPG", "0"))
        if STOPG == 1:
            return _finish(nc, ks, [])

        oT = dscr("oT", [16, 128, S], BF16)
        B_oT = [[Buf() for _ in range(NG)] for _ in range(16)]

        gst_ = ExitStack(); st.enter_context(gst_)
        B_g = Buf("gates")
        gt = sbt(gst_, "gt", [128, NT, 24], F32)
        prm = sbt(gst_, "prm", [128, 3, NH], F32)
        G8 = lambda nm: sbt(gst_, nm, [128, NH, NT], F32)
        beta = G8("beta"); nbeta = G8("nbeta"); gl = G8("gl"); Gc = G8("Gc"); nbg = G8("nbg")
        kds = G8("kds"); eGl = G8("eGl"); tmpg = G8("tmpg"); lf = G8("lf"); Fc = G8("Fc"); nF = G8("nF")
        nea = sbt(gst_, "nea", [128, NH], F32); nfb = sbt(gst_, "nfb", [128, NH], F32)
        ks.dma("sp", gt[:], gat.rearrange("(tt p) c -> p tt c", p=128), reads=[B_gat], writes=[B_g])
        ks.dma("sp", prm[:, 0, :], a_log[0:1, :].broadcast_to([128, NH]), writes=[B_g])
        ks.dma("sp", prm[:, 1, :], dt_bias[0:1, :].broadcast_to([128, NH]), writes=[B_g])
        ks.dma("sp", prm[:, 2, :], f_bias[0:1, :].broadcast_to([128, NH]), writes=[B_g])
        RG = [B_g, B_c]
        act(nea[:], prm[:, 0, :], AF.Exp, RG, [B_g])
        ts("dve", nea[:], nea[:], -1.0, ALU.mult, RG, [B_g])
        ts("dve", nfb[:], prm[:, 2, :], -1.0, ALU.mult, RG, [B_g])
        if STOPG == 2:
            return _finish(nc, ks, [])
        for h in range(NH):
            act(beta[:, h, :], gt[:, :, h], AF.Sigmoid, RG, [B_g])
        for h in range(NH):
            act(tmpg[:, h, :], gt[:, :, 8 + h], AF.Exp, RG, [B_g], bias=prm[:, 1, h:h + 1], scale=1.0)
            act(lf[:, h, :], gt[:, :, 16 + h], AF.Exp, RG, [B_g], bias=nfb[:, h:h + 1], scale=-1.0)
        gflat = lambda t_: t_[:, :, :].rearrange("p h t -> p (h t)")
        act(gflat(tmpg), gflat(tmpg), AF.Ln, RG, [B_g], bias=c1_, scale=1.0)
        act(gflat(lf), gflat(lf), AF.Ln, RG, [B_g], bias=c1_, scale=1.0)
        ts("dve", gflat(lf), gflat(lf), -1.0, ALU.mult, RG, [B_g])
        for h in range(NH):
            ts("dve", gl[:, h, :], tmpg[:, h, :], nea[:, h:h + 1], ALU.mult, RG, [B_g])
        ts("dve", gflat(nbeta), gflat(beta), -1.0, ALU.mult, RG, [B_g])
        if STOPG == 3:
            return _finish(nc, ks, [])
        with ExitStack() as gp:
            pg = pst(gp, "pg", [128, 2, NH * NT]); B_pg = PB("pg")
            mm(pg[:, 0, :], UT_f[:], gflat(gl), True, True, RG, [B_pg])
            mm(pg[:, 1, :], ones_f[:], gflat(gl), True, True, RG, [B_pg])
            cp("dve", gflat(Gc), pg[:, 0, :], [B_pg], [B_g])
            act(gflat(eGl), pg[:, 1, :], AF.Exp, [B_pg, B_c], [B_g])
            tt("dve", gflat(kds), pg[:, 1, :], gflat(Gc), ALU.subtract, [B_pg, B_g], [B_g])
            act(gflat(kds), gflat(kds), AF.Exp, RG, [B_g])
            act(gflat(tmpg), gflat(Gc), AF.Exp, RG, [B_g])
            tt("dve", gflat(nbg), gflat(tmpg), gflat(nbeta), ALU.mult, RG, [B_g])
            if STOPG == 4:
                return _finish(nc, ks, [])
            mm(pg[:, 0, :], UT_f[:], gflat(lf), True, True, RG, [B_pg])
            mm(pg[:, 1, :], ones_f[:], gflat(lf), True, True, RG, [B_pg])
            cp("dve", gflat(Fc), pg[:, 0, :], [B_pg], [B_g])
            cp("dve", gflat(nF), pg[:, 1, :], [B_pg], [B_g])
            for h in range(NH):
                ks.op("dve", lambda e, h=h: e.tensor_tensor_scan(out=tmpg[:, h, :], data0=ones_f[:, 0:NT], data1=nF[:, h, :],
                                                                 initial=0.0, op0=ALU.mult, op1=ALU.add), reads=RG, writes=[B_g])
            if STOPG == 5:
                return _finish(nc, ks, [])
            tt("dve", gflat(tmpg), gflat(tmpg), gflat(nF), ALU.subtract, RG, [B_g])
            tt("dve", gflat(Fc), gflat(Fc), gflat(tmpg), ALU.add, RG, [B_g])
            ts("dve", gflat(nF), gflat(Fc), -1.0, ALU.mult, RG, [B_g])
            if STOPG == 6:
                return _finish(nc, ks, [])
            ks.barrier()

        if stop_after == 1.5:
            return _finish(nc, ks, [])
        with ExitStack() as ph:
            raw = [[sbt(ph, "raw%d_%d" % (p_, i), [128, 515], BF16) for i in range(3)] for p_ in range(2)]
            B_raw = [[Buf() for i in range(3)] for p_ in range(2)]
            cw = sbt(ph, "cw", [128, 24, 4], F32); B_cw = Buf()
            nw = sbt(ph, "nw", [128, 1], F32)
            ks.dma("sp", cw[:], convw[:, :, :], writes=[B_cw])
            ks.dma("sp", nw[:], norm_w[:, :], writes=[B_cw])
            cacc = [sbt(ph, "cacc%d" % i, [128, 512], F32) for i in range(3)]; B_cacc = [Buf() for _ in range(3)]
            sqb = [sbt(ph, "sqb%d" % i, [128, 512], BF16) for i in range(2)]; B_sqb = [Buf(), Buf()]
            rsb = [sbt(ph, "rsb%d" % i, [128, 512], F32) for i in range(2)]; B_rsb = [Buf(), Buf()]
            qnG = sbt(ph, "qnG", [128, 2, NH, 512], BF16); knG = sbt(ph, "knG", [128, 2, NH, 512], BF16)
            vTG = sbt(ph, "vTG", [128, 2, NH, 512], BF16)
            B_qkv = [[Buf() for _ in range(NH)] for _ in range(2)]
            kdec = sbt(ph, "kdec", [128, NH, 4, 128], BF16); vb = sbt(ph, "vb", [128, NH, 4, 128], BF16)
            TT = sbt(ph, "TT", [128, NH, 4, 128], BF16); qkT = sbt(ph, "qkT", [128, NH, 4, 128], BF16)
            qg = sbt(ph, "qg", [128, NH, 4, 128], BF16)
            B_ck = [[Buf() for _ in range(4)] for _ in range(NH)]
            B_TT = [[Buf() for _ in range(4)] for _ in range(NH)]
            oTs = sbt(ph, "oTs", [128, NH, 512], F32); B_oTs = [[Buf() for _ in range(4)] for _ in range(NH)]
            S_f = sbt(ph, "S_f", [128, NH, 128], F32); S_h = sbt(ph, "S_h", [128, NH, 128], BF16); B_S = [Buf() for _ in range(NH)]
            NB = 2
            W4 = 4
            mk = lambda nm, dt_: [[sbt(ph, "%s%d_%d" % (nm, i, j), [128, 128], dt_) for j in range(W4)] for i in range(NB)]
            diagG4 = [sbt(ph, "diagG4_%d" % i, [128, W4, 128], F32) for i in range(NB)]
            diagG = [[diagG4[i][:, j, :] for j in range(W4)] for i in range(NB)]
            mL = mk("mL", F32); mU = mk("mU", F32); eGb = mk("eGb", F32)
            Pn = [mk("PnA", BF16), mk("PnB", BF16)]; Pt = [mk("PtA", BF16), mk("PtB", BF16)]; XT = [mk("XTA", BF16), mk("XTB", BF16)]
            B_tmp = [[Buf() for _ in range(W4)] for _ in range(NB)]
            rbuf = sbt(ph, "rbuf", [128, NH, 128], BF16); B_r = [Buf() for _ in range(NH)]
            vnew = sbt(ph, "vnew", [128, NH, 128], BF16); B_vn = [Buf() for _ in range(NH)]
            zb = [sbt(ph, "zb%d" % i, [128, 512], BF16) for i in range(2)]; B_zb = [Buf(), Buf()]
            zs = [sbt(ph, "zs%d" % i, [128, 512], F32) for i in range(2)]
            on_ = [sbt(ph, "on%d" % i, [128, 512], F32) for i in range(2)]
            og_ = [sbt(ph, "og%d" % i, [128, 512], BF16) for i in range(2)]; B_og = [Buf(), Buf()]
            P_L = [pst(ph, "P_L%d" % i, [128, 4, 128]) for i in range(3)]
            B_L = [[PB("P_L%d" % i) for _ in range(W4)] for i in range(3)]
            P_T = pst(ph, "P_T", [128, 8, 128], BF16); B_PT = [PB("P_T") for _ in range(8)]
            P_Sa = pst(ph, "P_Sa", [128, 4, 128]); B_Sa = [PB("P_Sa") for _ in range(4)]
            P_Sb = pst(ph, "P_Sb", [128, 4, 128]); B_Sb = [PB("P_Sb") for _ in range(4)]
            P_N = pst(ph, "P_N", [128, 512]); B_PN = PB("P_N")
            cnt = {"sq": 0, "z": 0, "w": 0}
            for h in range(NH):
                ks.op("dve", lambda e, h=h: e.memset(S_f[:, h, :], 0.0), writes=[B_S[h]])
                ks.op("dve", lambda e, h=h: e.memset(S_h[:, h, :], 0.0), writes=[B_S[h]])

            def gprep(h, tg):
                par = tg % 2
                t0 = tg * 512
                srcs = [projT[h], projT[8 + h], projT[16 + h]]
                dsts = [qnG[:, par, h, :], knG[:, par, h, :], vTG[:, par, h, :]]
                Bd = B_qkv[par][h]
                for i in range(3):
                    rb = raw[h % 2][i]; Br = B_raw[h % 2][i]
                    if tg == 0:
                        ks.op("pool", lambda e, rb=rb: e.memset(rb[:, 0:3], 0.0), writes=[Br])
                        ks.dma("sp", rb[:, 3:515], srcs[i][:, 0:512], reads=[B_projT[[h, 8 + h, 16 + h][i]]], writes=[Br])
                    else:
                        ks.dma("sp", rb[:, 0:515], srcs[i][:, t0 - 3:t0 + 512], reads=[B_projT[[h, 8 + h, 16 + h][i]]], writes=[Br])
                    blk = i * 8 + h
                    ca = cacc[i]; Bc = B_cacc[i]
                    ts("dve", ca[:], rb[:, 3:515], cw[:, blk, 3:4], ALU.mult, [Br, B_cw], [Bc])
                    for k in range(3):
                        stt(ca[:], rb[:, k:k + 512], cw[:, blk, k:k + 1], ca[:], ALU.mult, ALU.add, [Br, B_cw, Bc], [Bc])
                    if i == 2:
                        act(dsts[2], ca[:], AF.Silu, [Bc], [Bd])
                    else:
                        act(ca[:], ca[:], AF.Silu, [Bc], [Bc])
                        j = cnt["sq"] % 2; cnt["sq"] += 1
                        tt("pool", sqb[j][:], ca[:], ca[:], ALU.mult, [Bc], [B_sqb[j]])
                        mm(P_N[:], ones_h[:], sqb[j][:], True, True, [B_sqb[j], B_c], [B_PN])
                        act(rsb[j][:], P_N[:], AF.Ln, [B_PN, B_c], [B_rsb[j]], bias=ce6, scale=1.0)
                        act(rsb[j][:], rsb[j][:], AF.Exp, [B_rsb[j]], [B_rsb[j]], scale=-0.5)
                        if i == 0:
                            stt(dsts[0], ca[:], 128.0 ** -0.5, rsb[j][:], ALU.mult, ALU.mult, [Bc, B_rsb[j]], [Bd])
                        else:
                            tt("dve", dsts[1], ca[:], rsb[j][:], ALU.mult, [Bc, B_rsb[j]], [Bd])

            def wave(tg, c, hs):
                par = tg % 2; n = tg * 4 + c
                b = cnt["w"] % NB; cnt["w"] += 1
                cs = slice(c * 128, (c + 1) * 128)
                J = list(range(len(hs)))
                knc = lambda j: knG[:, par, hs[j], cs]
                qnc = lambda j: qnG[:, par, hs[j], cs]
                vTc = lambda j: vTG[:, par, hs[j], cs]
                Bq = lambda j: B_qkv[par][hs[j]]
                Bt = lambda j: B_tmp[b][j]
                Bk = lambda j: B_ck[hs[j]][c]
                parts = []

                def p0():
                    for j in J:
                        tr(P_T[:, j, :], knc(j), ident_h[:], [Bq(j), B_ident], [B_PT[j]])
                        tr(P_T[:, 4 + j, :], vTc(j), ident_h[:], [Bq(j), B_ident], [B_PT[4 + j]])
                    for j in J:
                        h = hs[j]
                        ts("dve", kdec[:, h, c, :], P_T[:, j, :], kds[:, h, n:n + 1], ALU.mult, [B_PT[j], B_g], [Bk(j)])
                        ts("dve", vb[:, h, c, :], P_T[:, 4 + j, :], beta[:, h, n:n + 1], ALU.mult, [B_PT[4 + j], B_g], [Bk(j)])
                    for j in J:
                        h = hs[j]
                        ts("pool", diagG[b][j], ident_f[:], Gc[:, h, n:n + 1], ALU.mult, [B_ident, B_g], [Bt(j)])
                    mm(P_L[0][:, :, :].rearrange("p a b -> p (a b)"), ones_f[:], diagG4[b][:, :, :].rearrange("p a b -> p (a b)"),
                       True, True, [Bt(j) for j in J] + [B_c], [B_L[0][j] for j in J])
                parts.append(p0)

                def p1():
                    for j in J:
                        h = hs[j]
                        stt(mL[b][j][:], P_L[0][:, j, :], Gc[:, h, n:n + 1], maskL[:], ALU.subtract, ALU.max, [B_L[0][j], B_g, B_c], [Bt(j)])
                        stt(mU[b][j][:], P_L[0][:, j, :], Gc[:, h, n:n + 1], maskU[:], ALU.subtract, ALU.min, [B_L[0][j], B_g, B_c], [Bt(j)])
                    for j in J:
                        act(eGb[b][j][:], P_L[0][:, j, :], AF.Exp, [B_L[0][j]], [Bt(j)])
                    for j in J:
                        act(mL[b][j][:], mL[b][j][:], AF.Exp, [Bt(j)], [Bt(j)], scale=-1.0)
                        act(mU[b][j][:], mU[b][j][:], AF.Exp, [Bt(j)], [Bt(j)])
                    for j in J:
                        tt("pool", qg[:, hs[j], c, :], qnc(j), eGb[b][j][:], ALU.mult, [Bq(j), Bt(j)], [Bk(j)])
                    for j in J:
                        mm(P_L[1][:, j, :], knc(j), knc(j), True, True, [Bq(j)], [B_L[1][j]])
                        mm(P_L[2][:, j, :], knc(j), qnc(j), True, True, [Bq(j)], [B_L[2][j]])
                parts.append(p1)

                def p2():
                    for j in J:
                        h = hs[j]
                        stt(Pn[0][b][j][:], P_L[1][:, j, :], nbeta[:, h, n:n + 1], mL[b][j][:], ALU.mult, ALU.mult, [B_L[1][j], B_g, Bt(j)], [Bt(j)])
                        tt("dve", qkT[:, h, c, :], P_L[2][:, j, :], mU[b][j][:], ALU.mult, [B_L[2][j], Bt(j)], [Bk(j)])
                    for j in J:
                        tr(P_T[:, j, :], Pn[0][b][j][:], ident_h[:], [Bt(j), B_ident], [B_PT[j]])
                    for j in J:
                        cp("act", Pt[0][b][j][:], P_T[:, j, :], [B_PT[j]], [Bt(j)])
                    for j in J:
                        tt("dve", XT[0][b][j][:], P_T[:, j, :], ident_f[:], ALU.add, [B_PT[j], B_ident], [Bt(j)])
                parts.append(p2)

                for k in range(1, 7):
                    def pl(k=k):
                        a = (k - 1) % 2; c_ = k % 2
                        for j in J:
                            mm(P_L[0][:, j, :], Pt[a][b][j][:], Pn[a][b][j][:], True, True, [Bt(j)], [B_L[0][j]])
                        if k < 6:
                            for j in J:
                                mm(P_L[1][:, j, :], Pn[a][b][j][:], Pt[a][b][j][:], True, True, [Bt(j)], [B_L[1][j]])
                        for j in J:
                            cp("act", Pn[c_][b][j][:], P_L[0][:, j, :], [B_L[0][j]], [Bt(j)])
                        if k < 6:
                            for j in J:
                                cp("dve", Pt[c_][b][j][:], P_L[1][:, j, :], [B_L[1][j]], [Bt(j)])
                        for j in J:
                            mm(P_L[2][:, j, :], Pn[c_][b][j][:], XT[a][b][j][:], True, True, [Bt(j)], [B_L[2][j]])
                        for j in J:
                            if k == 6:
                                tt("dve", TT[:, hs[j], c, :], P_L[2][:, j, :], XT[a][b][j][:], ALU.add, [B_L[2][j], Bt(j)], [B_TT[hs[j]][c]])
                            else:
                                tt("dve", XT[c_][b][j][:], P_L[2][:, j, :], XT[a][b][j][:], ALU.add, [B_L[2][j], Bt(j)], [Bt(j)])
                    parts.append(pl)
                return parts

            def sstep(tg, c, hs):
                par = tg % 2; n = tg * 4 + c
                cs = slice(c * 128, (c + 1) * 128)
                J = list(range(len(hs)))
                st_ = []

                def s0():
                    for j in J:
                        h = hs[j]
                        mm(P_Sa[:, j, :], knG[:, par, h, cs], S_h[:, h, :], True, True, [B_qkv[par][h], B_S[h]], [B_Sa[j]])
                    for j in J:
                        h = hs[j]
                        stt(rbuf[:, h, :], P_Sa[:, j, :], nbg[:, h, n:n + 1], vb[:, h, c, :], ALU.mult, ALU.add,
                            [B_Sa[j], B_g, B_ck[h][c]], [B_r[h]])
                st_.append(s0)

                def s1():
                    for j in J:
                        h = hs[j]
                        mm(P_Sa[:, j, :], TT[:, h, c, :], rbuf[:, h, :], True, True, [B_TT[h][c], B_r[h]], [B_Sa[j]])
                    for j in J:
                        h = hs[j]
                        cp("act", vnew[:, h, :], P_Sa[:, j, :], [B_Sa[j]], [B_vn[h]])
                st_.append(s1)

                def s2():
                    for j in J:
                        h = hs[j]
                        mm(P_Sb[:, j, :], S_h[:, h, :], qg[:, h, c, :], True, False, [B_S[h], B_ck[h][c]], [B_Sb[j]])
                        mm(P_Sb[:, j, :], vnew[:, h, :], qkT[:, h, c, :], False, True, [B_vn[h], B_ck[h][c]], [B_Sb[j]])
                    for j in J:
                        h = hs[j]
                        mm(P_Sa[:, j, :], kdec[:, h, c, :], vnew[:, h, :], True, True, [B_ck[h][c], B_vn[h]], [B_Sa[j]])
                    for j in J:
                        h = hs[j]
                        cp("act", oTs[:, h, cs], P_Sb[:, j, :], [B_Sb[j]], [B_oTs[h][c]])
                    for j in J:
                        h = hs[j]
                        ts("dve", S_f[:, h, :], S_f[:, h, :], eGl[:, h, n:n + 1], ALU.mult, [B_S[h], B_g], [B_S[h]])
                        tt("dve", S_f[:, h, :], P_Sa[:, j, :], S_f[:, h, :], ALU.add, [B_Sa[j], B_S[h]], [B_S[h]])
                    for j in J:
                        h = hs[j]
                        cp("act", S_h[:, h, :], S_f[:, h, :], [B_S[h]], [B_S[h]])
                st_.append(s2)
                return st_

            def pnorm(h, tg):
                t0 = tg * 512
                j = cnt["z"] % 2; cnt["z"] += 1
                Bo = [B_oTs[h][k] for k in range(4)]
                ks.dma("sp", zb[j][:], projT[24 + h][:, t0:t0 + 512], reads=[B_projT[24 + h]], writes=[B_zb[j]])
                q = cnt["sq"] % 2; cnt["sq"] += 1
                tt("pool", sqb[q][:], oTs[:, h, :], oTs[:, h, :], ALU.mult, Bo, [B_sqb[q]])
                mm(P_N[:], ones_h[:], sqb[q][:], True, True, [B_sqb[q], B_c], [B_PN])
                act(rsb[q][:], P_N[:], AF.Ln, [B_PN, B_c], [B_rsb[q]], bias=ce6, scale=1.0 / 128.0)
                act(rsb[q][:], rsb[q][:], AF.Exp, [B_rsb[q]], [B_rsb[q]], scale=-0.5)
                act(zs[j][:], zb[j][:], AF.Silu, [B_zb[j]], [B_zb[j]])
                tt("dve", on_[j][:], oTs[:, h, :], rsb[q][:], ALU.mult, Bo + [B_rsb[q]], [B_og[j]])
                stt(og_[j][:], on_[j][:], nw[:, 0:1], zs[j][:], ALU.mult, ALU.mult, [B_og[j], B_cw, B_zb[j]], [B_og[j]])
                ks.dma("sp", oT[h][:, t0:t0 + 512], og_[j][:], reads=[B_og[j]], writes=[B_oT[h][tg]])

            def merge(streams):
                tot = max(len(s_) for s_ in streams) if streams else 0
                idx = [0] * len(streams)
                for step in range(tot):
                    for si_, s_ in enumerate(streams):
                        tgt = ((step + 1) * len(s_) + tot - 1) // tot
                        while idx[si_] < tgt:
                            s_[idx[si_]](); idx[si_] += 1

            HW = [[0, 1, 2, 3], [4, 5, 6, 7]]
            for h in range(NH):
                gprep(h, 0)
            pending = []
            for tg in range(NG):
                for c in range(4):
                    streams = [wave(tg, c, HW[0]) + wave(tg, c, HW[1])]
                    if pending:
                        streams.append(pending)
                    merge(streams)
                    if pending and c == 0 and tg > 0:
                        for h in range(NH):
                            pnorm(h, tg - 1)
                    if tg + 1 < NG:
                        gprep(2 * c, tg + 1); gprep(2 * c + 1, tg + 1)
                    pending = sstep(tg, c, HW[0]) + sstep(tg, c, HW[1])
            merge([pending])
            for h in range(NH):
                pnorm(h, NG - 1)
            ks.barrier()
        if stop_after == 2:
            return _finish(nc, ks, [])

        f3d = dscr("f3d", [NH, S], F32); B_f3d = Buf()
        WROW = 24576
        use_moe = (stop_after is None) or (stop_after >= 5)
        B_wsc = Buf("wsc")
        if use_moe:
            w_moe = din("w_moe", [32, 128, WROW])
            wsc = dscr("wsc", [32 * 128, WROW], BF16)
        SCL = 128.0 ** -0.5
        with ExitStack() as ph:
            P_t = pst(ph, "fP_t", [128, 128]); B_PF = PB("fP_t")
            P_F = P_t[0:NT, :]
            f3t = sbt(ph, "f3t", [NT, NH, 128], F32); B_f3t = Buf()
            for h in range(NH):
                tr(P_F, Fc[:, h, :], ident_f[:], [B_g, B_ident], [B_PF])
                cp("dve", f3t[:, h, :], P_F, [B_PF], [B_f3t])
            ks.dma("sp", f3d.rearrange("h (tt p) -> tt h p", p=128), f3t[:], reads=[B_f3t], writes=[B_f3d])

            qT = [sbt(ph, "fqT%d" % i, [128, S], BF16) for i in range(2)]
            kT = [sbt(ph, "fkT%d" % i, [128, S], BF16) for i in range(2)]
            va = [sbt(ph, "fva%d" % i, [128, NT, 128], BF16) for i in range(2)]
            B_in = [Buf(), Buf()]
            Fb = [sbt(ph, "fFb%d" % i, [128, 512], F32) for i in range(2)]; B_Fb = [Buf(), Buf()]
            lgb = [sbt(ph, "flg%d" % i, [128, 512], F32) for i in range(3)]; B_lg = [Buf() for _ in range(3)]
            pT = [sbt(ph, "fpT%d" % i, [128, 512], BF16) for i in range(4)]; B_pT = [Buf() for _ in range(4)]
            rs_ = [sbt(ph, "frs%d" % i, [128, 512], F32) for i in range(2)]; B_rs_ = [Buf(), Buf()]
            oTg = [sbt(ph, "foTg%d" % i, [128, 512], BF16) for i in range(2)]; B_oTg = [Buf(), Buf()]
            P_s = [pst(ph, "fP_s%d" % i, [128, 512]) for i in range(3)]; B_Ps = [PB("fP_s%d" % i) for i in range(3)]
            P_o = [pst(ph, "fP_o%d" % i, [128, 512]) for i in range(2)]; B_Po = [PB("fP_o0"), PB("fP_o1")]
            P_m = [pst(ph, "fP_m%d" % i, [128, 512]) for i in range(2)]; B_Pm = [PB("fP_m0"), PB("fP_m1")]
            it = 0; gi = 0
            if use_moe:
                wst = [sbt(ph, "wst%d" % i, [128, WROW], BF16) for i in range(2)]; B_wst = [Buf(), Buf()]

            def precast(e_):
                i = e_ % 2
                wv = wst[i]
                ks.dma("pool", wv[:], w_moe[e_], writes=[B_wst[i]])
                ks.dma("sp", wsc[e_ * 128:(e_ + 1) * 128, :], wv[:], reads=[B_wst[i]], writes=[B_wsc])

            def load_head(h):
                hp = h % 2
                ks.dma("sp", qT[hp][:], projT[32 + h], reads=[B_projT[32 + h]], writes=[B_in[hp]])
                ks.dma("sp", kT[hp][:], projT[40 + h], reads=[B_projT[40 + h]], writes=[B_in[hp]])
                ks.dma("sp", va[hp][:], vtok[:, h * 128:(h + 1) * 128].rearrange("(tt p) c -> p tt c", p=128),
                       reads=[B_vtok], writes=[B_in[hp]])

            def load_F(idx):
                h_, q_ = idx // NG, idx % NG
                ks.dma("sp", Fb[idx % 2][:], f3d[h_:h_ + 1, q_ * 512:(q_ + 1) * 512].broadcast_to([128, 512]),
                       reads=[B_f3d], writes=[B_Fb[idx % 2]])

            load_head(0)
            load_F(0)
            for h in range(NH):
                hp = h % 2
                for qgi in range(NG):
                    q0 = qgi * 512
                    g_ = gi % 2; gi += 1
                    if gi < NH * NG:
                        load_F(gi)
                    if qgi == 1 and h + 1 < NH:
                        load_head(h + 1)
                    if use_moe and (h * NG + qgi) % 2 == 0:
                        precast((h * NG + qgi) // 2)
                    nj = 4 * qgi + 4

                    def front(j):
                        nonlocal it
                        bl = max(0, j - 4 * qgi)
                        c_lo = bl * 128
                        ps_ = P_s[it % 3]; Bps = B_Ps[it % 3]
                        lg_ = lgb[it % 3]; Blg = B_lg[it % 3]
                        pt_ = pT[it % 4]; Bpt = B_pT[it % 4]
                        it += 1
                        mm(ps_[:, c_lo:512], kT[hp][:, j * 128:(j + 1) * 128], qT[hp][:, q0 + c_lo:q0 + 512], True, True,
                           [B_in[hp]], [Bps])
                        stt(lg_[:, c_lo:512], ps_[:, c_lo:512], SCL, Fb[g_][:, c_lo:512], ALU.mult, ALU.add, [Bps, B_Fb[g_]], [Blg])
                        act(pt_[:, c_lo:512], lg_[:, c_lo:512], AF.Exp, [Blg, B_g], [Bpt], bias=nF[:, h, j:j + 1], scale=1.0)
                        if j >= 4 * qgi:
                            asel(pt_[:, c_lo:c_lo + 128], pt_[:, c_lo:c_lo + 128], "le", 0.0, [Bpt], [Bpt])
                        return (pt_, Bpt, c_lo)

                    fq = [front(0)]
                    if nj > 1:
                        fq.append(front(1))
                    for j in range(nj):
                        cur = fq.pop(0)
                        if j + 2 < nj:
                            fq.append(front(j + 2))
                        pt_, Bpt, c_lo = cur
                        mm(P_o[g_][:, c_lo:512], va[hp][:, j, :], pt_[:, c_lo:512], j == 0, j == nj - 1, [Bpt, B_in[hp]], [B_Po[g_]])
                        mm(P_m[g_][:, c_lo:512], ones_h[:], pt_[:, c_lo:512], j == 0, j == nj - 1, [Bpt, B_c], [B_Pm[g_]])
                    cp("dve", rs_[g_][:], P_m[g_][:], [B_Pm[g_]], [B_rs_[g_]])
                    ks.op("dve", lambda e, g_=g_: e.reciprocal(rs_[g_][:], rs_[g_][:]), reads=[B_rs_[g_]], writes=[B_rs_[g_]])
                    tt("dve", oTg[g_][:], P_o[g_][:], rs_[g_][:], ALU.mult, [B_Po[g_], B_rs_[g_]], [B_oTg[g_]])
                    ks.dma("sp", oT[8 + h][:, q0:q0 + 512], oTg[g_][:], reads=[B_oTg[g_]], writes=[B_oT[8 + h][qgi]])
            ks.barrier()
        gst_.close()
        if stop_after == 3:
            return _finish(nc, ks, [])

        NBLK = 64
        BLK = 256
        x1d = dscr("x1d", [S, D], F32); B_x1d = [Buf() for _ in range(NT)]
        h2d = dscr("h2d", [S, D], BF16); B_h2d = [Buf() for _ in range(NT)]
        xsd = dscr("xsd", [NBLK * BLK, D], BF16); B_xsd = Buf()
        ybd = dscr("ybd", [NBLK * BLK, D], BF16); B_ybd = Buf()
        mwd = dscr("mwd", [S, 32], F32); B_mwd = Buf()
        rst = ExitStack(); st.enter_context(rst)
        d01 = sbt(rst, "d01", [128, 2, NT], I32)
        w12 = sbt(rst, "w12", [128, 2, NT], F32)
        widx = sbt(rst, "widx", [128, NBLK], I32)
        B_rs = Buf("routing")

        def bcast_load(stack, name, src_row, B, plus1=False):
            t_ = sbt(stack, name, [128, D], F32)
            ks.dma("sp", t_[:], src_row.broadcast_to([128, D]), reads=[B_modrow], writes=[B])
            if plus1:
                ts("pool", t_[:], t_[:], 1.0, ALU.add, [B], [B])
            return t_

        with ExitStack() as ph:
            B_bc = Buf()
            g1p = bcast_load(ph, "g1p", modrow[0:1, 2 * D:3 * D], B_bc, True)
            sh2b = bcast_load(ph, "sh2b", modrow[0:1, 3 * D:4 * D], B_bc)
            sc2p = bcast_load(ph, "sc2p", modrow[0:1, 4 * D:5 * D], B_bc, True)
            l1g = bcast_load(ph, "l1g", ln1_g[0:1, :], B_bc)
            l1b = bcast_load(ph, "l1b", ln1_b[0:1, :], B_bc)
            wo = sbt(ph, "wo", [128, KC, D], BF16); B_wo = Buf()
            w_out_v = w_out.rearrange("(kc p) n -> p kc n", p=128)
            for kc in range(KC):
                ks.dma("pool", wo[:, kc, :], w_out_v[:, kc, :], writes=[B_wo])
            wr = sbt(ph, "wr", [128, KC, 36], F32); brb = sbt(ph, "brb", [128, 36], F32); B_wr = Buf()
            wrh = sbt(ph, "wrh", [128, KC, 36], BF16)
            ks.dma("sp", wr[:], w_r.rearrange("(kc p) n -> p kc n", p=128), writes=[B_wr])
            ks.dma("sp", brb[:], b_r[0:1, :].broadcast_to([128, 36]), writes=[B_wr])
            cp("dve", wrh[:], wr[:], [B_wr], [B_wr])
            ogb = [sbt(ph, "ogb%d" % i, [128, KC, 512], BF16) for i in range(2)]; B_ogb = [Buf(), Buf()]
            xt_ = sbt(ph, "xt0", [128, D], F32); B_xt = Buf()
            vv = sbt(ph, "vv0", [128, D], F32); B_vv = Buf()
            h2 = sbt(ph, "h2_0", [128, D], F32); B_h2 = Buf()
            h2h = [sbt(ph, "h2h%d" % i, [128, D], BF16) for i in range(2)]; B_h2h = [Buf(), Buf()]
            h2Tf = sbt(ph, "h2Tf", [128, KC, 128], BF16); B_h2Tf = Buf()
            st4 = sbt(ph, "st4", [128, 16], F32); B_st = Buf()
            rt = sbt(ph, "rt", [128, 128], F32); B_rt = Buf()
            OH = sbt(ph, "OH", [128, 2, NT, 32], F32); B_OH = Buf()
            junk = sbt(ph, "junk", [128, D], BF16); B_junk = Buf()
            P_y = [pst(ph, "P_y%d" % i, [128, 512]) for i in range(4)]; B_Py = [PB("P_y%d" % i) for i in range(4)]
            P_h = [pst(ph, "P_h%d" % i, [128, 8, 128], BF16) for i in range(2)]; B_Ph = [PB("P_h0"), PB("P_h1")]
            P_r = pst(ph, "P_r", [128, 36]); B_Pr = PB("P_r")
            oT_v = oT.rearrange("h p t -> p h t")
            py = 0; ph_i = 0
            ks.dma("sp", ogb[0][:], oT_v[:, :, 0:512], reads=[B_oT[h][0] for h in range(16)], writes=[B_ogb[0]])
            ks.dma("sp", xt_[:], xtok[0:128, :], writes=[B_xt])
            for tg in range(NG):
                gb = tg % 2
                if tg + 1 < NG:
                    ks.dma("sp", ogb[1 - gb][:], oT_v[:, :, (tg + 1) * 512:(tg + 2) * 512],
                           reads=[B_oT[h][tg + 1] for h in range(16)], writes=[B_ogb[1 - gb]])
                for ti in range(4):
                    tI = tg * 4 + ti
                    hb_ = tI % 2
                    for cg in range(4):
                        p_ = py % 4; py += 1
                        for hh in range(KC):
                            mm(P_y[p_][:], ogb[gb][:, hh, ti * 128:(ti + 1) * 128], wo[:, hh, cg * 512:(cg + 1) * 512],
                               hh == 0, hh == KC - 1, [B_ogb[gb], B_wo], [B_Py[p_]])
                        tt("dve", vv[:, cg * 512:(cg + 1) * 512], P_y[p_][:], g1p[:, cg * 512:(cg + 1) * 512], ALU.mult,
                           [B_Py[p_], B_bc], [B_vv])
                    stt(vv[:], xt_[:], ALPHA, vv[:], ALU.mult, ALU.add, [B_xt, B_vv], [B_vv])
                    if tI + 1 < NT:
                        ks.dma("sp", xt_[:], xtok[(tI + 1) * 128:(tI + 2) * 128, :], writes=[B_xt])
                    ks.op("dve", lambda e: e.reduce_sum(out=st4[:, 0:1], in_=vv[:], axis=AX.X), reads=[B_vv], writes=[B_st])
                    act(junk[:], vv[:], AF.Square, [B_vv], [B_junk, B_st], accum_out=st4[:, 1:2])
                    ts("dve", st4[:, 2:3], st4[:, 0:1], 1.0 / D, ALU.mult, [B_st], [B_st])
                    tt("dve", st4[:, 3:4], st4[:, 2:3], st4[:, 2:3], ALU.mult, [B_st], [B_st])
                    stt(st4[:, 4:5], st4[:, 1:2], 1.0 / D, st4[:, 3:4], ALU.mult, ALU.subtract, [B_st], [B_st])
                    act(st4[:, 5:6], st4[:, 4:5], AF.Sqrt, [B_st, B_c], [B_st], bias=ce5, scale=1.0)
                    ks.op("dve", lambda e: e.reciprocal(st4[:, 6:7], st4[:, 5:6]), reads=[B_st], writes=[B_st])
                    ts("dve", vv[:], vv[:], st4[:, 2:3], ALU.subtract, [B_vv, B_st], [B_vv], s2=st4[:, 6:7], op1=ALU.mult)
                    tt("dve", vv[:], vv[:], l1g[:], ALU.mult, [B_vv, B_bc], [B_vv])
                    tt("dve", vv[:], vv[:], l1b[:], ALU.add, [B_vv, B_bc], [B_vv])
                    ks.dma("sp", x1d[tI * 128:(tI + 1) * 128, :], vv[:], reads=[B_vv], writes=[B_x1d[tI]])
                    tt("dve", h2[:], vv[:], sc2p[:], ALU.mult, [B_vv, B_bc], [B_h2])
                    tt("dve", h2h[hb_][:], h2[:], sh2b[:], ALU.add, [B_h2, B_bc], [B_h2h[hb_]])
                    ks.dma("sp", h2d[tI * 128:(tI + 1) * 128, :], h2h[hb_][:], reads=[B_h2h[hb_]], writes=[B_h2d[tI]])
                    for k8 in range(2):
                        q_ = ph_i % 2; ph_i += 1
                        for kk in range(8):
                            kc = k8 * 8 + kk
                            tr(P_h[q_][:, kk, :], h2h[hb_][:, kc * 128:(kc + 1) * 128], ident_h[:], [B_h2h[hb_], B_ident], [B_Ph[q_]])
                        cp("act", h2Tf[:, k8 * 8:(k8 + 1) * 8, :], P_h[q_][:, :, :], [B_Ph[q_]], [B_h2Tf])
                    for kc in range(KC):
                        mm(P_r[:, :], h2Tf[:, kc, :], wrh[:, kc, :], kc == 0, kc == KC - 1, [B_h2Tf, B_wr], [B_Pr])
                    R_ = [B_rt, B_c]
                    lg = rt[:, 0:36]
                    tt("dve", lg, P_r[:, :], brb[:], ALU.add, [B_Pr, B_wr], [B_rt])
                    gmx = rt[:, 36:37]; ohg = rt[:, 40:44]; gsum = rt[:, 37:38]; gw = rt[:, 38:39]
                    ks.op("dve", lambda e: e.reduce_max(out=gmx, in_=rt[:, 0:4], axis=AX.X), reads=R_, writes=[B_rt])
                    ts("dve", ohg, rt[:, 0:4], gmx, ALU.is_equal, R_, [B_rt])
                    ts("dve", rt[:, 44:48], rt[:, 0:4], gmx, ALU.subtract, R_, [B_rt])
                    act(rt[:, 44:48], rt[:, 44:48], AF.Exp, R_, [B_rt])
                    ks.op("dve", lambda e: e.reduce_sum(out=gsum, in_=rt[:, 44:48], axis=AX.X), reads=R_, writes=[B_rt])
                    ks.op("dve", lambda e: e.reciprocal(gw, gsum), reads=R_, writes=[B_rt])
                    es = rt[:, 48:56]
                    ts("dve", es, rt[:, 4:12], ohg[:, 0:1], ALU.mult, R_, [B_rt])
                    for g_ in range(1, 4):
                        stt(es, rt[:, 4 + 8 * g_:12 + 8 * g_], ohg[:, g_:g_ + 1], es, ALU.mult, ALU.add, R_, [B_rt])
                    m1 = rt[:, 56:57]; m2 = rt[:, 57:58]; oh1 = rt[:, 64:72]; oh2 = rt[:, 72:80]; es2 = rt[:, 80:88]
                    ks.op("dve", lambda e: e.reduce_max(out=m1, in_=es, axis=AX.X), reads=R_, writes=[B_rt])
                    ts("dve", oh1, es, m1, ALU.is_equal, R_, [B_rt])
                    stt(es2, oh1, -1.0e30, es, ALU.mult, ALU.add, R_, [B_rt])
                    ks.op("dve", lambda e: e.reduce_max(out=m2, in_=es2, axis=AX.X), reads=R_, writes=[B_rt])
                    ts("dve", oh2, es2, m2, ALU.is_equal, R_, [B_rt])
                    w1 = rt[:, 58:59]; w2 = rt[:, 59:60]
                    tt("dve", w1, m2, m1, ALU.subtract, R_, [B_rt])
                    act(w1, w1, AF.Exp, R_, [B_rt])
                    ts("dve", w1, w1, 1.0, ALU.add, R_, [B_rt])
                    ks.op("dve", lambda e: e.reciprocal(w1, w1), reads=R_, writes=[B_rt])
                    ts("dve", w2, w1, -1.0, ALU.mult, R_, [B_rt], s2=1.0, op1=ALU.add)
                    tt("dve", w12[:, 0, tI:tI + 1], w1, gw, ALU.mult, R_, [B_rt, B_rs])
                    tt("dve", w12[:, 1, tI:tI + 1], w2, gw, ALU.mult, R_, [B_rt, B_rs])
                    for g_ in range(4):
                        ts("dve", OH[:, 0, tI, g_ * 8:(g_ + 1) * 8], oh1, ohg[:, g_:g_ + 1], ALU.mult, R_, [B_rt, B_OH])
                        ts("dve", OH[:, 1, tI, g_ * 8:(g_ + 1) * 8], oh2, ohg[:, g_:g_ + 1], ALU.mult, R_, [B_rt, B_OH])
            if "mwd" in dbg:
                mwt_ = sbt(ph, "mwt_", [128, NT, 32], F32)
                for tI in range(NT):
                    ts("dve", mwt_[:, tI, :], OH[:, 0, tI, :], w12[:, 0, tI:tI + 1], ALU.mult, [B_OH, B_rs], [B_mwd])
                    stt(mwt_[:, tI, :], OH[:, 1, tI, :], w12[:, 1, tI:tI + 1], mwt_[:, tI, :], ALU.mult, ALU.add, [B_OH, B_rs, B_mwd], [B_mwd])
                ks.dma("sp", mwd.rearrange("(tt p) c -> p tt c", p=128), mwt_[:], reads=[B_mwd], writes=[B_mwd])
            NE = 32
            Cc = sbt(ph, "Cc", [128, NT * NE], F32); B_s = Buf("sort")
            rk = sbt(ph, "rk", [128, NT * NE], F32)
            pf = sbt(ph, "pf", [128, NT * NE], F32)
            sm = sbt(ph, "sm", [128, 8, 64], F32)
            UTs = sbt(ph, "UTs", [128, 128], F32)
            ks.op("pool", lambda e: e.memset(UTs[:], 1.0), writes=[B_s])
            ks.op("pool", lambda e: e.affine_select(out=UTs[:], in_=UTs[:], pattern=[[1, 128]], compare_op=ALU.is_ge,
                                                    fill=0.0, base=-1, channel_multiplier=-1), reads=[B_s], writes=[B_s])
            OHf = lambda k: OH[:, k, :, :].rearrange("p t e -> p (t e)")
            tt("dve", Cc[:], OHf(0), OHf(1), ALU.add, [B_OH], [B_s])
            for half in range(2):
                sl = slice(half * 512, (half + 1) * 512)
                mm(P_y[0][:], UTs[:], Cc[:, sl], True, True, [B_s], [B_Py[0]])
                mm(P_y[1][:], ones_f[:], Cc[:, sl], True, True, [B_s, B_c], [B_Py[1]])
                cp("dve", rk[:, sl], P_y[0][:], [B_Py[0]], [B_s])
                cp("dve", pf[:, sl], P_y[1][:], [B_Py[1]], [B_s])
            pf3 = pf[:, :].rearrange("p (t e) -> p t e", e=NE)
            rk3 = rk[:, :].rearrange("p (t e) -> p t e", e=NE)
            tot = sm[:, 0, 0:NE]; run = sm[:, 1, 0:NE]
            ks.op("dve", lambda e: e.memset(run, 0.0), writes=[B_s])
            for tI in range(NT):
                tt("dve", rk3[:, tI, :], rk3[:, tI, :], run, ALU.add, [B_s], [B_s])
                tt("dve", run, run, pf3[:, tI, :], ALU.add, [B_s], [B_s])
            cp("dve", tot, run, [B_s], [B_s])
            thr = sm[:, 2, 0:32]; nbk = sm[:, 3, 0:NE]; tmp32 = sm[:, 4, 0:32]
            ks.op("pool", lambda e: e.iota(thr, pattern=[[BLK, 32]], base=0, channel_multiplier=0,
                                           allow_small_or_imprecise_dtypes=True), writes=[B_s])
            for e_ in range(NE):
                ts("dve", tmp32, thr, tot[:, e_:e_ + 1], ALU.is_lt, [B_s], [B_s])
                ks.op("dve", lambda e, e_=e_: e.reduce_sum(out=nbk[:, e_:e_ + 1], in_=tmp32, axis=AX.X), reads=[B_s], writes=[B_s])
            pend = sm[:, 5, 0:NE]; pstart = sm[:, 6, 0:NE]
            ks.op("dve", lambda e: e.tensor_tensor_scan(out=pend, data0=ones_f[:, 0:NE], data1=nbk, initial=0.0,
                                                        op0=ALU.mult, op1=ALU.add), reads=[B_s, B_c], writes=[B_s])
            tt("dve", pstart, pend, nbk, ALU.subtract, [B_s], [B_s])
            ts("dve", pstart, pstart, float(BLK), ALU.mult, [B_s], [B_s])
            ts("dve", pend, pend, float(BLK), ALU.mult, [B_s], [B_s])
            for tI in range(NT):
                tt("dve", rk3[:, tI, :], rk3[:, tI, :], pstart, ALU.add, [B_s], [B_s])
            dstf = sm[:, 7, :]
            for k in range(2):
                tt("dve", Cc[:], OHf(k), rk[:], ALU.mult, [B_OH, B_s], [B_s])
                ks.op("dve", lambda e, k=k: e.tensor_reduce(out=dstf[:, k * NT:(k + 1) * NT],
                                                            in_=Cc[:, :].rearrange("p (t e) -> p t e", e=NE),
                                                            axis=AX.X, op=ALU.add), reads=[B_s], writes=[B_s])
            cp("dve", d01[:, :, :].rearrange("p k t -> p (k t)"), dstf, [B_s], [B_rs])
            bthr = sm[:, 2, 0:NBLK]; bacc = sm[:, 3, 0:NBLK]; btmp = sm[:, 4, 0:NBLK]
            ks.op("pool", lambda e: e.iota(bthr, pattern=[[BLK, NBLK]], base=0, channel_multiplier=0,
                                           allow_small_or_imprecise_dtypes=True), reads=[B_s], writes=[B_s])
            ks.op("dve", lambda e: e.memset(bacc, 0.0), reads=[B_s], writes=[B_s])
            for e_ in range(NE):
                ts("dve", btmp, bthr, pend[:, e_:e_ + 1], ALU.is_ge, [B_s], [B_s])
                tt("dve", bacc, bacc, btmp, ALU.add, [B_s], [B_s])
            ts("dve", bacc, bacc, float(NE - 1), ALU.min, [B_s], [B_s], s2=128.0, op1=ALU.mult)
            pidx = sm[:, 0, 32:33]
            ks.op("pool", lambda e: e.iota(pidx, pattern=[[0, 1]], base=0, channel_multiplier=1,
                                           allow_small_or_imprecise_dtypes=True), reads=[B_s], writes=[B_s])
            ts("dve", bacc, bacc, pidx, ALU.add, [B_s], [B_s])
            cp("dve", widx[:], bacc, [B_s], [B_rs])
            for tI in range(NT):
                hb_ = tI % 2
                ks.dma("sp", h2h[hb_][:], h2d[tI * 128:(tI + 1) * 128, :], reads=[B_h2d[tI]], writes=[B_h2h[hb_]])
                for k in range(2):
                    ks.dma("pool", None, None, reads=[B_h2h[hb_], B_rs], writes=[B_xsd],
                           fn=lambda e, k=k, tI=tI, hb_=hb_: e.indirect_dma_start(
                               out=xsd[:, :], out_offset=bass.IndirectOffsetOnAxis(ap=d01[:, k, tI:tI + 1], axis=0),
                               in_=h2h[hb_][:], in_offset=None))
            ks.barrier()
        if stop_after == 4:
            return _finish(nc, ks, [])

        with ExitStack() as ph:
            wblk = [sbt(ph, "wblk%d" % i, [128, WROW], BF16) for i in range(2)]; B_wb = [Buf(), Buf()]
            xsb = [sbt(ph, "xsb%d" % i, [128, 2, D], BF16) for i in range(2)]; B_xsb = [Buf(), Buf()]
            xsT = [sbt(ph, "xsT%d" % i, [128, KC, BLK], BF16) for i in range(2)]; B_xsT = [Buf(), Buf()]
            sg = [sbt(ph, "sg%d" % i, [128, BLK], F32) for i in range(2)]; B_sg = [Buf(), Buf()]
            hid = [sbt(ph, "hid%d" % i, [128, 4, BLK], BF16) for i in range(2)]; B_hid = [Buf(), Buf()]
            yo = [sbt(ph, "yo%d" % i, [128, D], BF16) for i in range(2)]; B_yo = [Buf(), Buf()]
            P_x = [pst(ph, "P_x%d" % i, [128, 8, 128], BF16) for i in range(2)]; B_Px = [PB("P_x0"), PB("P_x1")]
            P_g = [pst(ph, "P_g%d" % i, [128, BLK]) for i in range(2)]; B_Pg = [PB("P_g0"), PB("P_g1")]
            P_u = [pst(ph, "P_u%d" % i, [128, BLK]) for i in range(2)]; B_Pu = [PB("P_u0"), PB("P_u1")]
            P_d = [pst(ph, "P_d%d" % i, [128, 512]) for i in range(2)]; B_Pd = [PB("P_d0"), PB("P_d1")]
            px = 0; pq = 0; pd = 0; sgi = 0; yi = 0
            for b in range(NBLK):
                wb = b % 2
                wv = wblk[wb]
                ks.dma("pool", None, None, reads=[B_rs, B_wsc], writes=[B_wb[wb]],
                       fn=lambda e, b=b, wv=wv: e.indirect_dma_start(
                           out=wv[:], out_offset=None, in_=wsc[:, :],
                           in_offset=bass.IndirectOffsetOnAxis(ap=widx[:, b:b + 1], axis=0)))
                if b == 0:
                    ks.dma("sp", xsb[0][:], xsd[0:BLK, :].rearrange("(t p) d -> p t d", p=128),
                           reads=[B_xsd], writes=[B_xsb[0]])
                if b + 1 < NBLK:
                    nb_ = (b + 1) % 2
                    ks.dma("sp", xsb[nb_][:], xsd[(b + 1) * BLK:(b + 2) * BLK, :].rearrange("(t p) d -> p t d", p=128),
                           reads=[B_xsd], writes=[B_xsb[nb_]])
                for t in range(2):
                    for k8 in range(2):
                        q_ = px % 2; px += 1
                        for kk in range(8):
                            kc = k8 * 8 + kk
                            tr(P_x[q_][:, kk, :], xsb[wb][:, t, kc * 128:(kc + 1) * 128], ident_h[:], [B_xsb[wb], B_ident], [B_Px[q_]])
                        if (px % 2) == 0:
                            cp("act", xsT[wb][:, k8 * 8:(k8 + 1) * 8, t * 128:(t + 1) * 128], P_x[q_][:, :, :], [B_Px[q_]], [B_xsT[wb]])
                        else:
                            cp("dve", xsT[wb][:, k8 * 8:(k8 + 1) * 8, t * 128:(t + 1) * 128], P_x[q_][:, :, :], [B_Px[q_]], [B_xsT[wb]])
                wgv = wv[:, 0:8192].rearrange("p (kc n) -> p kc n", n=512)
                wuv = wv[:, 8192:16384].rearrange("p (kc n) -> p kc n", n=512)
                wdv = wv[:, 16384:24576].rearrange("p (hc n) -> p hc n", n=D)
                hb = b % 2
                for hc in range(4):
                    q_ = pq % 2; pq += 1
                    for kc in range(KC):
                        mm(P_g[q_][:], wgv[:, kc, hc * 128:(hc + 1) * 128], xsT[wb][:, kc, :], kc == 0, kc == KC - 1,
                           [B_wb[wb], B_xsT[wb]], [B_Pg[q_]])
                    for kc in range(KC):
                        mm(P_u[q_][:], wuv[:, kc, hc * 128:(hc + 1) * 128], xsT[wb][:, kc, :], kc == 0, kc == KC - 1,
                           [B_wb[wb], B_xsT[wb]], [B_Pu[q_]])
                    s_ = sgi % 2; sgi += 1
                    act(sg[s_][:], P_g[q_][:], AF.Silu, [B_Pg[q_]], [B_sg[s_]])
                    tt("dve", hid[hb][:, hc, :], P_u[q_][:], sg[s_][:], ALU.mult, [B_sg[s_], B_Pu[q_]], [B_hid[hb]])
                for t in range(2):
                    y_ = yi % 2; yi += 1
                    for cg in range(4):
                        p_ = pd % 2; pd += 1
                        for hc in range(4):
                            mm(P_d[p_][:], hid[hb][:, hc, t * 128:(t + 1) * 128], wdv[:, hc, cg * 512:(cg + 1) * 512],
                               hc == 0, hc == 3, [B_hid[hb], B_wb[wb]], [B_Pd[p_]])
                        if cg % 2 == 0:
                            cp("act", yo[y_][:, cg * 512:(cg + 1) * 512], P_d[p_][:], [B_Pd[p_]], [B_yo[y_]])
                        else:
                            cp("dve", yo[y_][:, cg * 512:(cg + 1) * 512], P_d[p_][:], [B_Pd[p_]], [B_yo[y_]])
                    r0 = b * BLK + t * 128
                    ks.dma("sp", ybd[r0:r0 + 128, :], yo[y_][:], reads=[B_yo[y_]], writes=[B_ybd])
            ks.barrier()
        if stop_after == 5:
            return _finish(nc, ks, [])
        with ExitStack() as ph:
            B_bc = Buf()
            g2p = bcast_load(ph, "g2p", modrow[0:1, 5 * D:6 * D], B_bc, True)
            l2g = bcast_load(ph, "l2g", ln2_g[0:1, :], B_bc)
            l2b = bcast_load(ph, "l2b", ln2_b[0:1, :], B_bc)
            rr = [[sbt(ph, "rr%d_%d" % (i, k), [128, D], BF16) for k in range(2)] for i in range(2)]
            B_rr = [Buf(), Buf()]
            ya_ = [sbt(ph, "ya%d" % i, [128, D], F32) for i in range(2)]; B_ya = [Buf(), Buf()]
            x1t = [sbt(ph, "x1t%d" % i, [128, D], F32) for i in range(2)]; B_x1t = [Buf(), Buf()]
            st5 = sbt(ph, "st5", [128, 16], F32); B_st5 = Buf()
            junk5 = sbt(ph, "junk5", [128, D], BF16); B_j5 = Buf()
            for tI in range(NT):
                i = tI % 2
                for k in range(2):
                    ks.dma("pool", None, None, reads=[B_ybd, B_rs], writes=[B_rr[i]],
                           fn=lambda e, k=k, tI=tI, i=i: e.indirect_dma_start(
                               out=rr[i][k][:], out_offset=None, in_=ybd[:, :],
                               in_offset=bass.IndirectOffsetOnAxis(ap=d01[:, k, tI:tI + 1], axis=0)))
                if tI == 0:
                    ks.dma("sp", x1t[0][:], x1d[0:128, :], reads=[B_x1d[0]], writes=[B_x1t[0]])
                if tI + 1 < NT:
                    ks.dma("sp", x1t[(tI + 1) % 2][:], x1d[(tI + 1) * 128:(tI + 2) * 128, :], reads=[B_x1d[tI + 1]],
                           writes=[B_x1t[(tI + 1) % 2]])
                ya = ya_[i][:]; By = B_ya[i]
                ts("dve", ya, rr[i][0][:], w12[:, 0, tI:tI + 1], ALU.mult, [B_rr[i], B_rs], [By])
                stt(ya, rr[i][1][:], w12[:, 1, tI:tI + 1], ya, ALU.mult, ALU.add, [B_rr[i], B_rs, By], [By])
                tt("dve", ya, ya, g2p[:], ALU.mult, [By, B_bc], [By])
                stt(ya, x1t[i][:], ALPHA, ya, ALU.mult, ALU.add, [B_x1t[i], By], [By])
                ks.op("dve", lambda e, ya=ya: e.reduce_sum(out=st5[:, 0:1], in_=ya, axis=AX.X), reads=[By], writes=[B_st5])
                act(junk5[:], ya, AF.Square, [By], [B_j5, B_st5], accum_out=st5[:, 1:2])
                ts("dve", st5[:, 2:3], st5[:, 0:1], 1.0 / D, ALU.mult, [B_st5], [B_st5])
                tt("dve", st5[:, 3:4], st5[:, 2:3], st5[:, 2:3], ALU.mult, [B_st5], [B_st5])
                stt(st5[:, 4:5], st5[:, 1:2], 1.0 / D, st5[:, 3:4], ALU.mult, ALU.subtract, [B_st5], [B_st5])
                act(st5[:, 5:6], st5[:, 4:5], AF.Sqrt, [B_st5, B_c], [B_st5], bias=ce5, scale=1.0)
                ks.op("dve", lambda e: e.reciprocal(st5[:, 6:7], st5[:, 5:6]), reads=[B_st5], writes=[B_st5])
                ts("dve", ya, ya, st5[:, 2:3], ALU.subtract, [By, B_st5], [By], s2=st5[:, 6:7], op1=ALU.mult)
                tt("dve", ya, ya, l2g[:], ALU.mult, [By, B_bc], [By])
                tt("dve", ya, ya, l2b[:], ALU.add, [By, B_bc], [By])
                ks.dma("sp", out[tI * 128:(tI + 1) * 128, :], ya, reads=[By], writes=[Buf()])
            ks.barrier()

        return _finish(nc, ks, [])


def _finish(nc, ks, bufs):
    for key, val in ks.cnt.items():
        if key.startswith("d_") and val > 0:
            ks._wait("sp", (key, val))
    print("kernel build: insts=%d waits=%d" % (ks.n_inst, ks.n_wait))
    return nc


def _col_perm():
    idx = list(range(0, 4096)) + list(range(4112, 4112 + 3072)) + list(range(4096, 4112)) + list(range(7184, 7192))
    return np.asarray(idx)


def make_in_maps(inputs, n_cores=8):
    f = lambda a: np.ascontiguousarray(np.asarray(a, dtype=np.float32))
    x = np.asarray(inputs["x"]); c = np.asarray(inputs["c"])
    shared = {
        "w_ada": f(inputs["w_ada"][0]),
        "b_ada": f(inputs["b_ada"][0][None, :]),
        "w_in": f(inputs["w_in"][0][:, _col_perm()]),
        "convw": f(np.asarray(inputs["dn_conv_w"][0]).reshape(4, 24, 128).transpose(2, 1, 0)),
        "a_log": f(inputs["dn_a_log"][0][None, :]),
        "dt_bias": f(inputs["dn_dt_bias"][0][None, :]),
        "f_bias": f(inputs["fox_f_bias"][0][None, :]),
        "norm_w": f(np.asarray(inputs["dn_norm_w"][0])[:, None]),
        "w_out": f(inputs["w_out"][0]),
        "ln1_g": f(inputs["ln1_g"][0][None, :]), "ln1_b": f(inputs["ln1_b"][0][None, :]),
        "ln2_g": f(inputs["ln2_g"][0][None, :]), "ln2_b": f(inputs["ln2_b"][0][None, :]),
        "w_r": f(np.concatenate([np.asarray(inputs["w_router_group"][0])] +
                                [np.asarray(inputs["w_router_expert"][0][g]) for g in range(4)], axis=1)),
        "b_r": f(np.concatenate([np.asarray(inputs["b_router_group"][0])] +
                                [np.asarray(inputs["b_router_expert"][0][g]) for g in range(4)])[None, :]),
    }
    w_gate = np.asarray(inputs["w_gate"][0], dtype=np.float32).reshape(32, KC, 128, 512).transpose(0, 2, 1, 3).reshape(32, 128, KC * 512)
    w_up = np.asarray(inputs["w_up"][0], dtype=np.float32).reshape(32, KC, 128, 512).transpose(0, 2, 1, 3).reshape(32, 128, KC * 512)
    w_down = np.asarray(inputs["w_down"][0], dtype=np.float32).reshape(32, 4, 128, D).transpose(0, 2, 1, 3).reshape(32, 128, 4 * D)
    shared["w_moe"] = np.ascontiguousarray(np.concatenate([w_gate, w_up, w_down], axis=2))
    maps = []
    for b in range(n_cores):
        m = dict(shared)
        m["x"] = f(x[b])
        m["xT"] = f(x[b].T)
        m["ccol"] = f(c[b].reshape(KC, 128).T)
        maps.append(m)
    return maps


def kernel(**inputs):
    nc = build_nc()
    maps = make_in_maps(inputs)
    res = run_bass_kernel_spmd(nc, maps, core_ids=list(range(8)))
    return np.stack([np.asarray(r["out"], dtype=np.float32) for r in res.results], axis=0)
```
